# Optimizing a Trainium2 kernel written in Bass

```python
import math
import jax, jax.numpy as jnp
from jax import lax
import numpy as np

D_MODEL = 1024
BATCH = 4
SEQ = 8192
DEPTH = 2

HG_HEADS = 4
HG_DK = 128
HG_DV = 128
HG_KWIDTH = HG_HEADS * HG_DK
HG_WIDTH = HG_HEADS * HG_DV
HG_CHUNK = 64
NSA_HEADS = 8
NSA_GROUPS = 2
NSA_HPG = NSA_HEADS // NSA_GROUPS
NSA_DH = 64
NSA_WIDTH = NSA_HEADS * NSA_DH
KV_WIDTH = NSA_GROUPS * NSA_DH
CMP_LEN = 32
CMP_STRIDE = 16
CMP_HIDDEN = 2 * NSA_DH
SEL_BLOCK = 64
N_SEL = 16
WINDOW = 512
Q_BLOCK = 128
FORCE_SCORE = 1e9
NEG_BIG = -1e30
REL_BUCKETS = 32
REL_MAX_DIST = 2048
D_FF = 2752
N_EXPERTS = 8
TOP_K = 2
D_FF_EXPERT = 3584
MOE_ROW_BLOCK = 512
N_DENSE = (DEPTH + 1) // 2
N_MOE = DEPTH // 2
DN_ALPHA = (2 * DEPTH) ** 0.25
DN_BETA = (8 * DEPTH) ** -0.25
LN_EPS = 1e-5
IN_SIZES = (HG_KWIDTH, HG_KWIDTH, HG_WIDTH, HG_WIDTH, NSA_WIDTH,
            KV_WIDTH, KV_WIDTH, KV_WIDTH, KV_WIDTH, KV_WIDTH, KV_WIDTH,
            3 * NSA_HEADS, D_MODEL, D_MODEL)
IN_WIDTH = sum(IN_SIZES)
VALUE_SLOTS = (2, 6, 8, 10)

kernel_name = "hgrn2_nsa_gated_hybrid_deepnorm_moe"


def layer_norm(x, g, b):
    xf = x.astype(jnp.float32)
    mu = jnp.mean(xf, -1, keepdims=True)
    var = jnp.mean(jnp.square(xf - mu), -1, keepdims=True)
    return ((xf - mu) * lax.rsqrt(var + LN_EPS) * g + b).astype(x.dtype)


def rel_bucket(dist):
    n = jnp.maximum(dist, 0)
    exact = REL_BUCKETS // 2
    large = exact + (jnp.log(jnp.maximum(n, exact).astype(jnp.float32) / exact)
                     / math.log(REL_MAX_DIST / exact) * (REL_BUCKETS - exact)).astype(jnp.int32)
    return jnp.where(n < exact, n, jnp.minimum(large, REL_BUCKETS - 1))


def masked_softmax(logits, mask):
    logits = jnp.where(mask, logits, NEG_BIG)
    m = jnp.max(logits, -1, keepdims=True)
    p = jnp.where(mask, jnp.exp(logits - m), 0.0)
    return p / jnp.maximum(jnp.sum(p, -1, keepdims=True), 1e-30)


def hgrn2_mix(q, f_logit, i, g, lb, norm_w):
    B, S, _ = q.shape
    f32 = jnp.float32
    nc = S // HG_CHUNK
    lb = lb.astype(f32)
    k = (1.0 - lb) * jax.nn.sigmoid(-f_logit.astype(f32))
    log_f = jnp.log1p(-k)

    def to_chunks(t, d):
        return t.astype(f32).reshape(B, nc, HG_CHUNK, HG_HEADS, d).transpose(1, 0, 3, 2, 4)

    qc, kc, vc = to_chunks(q, HG_DK), to_chunks(k, HG_DK), to_chunks(i, HG_DV)
    bc = jnp.cumsum(to_chunks(log_f, HG_DK), axis=3)
    causal = jnp.tril(jnp.ones((HG_CHUNK, HG_CHUNK), bool))[:, :, None]

    def step(state, inp):
        qt, kt, vt, bt = inp
        o_inter = jnp.einsum('bhtd,bhdv->bhtv', qt * jnp.exp(bt), state)
        diff = jnp.where(causal, bt[:, :, :, None, :] - bt[:, :, None, :, :], NEG_BIG)
        att = jnp.einsum('bhtsd,bhsd->bhts', qt[:, :, :, None, :] * jnp.exp(diff), kt)
        o_intra = jnp.einsum('bhts,bhsv->bhtv', att, vt)
        b_last = bt[:, :, -1:, :]
        state = (jnp.exp(b_last[:, :, 0, :, None]) * state
                 + jnp.einsum('bhsd,bhsv->bhdv', kt * jnp.exp(b_last - bt), vt))
        return state, o_inter + o_intra

    s0 = jnp.zeros((B, HG_HEADS, HG_DK, HG_DV), f32)
    _, o = lax.scan(step, s0, (qc, kc, vc, bc))
    o = o.transpose(1, 0, 3, 2, 4).reshape(B, S, HG_HEADS, HG_DV)
    o = o * lax.rsqrt(jnp.mean(o * o, -1, keepdims=True) + 1e-6)
    o = o.reshape(B, S, HG_WIDTH) * norm_w * jax.nn.silu(g.astype(f32))
    return o.astype(q.dtype)


def compress_blocks(kv, pos, w1, w2):
    B, S = kv.shape[:2]
    nc = (S - CMP_LEN) // CMP_STRIDE + 1
    idx = np.arange(nc)[:, None] * CMP_STRIDE + np.arange(CMP_LEN)[None, :]
    blk = kv[:, idx] + pos[:, None, :]
    blk = blk.transpose(0, 3, 1, 2, 4).reshape(B, NSA_GROUPS, nc, CMP_LEN * NSA_DH)
    return jax.nn.gelu(blk @ w1) @ w2


def nsa_mix(q, kc_raw, vc_raw, ks, vs, kw, vw, gate_logit, cmp_pos, cmp_w1, cmp_w2, rel_bias):
    B, S, _ = q.shape
    f32 = jnp.float32
    G, P, dh = NSA_GROUPS, NSA_HPG, NSA_DH
    nc = (S - CMP_LEN) // CMP_STRIDE + 1
    ns = S // SEL_BLOCK
    n_sel = min(N_SEL, ns)
    scale = NSA_DH ** -0.5

    def kv_heads(t):
        return t.reshape(B, S, G, dh).transpose(0, 2, 1, 3)

    qh = q.reshape(B, S, G, P, dh).transpose(0, 2, 3, 1, 4)
    k_cmp = compress_blocks(kc_raw.reshape(B, S, G, dh), cmp_pos[0], cmp_w1[0], cmp_w2[0])
    v_cmp = compress_blocks(vc_raw.reshape(B, S, G, dh), cmp_pos[1], cmp_w1[1], cmp_w2[1]).astype(f32)
    ks_blk = kv_heads(ks).reshape(B, G, ns, SEL_BLOCK, dh)
    vs_blk = kv_heads(vs).reshape(B, G, ns, SEL_BLOCK, dh)
    pad = ((0, 0), (0, 0), (WINDOW, 0), (0, 0))
    kw_pad = jnp.pad(kv_heads(kw), pad)
    vw_pad = jnp.pad(kv_heads(vw), pad)
    gates = jax.nn.sigmoid(gate_logit.astype(f32)).reshape(B, S, G, P, 3).transpose(0, 2, 3, 1, 4)
    table = rel_bias.astype(f32).reshape(REL_BUCKETS, G, P).transpose(1, 0, 2)
    cmp_end = jnp.arange(nc) * CMP_STRIDE + CMP_LEN - 1
    cs = np.arange(nc)[:, None] * CMP_STRIDE
    ss = np.arange(ns)[None, :] * SEL_BLOCK
    overlap = np.clip(np.minimum(cs + CMP_LEN, ss + SEL_BLOCK) - np.maximum(cs, ss), 0, None) / CMP_LEN
    cmp_to_sel = jnp.asarray(overlap, f32)
    b_idx = jnp.arange(B)[:, None, None, None]
    g_idx = jnp.arange(G)[None, :, None, None]

    def bias_2d(dist):
        return table[:, rel_bucket(dist), :].transpose(0, 3, 1, 2)

    def block(qb):
        t0 = qb * Q_BLOCK
        tpos = t0 + jnp.arange(Q_BLOCK)
        qq = lax.dynamic_slice_in_dim(qh, t0, Q_BLOCK, axis=3)
        gg = lax.dynamic_slice_in_dim(gates, t0, Q_BLOCK, axis=3)
        dist_c = tpos[:, None] - cmp_end[None, :]
        lc = jnp.einsum('bgpqd,bgnd->bgpqn', qq, k_cmp, preferred_element_type=f32) * scale + bias_2d(dist_c)
        pc = masked_softmax(lc, dist_c >= 0)
        o_cmp = jnp.einsum('bgpqn,bgnd->bgpqd', pc, v_cmp)
        imp = jnp.einsum('bgpqn,ns->bgqs', pc, cmp_to_sel)
        sblk = jnp.arange(ns)[None, :]
        cur = (tpos // SEL_BLOCK)[:, None]
        forced = (sblk == 0) | (sblk == cur) | (sblk == cur - 1)
        score = jnp.where(forced, FORCE_SCORE, jnp.where(sblk <= cur, imp, NEG_BIG))
        _, sel = lax.top_k(score, n_sel)
        k_sel = ks_blk[b_idx, g_idx, sel].reshape(B, G, Q_BLOCK, n_sel * SEL_BLOCK, dh)
        v_sel = vs_blk[b_idx, g_idx, sel].reshape(B, G, Q_BLOCK, n_sel * SEL_BLOCK, dh).astype(f32)
        kpos = (sel[..., None] * SEL_BLOCK + jnp.arange(SEL_BLOCK)).reshape(B, G, Q_BLOCK, n_sel * SEL_BLOCK)
        dist_s = tpos[None, None, :, None] - kpos
        bias_s = table[g_idx, rel_bucket(dist_s)].transpose(0, 1, 4, 2, 3)
        ls = jnp.einsum('bgpqd,bgqkd->bgpqk', qq, k_sel, preferred_element_type=f32) * scale + bias_s
        ps = masked_softmax(ls, (dist_s >= 0)[:, :, None])
        o_sel = jnp.einsum('bgpqk,bgqkd->bgpqd', ps, v_sel)
        k_win = lax.dynamic_slice_in_dim(kw_pad, t0, WINDOW + Q_BLOCK, axis=2)
        v_win = lax.dynamic_slice_in_dim(vw_pad, t0, WINDOW + Q_BLOCK, axis=2).astype(f32)
        kpos_w = t0 - WINDOW + jnp.arange(WINDOW + Q_BLOCK)
        dist_w = tpos[:, None] - kpos_w[None, :]
        valid_w = (dist_w >= 0) & (dist_w < WINDOW) & (kpos_w[None, :] >= 0)
        lw = jnp.einsum('bgpqd,bgkd->bgpqk', qq, k_win, preferred_element_type=f32) * scale + bias_2d(dist_w)
        pw = masked_softmax(lw, valid_w)
        o_win = jnp.einsum('bgpqk,bgkd->bgpqd', pw, v_win)
        return gg[..., 0:1] * o_cmp + gg[..., 1:2] * o_sel + gg[..., 2:3] * o_win

    o = lax.map(block, jnp.arange(S // Q_BLOCK))
    o = o.transpose(1, 0, 4, 2, 3, 5).reshape(B, S, NSA_WIDTH)
    return o.astype(q.dtype)


def token_mixer(h, w_in, lb, hg_norm_w, cmp_pos, cmp_w1, cmp_w2, rel_bias, w_branch_a, w_branch_b, w_out):
    split_points = [int(c) for c in np.cumsum(IN_SIZES)[:-1]]
    proj = h @ w_in
    (hq, hf, hi, hg, nq, kcr, vcr, ksl, vsl, kwn, vwn, ngate, gate_a, gate_b) = jnp.split(proj, split_points, axis=-1)
    o_a = hgrn2_mix(hq, hf, hi, hg, lb, hg_norm_w)
    o_b = nsa_mix(nq, kcr, vcr, ksl, vsl, kwn, vwn, ngate, cmp_pos, cmp_w1, cmp_w2, rel_bias)
    y = jax.nn.sigmoid(gate_a) * (o_a @ w_branch_a) + jax.nn.sigmoid(gate_b) * (o_b @ w_branch_b)
    return y @ w_out


def swiglu(x, wg, wu, wd):
    return (jax.nn.silu(x @ wg) * (x @ wu)) @ wd


def moe_swiglu(x, w_router, wg, wu, wd):
    T, D = x.shape
    TK = T * TOP_K
    logits = jnp.matmul(x, w_router, preferred_element_type=jnp.float32)
    top_v, top_e = lax.top_k(logits, TOP_K)
    gate = jax.nn.softmax(top_v, -1).astype(x.dtype)
    flat_e = top_e.reshape(-1)
    flat_tok = jnp.arange(TK) // TOP_K
    order = jnp.argsort(flat_e)
    e_sorted = flat_e[order]
    tok_sorted = flat_tok[order]
    gate_sorted = gate.reshape(-1)[order]
    counts = jnp.bincount(flat_e, length=N_EXPERTS)
    padded = (counts + MOE_ROW_BLOCK - 1) // MOE_ROW_BLOCK * MOE_ROW_BLOCK
    start = jnp.cumsum(counts) - counts
    pstart = jnp.cumsum(padded) - padded
    dest = pstart[e_sorted] + jnp.arange(TK) - start[e_sorted]
    n_blocks = -(-(TK + N_EXPERTS * (MOE_ROW_BLOCK - 1)) // MOE_ROW_BLOCK)
    n_rows = n_blocks * MOE_ROW_BLOCK
    row_tok = jnp.zeros((n_rows,), jnp.int32).at[dest].set(tok_sorted)
    blk_e = jnp.minimum(jnp.searchsorted(jnp.cumsum(padded), jnp.arange(n_blocks) * MOE_ROW_BLOCK, side='right'),
                        N_EXPERTS - 1)
    xs = x[row_tok].reshape(n_blocks, MOE_ROW_BLOCK, D)
    ys = lax.map(lambda a: swiglu(a[0], wg[a[1]], wu[a[1]], wd[a[1]]), (xs, blk_e))
    ys = ys.reshape(n_rows, D)[dest] * gate_sorted[:, None]
    return jnp.zeros_like(x).at[tok_sorted].add(ys)


def setup_inputs(seed: int = 0) -> dict:
    key = jax.random.key(seed)
    k = jax.random.split(key, 22)
    f32 = jnp.float32

    def nrm(kk, shape, s):
        return jax.random.normal(kk, shape, f32) * s

    col_scale = jnp.asarray(np.concatenate(
        [np.full((n,), DN_BETA if j in VALUE_SLOTS else 1.0, np.float32) for j, n in enumerate(IN_SIZES)]))
    return {
        "x": nrm(k[0], (BATCH, SEQ, D_MODEL), 1.0),
        "w_in": nrm(k[1], (DEPTH, D_MODEL, IN_WIDTH), D_MODEL ** -0.5) * col_scale,
        "hg_lb_logits": nrm(k[2], (DEPTH, HG_KWIDTH), 0.5),
        "hg_norm_w": 1.0 + nrm(k[3], (DEPTH, HG_WIDTH), 0.02),
        "cmp_pos": nrm(k[4], (DEPTH, 2, CMP_LEN, NSA_DH), 0.1),
        "cmp_w1": nrm(k[5], (DEPTH, 2, CMP_LEN * NSA_DH, CMP_HIDDEN), (CMP_LEN * NSA_DH) ** -0.5),
        "cmp_w2": nrm(k[6], (DEPTH, 2, CMP_HIDDEN, NSA_DH), CMP_HIDDEN ** -0.5),
        "rel_bias": nrm(k[7], (REL_BUCKETS, NSA_HEADS), 0.3),
        "w_branch_a": nrm(k[8], (DEPTH, HG_WIDTH, D_MODEL), HG_WIDTH ** -0.5 * DN_BETA),
        "w_branch_b": nrm(k[9], (DEPTH, NSA_WIDTH, D_MODEL), NSA_WIDTH ** -0.5 * DN_BETA),
        "w_out": nrm(k[10], (DEPTH, D_MODEL, D_MODEL), D_MODEL ** -0.5 * DN_BETA),
        "ln1_g": 1.0 + nrm(k[11], (DEPTH, D_MODEL), 0.02),
        "ln1_b": nrm(k[12], (DEPTH, D_MODEL), 0.02),
        "ln2_g": 1.0 + nrm(k[13], (DEPTH, D_MODEL), 0.02),
        "ln2_b": nrm(k[14], (DEPTH, D_MODEL), 0.02),
        "ffn_w_gate": nrm(k[15], (N_DENSE, D_MODEL, D_FF), D_MODEL ** -0.5),
        "ffn_w_up": nrm(k[16], (N_DENSE, D_MODEL, D_FF), D_MODEL ** -0.5 * DN_BETA),
        "ffn_w_down": nrm(k[17], (N_DENSE, D_FF, D_MODEL), D_FF ** -0.5 * DN_BETA),
        "moe_router": nrm(k[18], (N_MOE, D_MODEL, N_EXPERTS), D_MODEL ** -0.5),
        "moe_w_gate": nrm(k[19], (N_MOE, N_EXPERTS, D_MODEL, D_FF_EXPERT), D_MODEL ** -0.5),
        "moe_w_up": nrm(k[20], (N_MOE, N_EXPERTS, D_MODEL, D_FF_EXPERT), D_MODEL ** -0.5 * DN_BETA),
        "moe_w_down": nrm(k[21], (N_MOE, N_EXPERTS, D_FF_EXPERT, D_MODEL), D_FF_EXPERT ** -0.5 * DN_BETA),
    }


def reference(x, w_in, hg_lb_logits, hg_norm_w, cmp_pos, cmp_w1, cmp_w2, rel_bias, w_branch_a, w_branch_b,
              w_out, ln1_g, ln1_b, ln2_g, ln2_b, ffn_w_gate, ffn_w_up, ffn_w_down, moe_router, moe_w_gate,
              moe_w_up, moe_w_down):
    B, S, D = x.shape
    p_lb = jax.nn.softmax(hg_lb_logits.astype(jnp.float32), axis=0)
    lbs = jnp.cumsum(p_lb, axis=0) - p_lb[0]
    for l in range(DEPTH):
        mix = token_mixer(x, w_in[l], lbs[l], hg_norm_w[l], cmp_pos[l], cmp_w1[l], cmp_w2[l], rel_bias,
                          w_branch_a[l], w_branch_b[l], w_out[l])
        x = layer_norm(DN_ALPHA * x + mix, ln1_g[l], ln1_b[l])
        h = x.reshape(B * S, D)
        if l % 2 == 0:
            f = swiglu(h, ffn_w_gate[l // 2], ffn_w_up[l // 2], ffn_w_down[l // 2])
        else:
            f = moe_swiglu(h, moe_router[l // 2], moe_w_gate[l // 2], moe_w_up[l // 2], moe_w_down[l // 2])
        x = layer_norm(DN_ALPHA * x + f.reshape(B, S, D), ln2_g[l], ln2_b[l])
    return x
```

```python
import numpy as np
import concourse.bass as bass
import concourse.mybir as mybir

F32 = mybir.dt.float32
BF16 = mybir.dt.bfloat16
I32 = mybir.dt.int32
AF = mybir.ActivationFunctionType
ALU = mybir.AluOpType
AX = mybir.AxisListType

ENGS = ("pe", "act", "dve", "pool", "sp")


class Op:
    __slots__ = ("eng", "fn", "deps", "is_dma", "idx", "inc", "semval", "dma_id", "dma_prev", "cc_id")

    def __init__(self, eng, fn, is_dma):
        self.eng = eng
        self.fn = fn
        self.deps = set()
        self.is_dma = is_dma
        self.inc = False
        self.semval = 0
        self.dma_id = -1
        self.dma_prev = None
        self.cc_id = -1


class Prog:
    NDMA_SEM = 24

    def __init__(self, nc, same_engine_sync=True):
        self.nc = nc
        self.ops = []
        self.last_w = {}
        self.readers = {}
        self.same_engine_sync = same_engine_sync
        self.n_dma = 0
        self.dma_ops = []
        self.cc_ops = []
        self._n = 0

    def sb(self, shape, dt, name=None):
        self._n += 1
        return self.nc.alloc_sbuf_tensor(name or f"sb{self._n}", list(shape), dt)

    def ps(self, shape, dt=F32, name=None):
        self._n += 1
        n = 2048 // (4 if dt == F32 else 2)
        assert shape[1] <= n
        t = self.nc.alloc_psum_tensor(f"psb{self._n}", [128, n], dt)
        return t[0:shape[0], 0:shape[1]]

    def op(self, eng, fn, reads=(), writes=(), dma=False):
        o = Op(eng, fn, dma)
        o.idx = len(self.ops)
        bank_r = [r for r in reads if isinstance(r, tuple) and r[0] == "bank"]
        if bank_r:
            reads = [r for r in reads if r not in bank_r]
            writes = list(writes) + bank_r
        for r in reads:
            w = self.last_w.get(r)
            if w is not None:
                o.deps.add(w)
        for w_ in writes:
            w = self.last_w.get(w_)
            if w is not None:
                o.deps.add(w)
            for r in self.readers.get(w_, ()):
                o.deps.add(r)
        o.deps.discard(o.idx)
        for r in reads:
            self.readers.setdefault(r, []).append(o.idx)
        for w_ in writes:
            self.last_w[w_] = o.idx
            self.readers[w_] = []
        if dma:
            o.dma_id = self.n_dma
            self.n_dma += 1
            self.dma_ops.append(o.idx)
            if o.dma_id >= self.NDMA_SEM:
                o.dma_prev = self.dma_ops[o.dma_id - self.NDMA_SEM]
        self.ops.append(o)
        return o

    def dma(self, eng, out, in_, reads=(), writes=(), **kw):
        return self.op(eng, lambda e: e.dma_start(out=out, in_=in_, **kw), reads, writes, dma=True)

    def cc(self, fn, reads=(), writes=()):
        o = self.op("pool", fn, reads, writes)
        o.cc_id = len(self.cc_ops)
        self.cc_ops.append(o.idx)
        return o

    def barrier(self, wait_cc=True):
        last = {}
        for o in self.ops:
            if o.fn is not None and not o.is_dma and o.cc_id < 0 and o.eng != "sp":
                last[o.eng] = o.idx
        deps = set(last.values()) | set(self.dma_ops[-self.NDMA_SEM:])
        if wait_cc:
            deps |= set(self.cc_ops)
        for e in ENGS:
            b = Op(e, None, False)
            b.idx = len(self.ops)
            b.deps = set(deps)
            self.ops.append(b)
        keep = {t: w for t, w in self.last_w.items() if t in getattr(self, "persist", ())}
        self.last_w = keep
        self.readers = {}

    def emit(self):
        nc = self.nc
        ops = self.ops
        fin = Op("sp", None, False)
        fin.idx = len(ops)
        fin.deps = set(self.dma_ops[-self.NDMA_SEM:]) | {i for i in self.dma_ops}
        ops.append(fin)
        for o in ops:
            nd = set()
            for d in o.deps:
                p = ops[d]
                if p.is_dma or p.cc_id >= 0:
                    nd.add(d)
                    continue
                if p.eng == o.eng and not o.is_dma:
                    if p.eng == "pe" or p.eng == "sp" or not self.same_engine_sync:
                        continue
                nd.add(d)
            if o.dma_prev is not None:
                nd.add(o.dma_prev)
            o.deps = nd
            for d in nd:
                ops[d].inc = True
        val = {e: 0 for e in ENGS}
        for o in ops:
            if o.is_dma:
                o.semval = 16 * (o.dma_id // self.NDMA_SEM + 1)
            elif o.cc_id >= 0:
                o.semval = 1
            elif o.inc:
                val[o.eng] += 1
                o.semval = val[o.eng]
        self.maxval = dict(val)
        sem = {e: nc.alloc_semaphore(f"s_{e}") for e in ENGS if e != "sp"}
        dsem = [nc.alloc_semaphore(f"s_dma{i}") for i in range(min(self.NDMA_SEM, max(1, self.n_dma)))]
        csem = [nc.alloc_semaphore(f"s_cc{i}") for i in range(len(self.cc_ops))]
        per = {e: [o for o in ops if o.eng == e] for e in ENGS}

        def run(e, engobj):
            seen = {}
            for o in per[e]:
                need = {}
                for d in o.deps:
                    p = ops[d]
                    key = ("d", p.dma_id % self.NDMA_SEM) if p.is_dma else (("c", p.cc_id) if p.cc_id >= 0 else ("e", p.eng))
                    if p.semval > need.get(key, 0):
                        need[key] = p.semval
                for key, v in need.items():
                    if seen.get(key, 0) >= v:
                        continue
                    seen[key] = v
                    s = dsem[key[1]] if key[0] == "d" else (csem[key[1]] if key[0] == "c" else sem[key[1]])
                    engobj.wait_ge(s, v)
                if o.fn is None:
                    continue
                ins = o.fn(engobj)
                if o.is_dma:
                    ins.then_inc(dsem[o.dma_id % self.NDMA_SEM], 16)
                elif o.cc_id >= 0:
                    ins.then_inc(csem[o.cc_id])
                elif o.inc:
                    ins.then_inc(sem[e], 1)

        with nc.Block() as block:
            if per["pe"]:
                block.tensor(lambda t: run("pe", t))
            if per["act"]:
                block.scalar(lambda t: run("act", t))
            if per["dve"]:
                block.vector(lambda t: run("dve", t))
            if per["pool"]:
                block.gpsimd(lambda t: run("pool", t))
            block.sync(lambda t: run("sp", t))


D_MODEL = 1024
DEPTH = 2
DN_ALPHA = (2 * DEPTH) ** 0.25
LN_EPS = 1e-5
D_FF = 2752


def new_nc():
    return bass.Bass("TRN2", target_bir_lowering=False)


class Ctx:
    def __init__(self, nc=None, P=None, banks=None, prefix="", ov=None):
        self.standalone = nc is None
        self.nc = nc if nc is not None else new_nc()
        self.P = P if P is not None else Prog(self.nc)
        self.banks = banks if banks is not None else [self.P.ps([128, 512]) for _ in range(8)]
        self.prefix = prefix
        self.ov = ov or {}
        if not hasattr(self.P, "_tens"):
            self.P._tens = {}

    def inp(self, name, shape, dt=F32, shared=False):
        if name in self.ov:
            return self.ov[name]
        full = name if shared else self.prefix + name
        if full not in self.P._tens:
            self.P._tens[full] = self.nc.dram_tensor(full, list(shape), dt, kind="ExternalInput").ap()
        return self.P._tens[full]

    def out(self, name, shape, dt=F32):
        if name in self.ov:
            return self.ov[name]
        return self.nc.dram_tensor(name, list(shape), dt, kind="ExternalOutput").ap()

    def done(self):
        if self.standalone:
            self.P.emit()
            return self.nc
        return None


def mm_acc(P, out, pairs, reads, writes):
    n = len(pairs)
    for i, (l, r) in enumerate(pairs):
        P.op("pe", (lambda e, l=l, r=r, i=i: e.matmul(out, l, r, start=(i == 0), stop=(i == n - 1))),
             reads=reads[i] if (len(reads) and isinstance(reads[0], list)) else reads, writes=writes)


def load_w_bf16(P, dst, w, K, N, tok, nsplit=4):
    kc_full = K // 128
    step = max(1, (kc_full + nsplit - 1) // nsplit)
    for k0 in range(0, kc_full, step):
        k1 = min(kc_full, k0 + step)
        P.dma("pool", dst[:, k0:k1, :], w[k0 * 128:k1 * 128, :].rearrange("(c p) n -> p c n", p=128), writes=[tok])
    rem = K - kc_full * 128
    if rem:
        P.dma("pool", dst[0:rem, kc_full, :], w[kc_full * 128:K, :], writes=[tok])


def load_w_cols(P, dst, w, K, c0, c1, tok):
    kc_full = K // 128
    P.dma("pool", dst[:, 0:kc_full, c0:c1], w[0:kc_full * 128, c0:c1].rearrange("(c p) n -> p c n", p=128), writes=[tok])
    rem = K - kc_full * 128
    if rem:
        P.dma("pool", dst[0:rem, kc_full, c0:c1], w[kc_full * 128:K, c0:c1], writes=[tok])


def layer_norm_T(P, xf, b, NT, ones_b, rb, rsq, ps_mean, ps_msq, st, lng, lnb, tag,
                 rbt=lambda mo: ("rb", mo), rsqt=lambda mo: ("rsq", mo)):
    for mo in range(8):
        P.op("act", lambda e, mo=mo: e.activation(out=rb[:, mo, :], in_=xf[:, mo, :], func=AF.Copy),
             reads=[("xf", b, mo)], writes=[rbt(mo)])
        P.op("act", lambda e, mo=mo: e.activation(out=rsq[:, mo, :], in_=xf[:, mo, :], func=AF.Square),
             reads=[("xf", b, mo)], writes=[rsqt(mo)])
    mm_acc(P, ps_mean, [(ones_b[:], rb[:, mo, :]) for mo in range(8)],
           [[rbt(mo), "ones"] for mo in range(8)], [("bank", 6)])
    mm_acc(P, ps_msq, [(ones_b[:], rsq[:, mo, :]) for mo in range(8)],
           [[rsqt(mo), "ones"] for mo in range(8)], [("bank", 7)])
    mean_s, m2, rstd = st[:, 0, :], st[:, 1, :], st[:, 2, :]
    P.op("dve", lambda e: e.tensor_copy(out=mean_s, in_=ps_mean), reads=[("bank", 6)], writes=["mean_s"])
    P.op("dve", lambda e: e.tensor_tensor(out=m2, in0=mean_s, in1=mean_s, op=ALU.mult), reads=["mean_s"], writes=["m2"])
    P.op("dve", lambda e: e.tensor_tensor(out=rstd, in0=ps_msq, in1=m2, op=ALU.subtract), reads=[("bank", 7), "m2"], writes=["rstd"])
    P.op("dve", lambda e: e.tensor_scalar(out=rstd, in0=rstd, scalar1=LN_EPS, scalar2=None, op0=ALU.add),
         reads=["rstd"], writes=["rstd"])
    P.op("act", lambda e: e.activation(out=rstd, in_=rstd, func=AF.Sqrt), reads=["rstd"], writes=["rstd"])
    P.op("dve", lambda e: e.reciprocal(out=rstd, in_=rstd), reads=["rstd"], writes=["rstd"])
    for mo in range(8):
        P.op("dve", lambda e, mo=mo: e.tensor_tensor(out=xf[:, mo, :], in0=xf[:, mo, :], in1=mean_s, op=ALU.subtract),
             reads=[("xf", b, mo), "mean_s"], writes=[("xf", b, mo)])
        P.op("dve", lambda e, mo=mo: e.tensor_tensor(out=xf[:, mo, :], in0=xf[:, mo, :], in1=rstd, op=ALU.mult),
             reads=[("xf", b, mo), "rstd"], writes=[("xf", b, mo)])
        P.op("dve", lambda e, mo=mo: e.tensor_scalar(out=xf[:, mo, :], in0=xf[:, mo, :], scalar1=lng[:, mo:mo + 1],
                                                      scalar2=lnb[:, mo:mo + 1], op0=ALU.mult, op1=ALU.add),
             reads=[("xf", b, mo), "lnp"], writes=[("xf", b, mo)])


def build_ffn(T, NT=512, cx=None):
    cx = cx or Ctx()
    nc, P = cx.nc, cx.P
    xT = cx.inp("xT", [1024, T])
    wg = cx.inp("wg", [1024, D_FF]); wu = cx.inp("wu", [1024, D_FF]); wd = cx.inp("wd", [D_FF, 1024])
    lnp = cx.inp("lnp", [128, 16])
    outT = cx.out("outT", [1024, T])
    outb = cx.ov.get("outb")
    MT = 22
    wgb = P.sb([128, 8, D_FF], BF16)
    wub = P.sb([128, 8, D_FF], BF16)
    wdb = P.sb([128, MT, 1024], BF16)
    lns = P.sb([128, 16], F32)
    ones_b = P.sb([128, 128], BF16)
    xb = [P.sb([128, 8, NT], BF16) for _ in range(2)]
    xf1 = P.sb([128, 8, NT], F32)
    xf = [xf1, xf1]
    h = P.sb([128, MT, NT], BF16)
    sg = [P.sb([128, NT], F32) for _ in range(2)]
    rb = h[:, 0:8, :]
    rsq = h[:, 8:16, :]
    st = P.sb([128, 3, NT], F32)
    bk = [b_[:, 0:NT] for b_ in cx.banks]
    psg, psu, psf, ps_mean, ps_msq = bk[0:2], bk[2:4], bk[4:6], bk[6], bk[7]
    BG, BU, BF_, BMEAN, BMSQ = (0, 1), (2, 3), (4, 5), 6, 7

    P.dma("sp", lns[:], lnp, writes=["lnp"])
    P.op("dve", lambda e: e.memset(ones_b[:], 1.0 / 1024), writes=["ones"])
    xTv = xT.rearrange("(c p) t -> p c t", p=128)
    oTv = outT.rearrange("(c p) t -> p c t", p=128)
    ntiles = T // NT

    def load_x(ti):
        b = ti % 2
        P.dma("pool", xb[b][:], xTv[:, :, ti * NT:(ti + 1) * NT], writes=[("xb", b)])

    def load_xf(ti):
        P.dma("sp", xf1[:], xTv[:, :, ti * NT:(ti + 1) * NT], writes=[("xf", 0, mo) for mo in range(8)])

    load_x(0)
    load_xf(0)
    WB = 6
    for blk in range((MT + WB - 1) // WB):
        c0, c1 = blk * WB * 128, min(D_FF, (blk + 1) * WB * 128)
        load_w_cols(P, wgb, wg, 1024, c0, c1, ("wg", blk))
        load_w_cols(P, wub, wu, 1024, c0, c1, ("wu", blk))
    load_w_bf16(P, wdb, wd, D_FF, 1024, "wd")
    for ti in range(ntiles):
        b = ti % 2
        if ti + 1 < ntiles:
            load_x(ti + 1)
        for m in range(MT):
            msz = min(128, D_FF - m * 128)
            ms = slice(m * 128, m * 128 + msz)
            pb = m % 2
            mm_acc(P, psg[pb][0:msz, :], [(wgb[:, kc, ms], xb[b][:, kc, :]) for kc in range(8)],
                   [("wg", m // WB), ("xb", b)], [("bank", BG[pb])])
            mm_acc(P, psu[pb][0:msz, :], [(wub[:, kc, ms], xb[b][:, kc, :]) for kc in range(8)],
                   [("wu", m // WB), ("xb", b)], [("bank", BU[pb])])
            P.op("act", lambda e, pb=pb, msz=msz: e.activation(out=sg[pb][0:msz, :], in_=psg[pb][0:msz, :], func=AF.Silu),
                 reads=[("bank", BG[pb])], writes=[("sg", pb)])
            P.op("dve", lambda e, pb=pb, msz=msz, m=m: e.tensor_tensor(out=h[0:msz, m, :], in0=sg[pb][0:msz, :],
                                                                       in1=psu[pb][0:msz, :], op=ALU.mult),
                 reads=[("sg", pb), ("bank", BU[pb])], writes=[("h", m)])
        for mo in range(8):
            pb = mo % 2
            pairs, rds = [], []
            for kc in range(MT):
                ksz = min(128, D_FF - kc * 128)
                pairs.append((wdb[0:ksz, kc, mo * 128:(mo + 1) * 128], h[0:ksz, kc, :]))
                rds.append(["wd", ("h", kc)])
            mm_acc(P, psf[pb], pairs, rds, [("bank", BF_[pb])])
            P.op("dve", lambda e, pb=pb, mo=mo: e.scalar_tensor_tensor(
                out=xf1[:, mo, :], in0=xf1[:, mo, :], scalar=float(DN_ALPHA), in1=psf[pb],
                op0=ALU.mult, op1=ALU.add), reads=[("bank", BF_[pb]), ("xf", 0, mo)], writes=[("xf", 0, mo)])
        layer_norm_T(P, xf1, 0, NT, ones_b, rb, rsq, ps_mean, ps_msq, st, lns[:, 0:8], lns[:, 8:16], "ln2",
                     rbt=lambda mo: ("h", mo), rsqt=lambda mo: ("h", 8 + mo))
        P.dma("sp", oTv[:, :, ti * NT:(ti + 1) * NT], xf1[:], reads=[("xf", 0, mo) for mo in range(8)])
        if outb is not None:
            P.dma("pool", outb(ti * NT, NT), xf1[:], reads=[("xf", 0, mo) for mo in range(8)], writes=cx.ov.get("otok", lambda t0: [])(ti * NT))
            if cx.ov.get("after_store") is not None:
                cx.ov["after_store"]((ti + 1) * NT)
        if ti + 1 < ntiles:
            load_xf(ti + 1)
    return cx.done()


def build_merge(T, NT=512, cx=None):
    cx = cx or Ctx()
    nc, P = cx.nc, cx.P
    xT = cx.inp("xT", [1024, T])
    osrc = cx.ov.get("osrc")
    if osrc is None:
        oaT = cx.inp("oaT", [512, T], BF16); obT = cx.inp("obT", [512, T], BF16)
    wgt = cx.inp("wgt", [1024, 2048]); wa = cx.inp("wa", [512, 1024]); wb = cx.inp("wb", [512, 1024]); wo = cx.inp("wo", [1024, 1024])
    lnp = cx.inp("lnp", [128, 16])
    outT = cx.out("outT", [1024, T])
    wgtb = P.sb([128, 8, 2048], BF16)
    wab = P.sb([128, 4, 1024], BF16)
    wbb = P.sb([128, 4, 1024], BF16)
    wob = P.sb([128, 8, 1024], BF16)
    lns = P.sb([128, 16], F32)
    ones_b = P.sb([128, 128], BF16)
    xb = [P.sb([128, 8, NT], BF16) for _ in range(2)]
    xf = [P.sb([128, 8, NT], F32) for _ in range(2)]
    oa = [P.sb([128, 4, NT], BF16) for _ in range(2)]
    ob = [P.sb([128, 4, NT], BF16) for _ in range(2)]
    y = P.sb([128, 8, NT], BF16)
    sga = [P.sb([128, NT], F32) for _ in range(2)]
    sgb = [P.sb([128, NT], F32) for _ in range(2)]
    rb = P.sb([128, 8, NT], BF16)
    rsq = P.sb([128, 8, NT], BF16)
    st = P.sb([128, 3, NT], F32)
    bk = [b_[:, 0:NT] for b_ in cx.banks]
    pq, pmx, ps_mean, ps_msq = bk[0:4], bk[4:6], bk[6], bk[7]
    P.dma("sp", lns[:], lnp, writes=["lnp"])
    P.op("dve", lambda e: e.memset(ones_b[:], 1.0 / 1024), writes=["ones"])
    xTv = xT.rearrange("(c p) t -> p c t", p=128)
    if osrc is None:
        oaTv = oaT.rearrange("(c p) t -> p c t", p=128)
        obTv = obT.rearrange("(c p) t -> p c t", p=128)
        osrc = lambda which, t0, n: [(0, 4, (oaTv if which == "a" else obTv)[:, :, t0:t0 + n])]
    oTv = outT.rearrange("(c p) t -> p c t", p=128)
    ntiles = T // NT

    selh = cx.ov.get("selh")
    if selh is not None:
        sels = P.sb([128, 2], F32)
        P.dma("sp", sels[:], selh, writes=["sels"])
        cand = {(w_, h_): P.sb([128, 4, NT], BF16) for w_ in "ab" for h_ in range(2)}

    def load_x(ti):
        b = ti % 2
        ts = slice(ti * NT, (ti + 1) * NT)
        P.dma("pool", xb[b][:], xTv[:, :, ts], writes=[("xb", b)])
        P.dma("sp", xf[b][:], xTv[:, :, ts], writes=[("xf", b, mo) for mo in range(8)])
        if selh is not None:
            for w_, dst_, tk_ in (("a", oa, "oa"), ("b", ob, "ob")):
                for h_ in range(2):
                    for (c0, c1, ap_) in cx.ov["osrc2"](w_, h_, ti * NT, NT):
                        P.dma("sp", cand[(w_, h_)][:, c0:c1, :], ap_, reads=cx.ov.get("ctok", lambda h, t0: [])(h_, ti * NT),
                              writes=[("cand", w_, h_)])
                P.op("dve", lambda e, w_=w_: e.tensor_scalar(out=cand[(w_, 0)][:], in0=cand[(w_, 0)][:], scalar1=sels[:, 0:1], scalar2=None, op0=ALU.mult),
                     reads=["sels", ("cand", w_, 0)], writes=[("cand", w_, 0)])
                P.op("dve", lambda e, w_=w_, dst_=dst_, b=b: e.scalar_tensor_tensor(out=dst_[b][:], in0=cand[(w_, 1)][:], scalar=sels[:, 1:2], in1=cand[(w_, 0)][:],
                                                                                 op0=ALU.mult, op1=ALU.add),
                     reads=["sels", ("cand", w_, 0), ("cand", w_, 1)], writes=[(tk_, b)])
            return
        for (c0, c1, ap_) in osrc("a", ti * NT, NT):
            P.dma("sp", oa[b][:, c0:c1, :], ap_, writes=[("oa", b)])
        for (c0, c1, ap_) in osrc("b", ti * NT, NT):
            P.dma("sp", ob[b][:, c0:c1, :], ap_, writes=[("ob", b)])

    load_x(0)
    for blk in range(4):
        c0, c1 = blk * 256, (blk + 1) * 256
        load_w_cols(P, wgtb, wgt, 1024, c0, c1, ("wgt", blk))
        load_w_cols(P, wab, wa, 512, c0, c1, ("wa", blk))
        load_w_cols(P, wgtb, wgt, 1024, 1024 + c0, 1024 + c1, ("wgt", blk))
        load_w_cols(P, wbb, wb, 512, c0, c1, ("wb", blk))
    load_w_bf16(P, wob, wo, 1024, 1024, "wo", nsplit=2)
    for ti in range(ntiles):
        b = ti % 2
        if ti + 1 < ntiles:
            load_x(ti + 1)
        for mo in range(8):
            pb = mo % 2
            ms = slice(mo * 128, (mo + 1) * 128)
            ms2 = slice(1024 + mo * 128, 1024 + (mo + 1) * 128)
            mm_acc(P, pq[0], [(wgtb[:, kc, ms], xb[b][:, kc, :]) for kc in range(8)], [("wgt", mo // 2), ("xb", b)], [("bank", 0)])
            mm_acc(P, pq[1], [(wab[:, kc, ms], oa[b][:, kc, :]) for kc in range(4)], [("wa", mo // 2), ("oa", b)], [("bank", 1)])
            mm_acc(P, pq[2], [(wgtb[:, kc, ms2], xb[b][:, kc, :]) for kc in range(8)], [("wgt", mo // 2), ("xb", b)], [("bank", 2)])
            mm_acc(P, pq[3], [(wbb[:, kc, ms], ob[b][:, kc, :]) for kc in range(4)], [("wb", mo // 2), ("ob", b)], [("bank", 3)])
            P.op("act", lambda e, pb=pb: e.activation(out=sga[pb][:], in_=pq[0], func=AF.Sigmoid),
                 reads=[("bank", 0)], writes=[("sga", pb)])
            P.op("act", lambda e, pb=pb: e.activation(out=sgb[pb][:], in_=pq[2], func=AF.Sigmoid),
                 reads=[("bank", 2)], writes=[("sgb", pb)])
            P.op("dve", lambda e, pb=pb: e.tensor_tensor(out=sga[pb][:], in0=sga[pb][:], in1=pq[1], op=ALU.mult),
                 reads=[("sga", pb), ("bank", 1)], writes=[("sga", pb)])
            P.op("dve", lambda e, pb=pb: e.tensor_tensor(out=sgb[pb][:], in0=sgb[pb][:], in1=pq[3], op=ALU.mult),
                 reads=[("sgb", pb), ("bank", 3)], writes=[("sgb", pb)])
            P.op("dve", lambda e, pb=pb, mo=mo: e.tensor_tensor(out=y[:, mo, :], in0=sga[pb][:], in1=sgb[pb][:], op=ALU.add),
                 reads=[("sga", pb), ("sgb", pb)], writes=[("y", mo)])
        for mo in range(8):
            pb = mo % 2
            ms = slice(mo * 128, (mo + 1) * 128)
            mm_acc(P, pmx[pb], [(wob[:, kc, ms], y[:, kc, :]) for kc in range(8)],
                   [["wo", ("y", kc)] for kc in range(8)], [("bank", 4 + pb)])
            P.op("dve", lambda e, pb=pb, mo=mo, b=b: e.scalar_tensor_tensor(
                out=xf[b][:, mo, :], in0=xf[b][:, mo, :], scalar=float(DN_ALPHA), in1=pmx[pb],
                op0=ALU.mult, op1=ALU.add), reads=[("bank", 4 + pb), ("xf", b, mo)], writes=[("xf", b, mo)])
        layer_norm_T(P, xf[b], b, NT, ones_b, rb, rsq, ps_mean, ps_msq, st, lns[:, 0:8], lns[:, 8:16], "ln1")
        P.dma("sp", oTv[:, :, ti * NT:(ti + 1) * NT], xf[b][:], reads=[("xf", b, mo) for mo in range(8)])
    return cx.done()


def build_hgrn(S, NT=512, layer=0, cx=None):
    cx = cx or Ctx()
    nc, P = cx.nc, cx.P
    xsrc = cx.ov.get("xsrc")
    if xsrc is None:
        xT = cx.inp("xT", [1024, S])
        xTv_ = xT.rearrange("(c p) t -> p c t", p=128)
        xsrc = lambda t0, n: (xTv_[:, :, t0:t0 + n], "pool")
    wq = cx.inp("hwq", [1024, 256]); wf = cx.inp("hwf", [1024, 256]); wi = cx.inp("hwi", [1024, 256]); wg = cx.inp("hwg", [1024, 256])
    hp = cx.inp("hp", [128, 4]); lbl = cx.inp("lbl", [128, 4])
    cmask = cx.inp("cmask", [128, NT], shared=True); tri = cx.inp("tri", [64, NT], shared=True)
    ident = cx.inp("ident", [128, 128], shared=True)
    odst = cx.ov.get("odst")
    if odst is None:
        oaT = cx.out("oaT", [256, S], BF16)
        odst = lambda row0, t0, n: oaT[row0:row0 + 128, t0:t0 + n]
    NCH = NT // 64
    wqb = P.sb([128, 8, 256], BF16); wfb = P.sb([128, 8, 256], BF16)
    wib = P.sb([128, 8, 256], BF16); wgb = P.sb([128, 8, 256], BF16)
    hps = P.sb([128, 4], F32)
    cm = P.sb([128, NT], F32)
    tr = P.sb([64, NT], F32)
    idb = P.sb([128, 128], BF16)
    ones_b = P.sb([128, 128], BF16)
    xb = [P.sb([128, 8, NT], BF16) for _ in range(2)]
    banks = cx.banks
    rot = [0]

    def rbank():
        i = rot[0] % 3
        rot[0] += 1
        return i
    B_ATT = 3
    H = []
    for hd in range(2):
        d = dict(
            kk=P.sb([128, NT], F32), lf=P.sb([128, NT], F32), bb=P.sb([128, NT], F32), eb=P.sb([128, NT], F32),
            ex=P.sb([128, NT], F32), qf=P.sb([128, NT], F32), sgl=P.sb([128, NT], F32), rs=P.sb([128, NT], F32),
            qt=P.sb([128, NT], BF16), kt=P.sb([128, NT], BF16), kd=P.sb([128, NT], BF16),
            vtm=P.sb([64, NCH, 128], BF16), kdt=P.sb([64, NCH, 128], BF16), attm=P.sb([64, NT], BF16),
            Sf=P.sb([128, 128], F32), Sb=[P.sb([128, 128], BF16) for _ in range(2)], osq=P.sb([128, NT], BF16),
            ob=P.sb([128, NT], BF16))
        H.append(d)
    P.dma("sp", hps[:], hp, writes=["hp"])
    lbs_ = P.sb([128, 4], F32)
    P.dma("sp", lbs_[:], lbl, writes=["lbl"])
    if layer == 0:
        P.op("dve", lambda e: e.memset(hps[:, 0:2], 1.0), reads=["lbl"], writes=["hp"])
    else:
        P.op("dve", lambda e: e.tensor_tensor(out=hps[:, 0:2], in0=lbs_[:, 0:2], in1=lbs_[:, 2:4], op=ALU.subtract), reads=["lbl"], writes=["hp"])
        P.op("act", lambda e: e.activation(out=hps[:, 0:2], in_=hps[:, 0:2], func=AF.Sigmoid), reads=["hp"], writes=["hp"])
    P.dma("sp", cm[:], cmask, writes=["cm"])
    P.dma("sp", tr[:], tri, writes=["tr"])
    P.dma("pool", idb[:], ident, writes=["idb"])
    P.op("dve", lambda e: e.memset(ones_b[:], 1.0 / 128), writes=["ones"])
    for hd in range(2):
        P.op("dve", lambda e, hd=hd: e.memset(H[hd]["Sf"][:], 0.0), writes=[("Sf", hd)])
    ntiles = S // NT

    def load_x(ti):
        b = ti % 2
        r_ = xsrc(ti * NT, NT)
        P.dma(r_[1], xb[b][:], r_[0], reads=(r_[2] if len(r_) > 2 else []), writes=[("xb", b)])
    load_x(0)
    load_w_bf16(P, wqb, wq, 1024, 256, "wq", nsplit=1)
    load_w_bf16(P, wfb, wf, 1024, 256, "wf", nsplit=1)
    load_w_bf16(P, wib, wi, 1024, 256, "wi", nsplit=1)
    load_w_bf16(P, wgb, wg, 1024, 256, "wg", nsplit=1)
    gch = 0
    for ti in range(ntiles):
        b = ti % 2
        if ti + 1 < ntiles:
            load_x(ti + 1)
        def prep(hd):
            h = H[hd]
            hs = slice(hd * 128, (hd + 1) * 128)
            T = lambda n: (n, hd)
            B_O, B_S = 4 + hd, 6 + hd
            bi = rbank()
            mm_acc(P, banks[bi], [(wfb[:, kc, hs], xb[b][:, kc, :]) for kc in range(8)], ["wf", ("xb", b)], [("bank", bi)])
            P.op("act", lambda e, h=h, bi=bi: e.activation(out=h["kk"][:], in_=banks[bi], func=AF.Sigmoid, scale=-1.0),
                 reads=[("bank", bi)], writes=[T("kk")])
            P.op("dve", lambda e, h=h, hd=hd: e.tensor_scalar(out=h["kk"][:], in0=h["kk"][:], scalar1=hps[:, hd:hd + 1], scalar2=None, op0=ALU.mult),
                 reads=[T("kk"), "hp"], writes=[T("kk")])
            P.op("act", lambda e, h=h: e.activation(out=h["lf"][:], in_=h["kk"][:], func=AF.Ln, scale=-1.0, bias=1.0),
                 reads=[T("kk")], writes=[T("lf")])
            P.op("dve", lambda e, h=h: e.tensor_tensor_scan(out=h["bb"][:], data0=cm[:], data1=h["lf"][:], initial=0.0,
                                                            op0=ALU.mult, op1=ALU.add),
                 reads=[T("lf"), "cm"], writes=[T("bb")])
            P.op("act", lambda e, h=h: e.activation(out=h["eb"][:], in_=h["bb"][:], func=AF.Exp), reads=[T("bb")], writes=[T("eb")])
            bi = rbank()
            mm_acc(P, banks[bi], [(wqb[:, kc, hs], xb[b][:, kc, :]) for kc in range(8)], ["wq", ("xb", b)], [("bank", bi)])
            P.op("dve", lambda e, h=h, bi=bi: e.tensor_tensor(out=h["qt"][:], in0=banks[bi], in1=h["eb"][:], op=ALU.mult),
                 reads=[("bank", bi), T("eb")], writes=[T("qt")])
            P.op("act", lambda e, h=h: e.activation(out=h["ex"][:], in_=h["bb"][:], func=AF.Exp, scale=-1.0), reads=[T("bb")], writes=[T("ex")])
            P.op("dve", lambda e, h=h: e.tensor_tensor(out=h["kt"][:], in0=h["kk"][:], in1=h["ex"][:], op=ALU.mult),
                 reads=[T("kk"), T("ex")], writes=[T("kt")])
            for c in range(NCH):
                cs = slice(c * 64, (c + 1) * 64)
                P.op("act", lambda e, h=h, cs=cs, c=c: e.activation(out=h["ex"][:, cs], in_=h["bb"][:, cs], func=AF.Exp, scale=-1.0,
                                                                    bias=h["bb"][:, c * 64 + 63:c * 64 + 64]),
                     reads=[T("bb"), T("kt")], writes=[T("ex")])
            P.op("dve", lambda e, h=h: e.tensor_tensor(out=h["kd"][:], in0=h["kk"][:], in1=h["ex"][:], op=ALU.mult),
                 reads=[T("kk"), T("ex")], writes=[T("kd")])
            bi = rbank()
            mm_acc(P, banks[bi], [(wgb[:, kc, hs], xb[b][:, kc, :]) for kc in range(8)], ["wg", ("xb", b)], [("bank", bi)])
            P.op("act", lambda e, h=h, bi=bi: e.activation(out=h["sgl"][:], in_=banks[bi], func=AF.Silu), reads=[("bank", bi)], writes=[T("sgl")])
            for half in range(NCH // 4):
                bi = rbank()
                for cc in range(4):
                    c = half * 4 + cc
                    mm_acc(P, banks[bi][0:64, cc * 128:(cc + 1) * 128],
                           [(xb[b][:, kc, c * 64:(c + 1) * 64], wib[:, kc, hs]) for kc in range(8)], ["wi", ("xb", b)], [("bank", bi)])
                P.op("act", lambda e, h=h, bi=bi, half=half: e.activation(
                    out=h["vtm"][:, half * 4:(half + 1) * 4, :], in_=banks[bi][0:64, :].rearrange("p (c v) -> p c v", v=128), func=AF.Copy),
                    reads=[("bank", bi)], writes=[T("vtm")])
            for half in range(NCH // 4):
                bi = rbank()
                for cc in range(4):
                    c = half * 4 + cc
                    P.op("pe", lambda e, h=h, bi=bi, cc=cc, c=c: e.matmul(banks[bi][0:64, cc * 128:(cc + 1) * 128], h["kd"][:, c * 64:(c + 1) * 64],
                                                                          idb[:], start=True, stop=True),
                         reads=[T("kd"), "idb"], writes=[("bank", bi)])
                P.op("dve", lambda e, h=h, bi=bi, half=half: e.tensor_copy(
                    out=h["kdt"][:, half * 4:(half + 1) * 4, :], in_=banks[bi][0:64, :].rearrange("p (c v) -> p c v", v=128)),
                    reads=[("bank", bi)], writes=[T("kdt")])
            for c in range(NCH):
                cs = slice(c * 64, (c + 1) * 64)
                P.op("pe", lambda e, h=h, cs=cs: e.matmul(banks[B_ATT][0:64, cs], h["kt"][:, cs], h["qt"][:, cs], start=True, stop=True),
                     reads=[T("kt"), T("qt")], writes=[("bank", B_ATT)])
            P.op("dve", lambda e, h=h: e.tensor_tensor(out=h["attm"][:], in0=banks[B_ATT][0:64, 0:NT], in1=tr[:], op=ALU.mult),
                 reads=[("bank", B_ATT), "tr"], writes=[T("attm")])
        def chunk(hd, c):
            h = H[hd]
            hs = slice(hd * 128, (hd + 1) * 128)
            T = lambda n: (n, hd)
            B_O, B_S = 4 + hd, 6 + hd
            cs = slice(c * 64, (c + 1) * 64)
            g = ti * NCH + c
            sb_cur = h["Sb"][g % 2]
            first = (g == 0)
            P.op("pe", lambda e, h=h, cs=cs, c=c, first=first: e.matmul(banks[B_O][:, cs], h["vtm"][:, c, :], h["attm"][:, cs],
                                                                        start=True, stop=first),
                 reads=[T("vtm"), T("attm")], writes=[("bank", B_O)])
            if not first:
                P.op("pe", lambda e, h=h, cs=cs, sb_cur=sb_cur: e.matmul(banks[B_O][:, cs], sb_cur[:], h["qt"][:, cs], start=False, stop=True),
                     reads=[("Sb", hd, g % 2), T("qt")], writes=[("bank", B_O)])
            P.op("pe", lambda e, h=h, c=c: e.matmul(banks[B_S][:, 0:128], h["kdt"][:, c, :], h["vtm"][:, c, :], start=True, stop=True),
                 reads=[T("kdt"), T("vtm")], writes=[("bank", B_S)])
            P.op("dve", lambda e, h=h, c=c: e.scalar_tensor_tensor(out=h["Sf"][:], in0=h["Sf"][:], scalar=h["eb"][:, c * 64 + 63:c * 64 + 64],
                                                                     in1=banks[B_S][:, 0:128], op0=ALU.mult, op1=ALU.add),
                 reads=[("Sf", hd), ("bank", B_S), T("eb")], writes=[("Sf", hd)])
            nxt = h["Sb"][(g + 1) % 2]
            P.op("act", lambda e, h=h, nxt=nxt: e.activation(out=nxt[:], in_=h["Sf"][:], func=AF.Copy),
                 reads=[("Sf", hd)], writes=[("Sb", hd, (g + 1) % 2)])
        def post(hd):
            h = H[hd]
            hs = slice(hd * 128, (hd + 1) * 128)
            T = lambda n: (n, hd)
            B_O, B_S = 4 + hd, 6 + hd
            P.op("act", lambda e, h=h: e.activation(out=h["osq"][:], in_=banks[B_O][:, 0:NT], func=AF.Square), reads=[("bank", B_O)], writes=[T("osq")])
            bi = rbank()
            P.op("pe", lambda e, h=h, bi=bi: e.matmul(banks[bi][:, 0:NT], ones_b[:], h["osq"][:], start=True, stop=True),
                 reads=["ones", T("osq")], writes=[("bank", bi)])
            P.op("dve", lambda e, h=h, bi=bi: e.tensor_scalar(out=h["rs"][:], in0=banks[bi][:, 0:NT], scalar1=1e-6, scalar2=None, op0=ALU.add),
                 reads=[("bank", bi)], writes=[T("rs")])
            P.op("act", lambda e, h=h: e.activation(out=h["rs"][:], in_=h["rs"][:], func=AF.Sqrt), reads=[T("rs")], writes=[T("rs")])
            P.op("dve", lambda e, h=h: e.reciprocal(out=h["rs"][:], in_=h["rs"][:]), reads=[T("rs")], writes=[T("rs")])
            P.op("dve", lambda e, h=h: e.tensor_tensor(out=h["rs"][:], in0=h["rs"][:], in1=banks[B_O][:, 0:NT], op=ALU.mult),
                 reads=[T("rs"), ("bank", B_O)], writes=[T("rs")])
            P.op("dve", lambda e, h=h, hd=hd: e.scalar_tensor_tensor(out=h["ob"][:], in0=h["rs"][:], scalar=hps[:, 2 + hd:3 + hd], in1=h["sgl"][:],
                                                                      op0=ALU.mult, op1=ALU.mult),
                 reads=[T("rs"), T("sgl"), "hp"], writes=[T("ob")])
            P.dma("sp", odst(hd * 128, ti * NT, NT), h["ob"][:], reads=[T("ob")])
        for hd in range(2):
            prep(hd)
        for c in range(NCH):
            for hd in range(2):
                chunk(hd, c)
        for hd in range(2):
            post(hd)
    return cx.done()


import math

NEG = -30000.0
GELU_C2 = 2.0 * 0.7978845608028654
W_C, W_S, W_W = 4224, 2176, 640
OFFD, ND = 2064, 6272
OFFW, NDW = 128, 768


def _bucket(d):
    n = np.maximum(d, 0)
    large = 16 + (np.log(np.maximum(n, 16).astype(np.float32) / np.float32(16)) / np.float32(math.log(2048 / 16))
                  * np.float32(16)).astype(np.int32)
    return np.where(n < 16, n, np.minimum(large, 31))


def nsa_static():
    d = np.arange(ND) - OFFD
    ohd = np.zeros((33, ND), np.float32)
    bk = _bucket(d)
    ohd[bk[d >= 0], np.nonzero(d >= 0)[0]] = 1.0
    ohd[32, d < 0] = 1.0
    dw = np.arange(NDW) - OFFW
    ohw = np.zeros((33, NDW), np.float32)
    okw = (dw >= 0) & (dw < 512)
    ohw[_bucket(dw)[okw], np.nonzero(okw)[0]] = 1.0
    ohw[32, ~okw] = 1.0
    n = np.arange(512)[:, None]
    s = np.arange(128)[None, :]
    ov = np.clip(np.minimum(n * 16 + 32, s * 64 + 64) - np.maximum(n * 16, s * 64), 0, None) / 32.0
    ov[511] = 0.0
    c2s = np.ascontiguousarray(ov.reshape(4, 128, 128).transpose(1, 0, 2)).astype(np.float32)
    q = np.arange(128)[:, None]
    c = np.arange(256)[None, :]
    hi = (q >= 64).astype(np.int64)
    vt = ((c - 126) <= hi - 2).astype(np.float32)
    ft = (((c - 126) == hi) | ((c - 126) == hi - 1)).astype(np.float32)
    ident = np.eye(128, dtype=np.float32)
    return dict(ohd=ohd, ohw=ohw, c2s=c2s, vt=vt, vm1=vt - 1.0, ft=ft, ident=ident, ident4=np.tile(ident, (1, 4)), aident=np.ascontiguousarray(ident[::-1]),
                lstrict=np.triu(np.ones((128, 128), np.float32), 1))


def build_nsa(S, upto=99, qbs=None, cx=None):
    cx = cx or Ctx()
    nc, P = cx.nc, cx.P
    NQB = S // 128
    NT = 512
    I = cx.inp
    SI = lambda name, shape, dt=F32: cx.inp(name, shape, dt, shared=True)
    xsrc = cx.ov.get("xsrc")
    if xsrc is None:
        xT = I("xT", [1024, S])
        xTv_ = xT.rearrange("(c p) t -> p c t", p=128)
        xsrc = lambda t0, n: (xTv_[:, :, t0:t0 + n], "pool")
    wq = I("nwq", [1024, 256]); wk = I("nwk", [1024, 256]); wt = I("nwt", [1024, 140])
    w1 = I("w1", [2, 64, 32, 128]); posT = I("posT", [2, 64, 32]); w2 = I("w2", [2, 128, 64])
    tabaug = SI("tabaug", [33, 4])
    ohd = SI("ohd", [33, ND]); ohw = SI("ohw", [33, NDW])
    c2s_d = SI("c2s", [128, 4, 128]); vt_d = SI("vt", [128, 256]); vm1_d = SI("vm1", [128, 256]); ft_d = SI("ft", [128, 256])
    ident_d = SI("ident", [128, 128]); ident4_d = SI("ident4", [128, 512]); aident_d = SI("aident", [128, 128])
    odst = cx.ov.get("odst")
    if odst is None:
        obT = cx.out("obT", [256, S], BF16)
        odst = lambda row0, t0, n: obT[row0:row0 + 128, t0:t0 + n]
    otok = cx.ov.get("otok", lambda t0: [])
    after_store = cx.ov.get("after_store")
    fD = nc.dram_tensor(cx.prefix + "fD", [4, ND], F32, kind="Internal")
    fW = nc.dram_tensor(cx.prefix + "fW", [4, NDW], F32, kind="Internal")
    banks = cx.banks
    B_OC, B_U, B_OS, B_OW, B_M = 3, 4, 5, 6, 7
    rot = [0]

    def rbank():
        i = rot[0] % 3
        rot[0] += 1
        return i
    wqb = P.sb([128, 8, 256], BF16)
    KsT = P.sb([65, S], BF16); KwT = P.sb([64, S], BF16)
    Vs = P.sb([128, NQB, 65], BF16); Vw = P.sb([128, NQB, 65], BF16)
    gsig = P.sb([128, NQB, 12], F32)
    kcT = P.sb([65, 512], BF16); vc = P.sb([128, 4, 65], BF16)
    c2s = P.sb([128, 4, 128], BF16)
    vt = P.sb([128, 256], F32); vm1 = P.sb([128, 256], F32); ft = P.sb([128, 256], F32)
    idb = P.sb([128, 128], BF16); id4 = P.sb([128, 512], BF16); jdb = P.sb([128, 128], BF16)
    tq = P.sb([65, 4], F32)
    AR = max(4 * (W_C + W_S + W_W), 2 * 8 * NT + 8 * 256 + 8 * 140 + 2 * 32 * 128 + 2 * S)
    arena = P.sb([128, AR], BF16)
    o = 0

    def carve(n):
        nonlocal o
        a = arena[:, o:o + n]
        o += n
        return a
    xb = [carve(8 * NT).rearrange("p (c t) -> p c t", t=NT) for _ in range(2)]
    wkb = carve(8 * 256).rearrange("p (c n) -> p c n", n=256)
    wtb = carve(8 * 140).rearrange("p (c n) -> p c n", n=140)
    w1b = carve(2 * 32 * 128).rearrange("p (k l j) -> p k l j", k=2, l=32)
    KcT = carve(S); VcT = carve(S)
    assert o <= AR, (o, AR)
    TcT = arena[:, 0:4 * W_C].rearrange("p (h w) -> p h w", h=4)
    TsT = arena[:, 4 * W_C:4 * (W_C + W_S)].rearrange("p (h w) -> p h w", h=4)
    TwT = arena[:, 4 * (W_C + W_S):4 * (W_C + W_S + W_W)].rearrange("p (h w) -> p h w", h=4)
    ARENA_TOK = [("xb", 0), ("xb", 1), "wk", "wt", "w1", "KcT", "VcT"]
    posb = P.sb([64, 2, 32], BF16); w2b = P.sb([128, 2, 64], BF16)
    cst = P.sb([128, 2], F32)
    g1 = P.sb([128, 512], F32); g2 = P.sb([128, 512], F32); h1g = P.sb([128, 512], BF16)
    tabs = P.sb([33, 4], F32); ohs = [P.sb([33, 512], F32) for _ in range(2)]
    fsb = P.sb([4, ND], F32); fwsb = P.sb([4, NDW], F32)
    xq = [P.sb([128, 8, 128], BF16) for _ in range(2)]
    Qb = [P.sb([65, 4, 128], BF16) for _ in range(2)]
    pT = [P.sb([128, 512], BF16) for _ in range(3)]
    rz = P.sb([128, 12], F32); coef = P.sb([128, 12], F32)
    imp = P.sb([128, 128], F32); imp2 = P.sb([128, 128], F32); m8a = P.sb([128, 8], F32); m8b = P.sb([128, 8], F32)
    selneg = P.sb([128, 128], BF16)
    selx = P.sb([128, S], BF16)
    ofin = P.sb([128, 256], F32); ofb = P.sb([128, 256], BF16); oT = [P.sb([128, 2, 128], BF16) for _ in range(2)]

    P.dma("pool", wqb[:], wq.rearrange("(c p) n -> p c n", p=128), writes=["wq"])
    P.dma("pool", wkb, wk.rearrange("(c p) n -> p c n", p=128), writes=["wk"])
    P.dma("pool", wtb, wt.rearrange("(c p) n -> p c n", p=128), writes=["wt"])
    P.dma("pool", w1b[0:64], w1.rearrange("k e l j -> e k l j"), writes=["w1"])
    P.dma("pool", posb[:], posT.rearrange("k e l -> e k l"), writes=["pos"])
    P.dma("pool", w2b[:], w2.rearrange("k j d -> j k d"), writes=["w2"])
    P.dma("pool", c2s[:], c2s_d, writes=["c2s"])
    P.dma("pool", idb[:], ident_d, writes=["idb"])
    P.dma("pool", id4[:], ident4_d, writes=["id4"])
    P.dma("pool", jdb[:], aident_d, writes=["jdb"])
    P.dma("sp", vt[:], vt_d, writes=["vt"]); P.dma("sp", vm1[:], vm1_d, writes=["vm1"]); P.dma("sp", ft[:], ft_d, writes=["ft"])
    P.dma("sp", tabs[:], tabaug, writes=["tabs"])
    P.dma("sp", tq[64:65, :], tabaug[31:32, :], writes=["tq"])
    P.op("dve", lambda e: e.memset(KsT[64:65, :], 1.0), writes=["KsT_aug"])
    P.op("dve", lambda e: e.memset(kcT[:], 0.0), writes=["kcT"])
    P.op("dve", lambda e: e.memset(kcT[64:65, :], 1.0), writes=["kcT"])
    P.op("dve", lambda e: e.memset(vc[:], 0.0), writes=["vc"])
    P.op("dve", lambda e: e.memset(vc[:, :, 64:65], 1.0), writes=["vc"])
    P.op("pool", lambda e: e.memset(Vs[:, :, 64:65], 1.0), writes=["Vs_aug"])
    P.op("pool", lambda e: e.memset(Vw[:, :, 64:65], 1.0), writes=["Vw_aug"])
    for b in range(2):
        for h in range(4):
            P.op("pool", lambda e, b=b, h=h: e.memset(Qb[b][64:65, h, :], 0.0), writes=[("Qaug", b)])
            P.op("pool", lambda e, b=b, h=h: e.tensor_scalar(out=Qb[b][64:65, h, :], in0=Qb[b][64:65, h, :], scalar1=tq[64:65, h:h + 1],
                                                             scalar2=None, op0=ALU.add), reads=["tq"], writes=[("Qaug", b)])
    for (oh_d, n_d, dst_s, dst_d, tk) in ((ohd, ND, fsb, fD, "fD"), (ohw, NDW, fwsb, fW, "fW")):
        for ci, c0 in enumerate(range(0, n_d, 512)):
            c1 = min(n_d, c0 + 512)
            ob_ = ohs[ci % 2]
            P.dma("sp", ob_[:, 0:c1 - c0], oh_d[:, c0:c1], writes=[("ohs", ci % 2)])
            P.op("pe", lambda e, ob_=ob_, n=c1 - c0: e.matmul(banks[B_M][0:4, 0:n], tabs[:], ob_[:, 0:n], start=True, stop=True),
                 reads=["tabs", ("ohs", ci % 2)], writes=[("bank", B_M)])
            P.op("act", lambda e, dst_s=dst_s, c0=c0, c1=c1: e.activation(out=dst_s[:, c0:c1], in_=banks[B_M][0:4, 0:c1 - c0], func=AF.Copy),
                 reads=[("bank", B_M)], writes=[tk + "s"])
        P.dma("sp", dst_d.ap(), dst_s[:], reads=[tk + "s"], writes=[tk])

    if upto < 1:
        return cx.done()
    def load_x(ti):
        r_ = xsrc(ti * NT, NT)
        P.dma(r_[1], xb[ti % 2], r_[0], reads=(r_[2] if len(r_) > 2 else []), writes=[("xb", ti % 2)])
    load_x(0)
    for ti in range(S // NT):
        b = ti % 2
        ts = slice(ti * NT, (ti + 1) * NT)
        if ti + 1 < S // NT:
            load_x(ti + 1)
        for j, (dst, tk) in enumerate(((KsT, "KsT"), (KwT, "KwT"), (KcT, "KcT"), (VcT, "VcT"))):
            bi = rbank()
            mm_acc(P, banks[bi][0:64, :], [(wkb[:, kc, j * 64:(j + 1) * 64], xb[b][:, kc, :]) for kc in range(8)], ["wk", ("xb", b)], [("bank", bi)])
            eng = "act" if j % 2 == 0 else "dve"
            if eng == "act":
                P.op("act", lambda e, dst=dst, bi=bi, ts=ts: e.activation(out=dst[0:64, ts], in_=banks[bi][0:64, :], func=AF.Copy),
                     reads=[("bank", bi)], writes=[tk])
            else:
                P.op("dve", lambda e, dst=dst, bi=bi, ts=ts: e.tensor_copy(out=dst[0:64, ts], in_=banks[bi][0:64, :]),
                     reads=[("bank", bi)], writes=[tk])
        for st in range(4):
            bi = rbank()
            qi = ti * 4 + st
            mm_acc(P, banks[bi][:, 0:140], [(xb[b][:, kc, st * 128:(st + 1) * 128], wtb[:, kc, :]) for kc in range(8)], ["wt", ("xb", b)], [("bank", bi)])
            P.op("dve", lambda e, bi=bi, qi=qi: e.tensor_copy(out=Vs[:, qi, 0:64], in_=banks[bi][:, 0:64]), reads=[("bank", bi)], writes=["Vs"])
            P.op("dve", lambda e, bi=bi, qi=qi: e.tensor_copy(out=Vw[:, qi, 0:64], in_=banks[bi][:, 64:128]), reads=[("bank", bi)], writes=["Vw"])
            P.op("act", lambda e, bi=bi, qi=qi: e.activation(out=gsig[:, qi, :], in_=banks[bi][:, 128:140], func=AF.Sigmoid),
                 reads=[("bank", bi)], writes=["gsig"])
    if upto < 2:
        return cx.done()
    NCB = (S - 32) // 16 + 1
    for kv, (src, tk) in enumerate(((KcT, "KcT"), (VcT, "VcT"))):
        bi = rbank()
        mm_acc(P, banks[bi][:, 0:1], [(w1b[0:64, kv, l, :], posb[:, kv, l:l + 1]) for l in range(32)], ["w1", "pos"], [("bank", bi)])
        P.op("dve", lambda e, bi=bi, kv=kv: e.tensor_copy(out=cst[:, kv:kv + 1], in_=banks[bi][:, 0:1]), reads=[("bank", bi)], writes=["cst"])
        bi = rbank()
        srcv = src[0:64, :]
        mm_acc(P, banks[bi][:, 0:NCB],
               [(w1b[0:64, kv, l, :], src[0:64, l:l + 16 * (NCB - 1) + 1:16]) for l in range(32)], ["w1", tk], [("bank", bi)])
        P.op("dve", lambda e, bi=bi, kv=kv: e.tensor_scalar(out=g1[:, 0:NCB], in0=banks[bi][:, 0:NCB], scalar1=cst[:, kv:kv + 1], scalar2=None, op0=ALU.add),
             reads=[("bank", bi), "cst"], writes=["g1"])
        P.op("dve", lambda e: e.tensor_tensor(out=g2[:, 0:NCB], in0=g1[:, 0:NCB], in1=g1[:, 0:NCB], op=ALU.mult), reads=["g1"], writes=["g2"])
        P.op("dve", lambda e: e.tensor_scalar(out=g2[:, 0:NCB], in0=g2[:, 0:NCB], scalar1=0.044715, scalar2=1.0, op0=ALU.mult, op1=ALU.add),
             reads=["g2"], writes=["g2"])
        P.op("dve", lambda e: e.tensor_tensor(out=g2[:, 0:NCB], in0=g2[:, 0:NCB], in1=g1[:, 0:NCB], op=ALU.mult), reads=["g1", "g2"], writes=["g2"])
        P.op("act", lambda e: e.activation(out=g2[:, 0:NCB], in_=g2[:, 0:NCB], func=AF.Sigmoid, scale=GELU_C2), reads=["g2"], writes=["g2"])
        P.op("dve", lambda e: e.memset(h1g[:], 0.0), reads=["h1g"], writes=["h1g"])
        P.op("dve", lambda e: e.tensor_tensor(out=h1g[:, 0:NCB], in0=g2[:, 0:NCB], in1=g1[:, 0:NCB], op=ALU.mult), reads=["g1", "g2"], writes=["h1g"])
        if kv == 0:
            bi = rbank()
            P.op("pe", lambda e, bi=bi: e.matmul(banks[bi][0:64, 0:NCB], w2b[:, 0, :], h1g[:, 0:NCB], start=True, stop=True),
                 reads=["w2", "h1g"], writes=[("bank", bi)])
            P.op("act", lambda e, bi=bi: e.activation(out=kcT[0:64, 0:NCB], in_=banks[bi][0:64, 0:NCB], func=AF.Copy), reads=[("bank", bi)], writes=["kcT"])
        else:
            bi = rbank()
            for i in range((NCB + 127) // 128):
                P.op("pe", lambda e, bi=bi, i=i: e.matmul(banks[bi][:, i * 64:(i + 1) * 64], h1g[:, i * 128:(i + 1) * 128], w2b[:, 1, :], start=True, stop=True),
                     reads=["w2", "h1g"], writes=[("bank", bi)])
            ni = (NCB + 127) // 128
            P.op("act", lambda e, bi=bi, ni=ni: e.activation(out=vc[:, 0:ni, 0:64], in_=banks[bi][:, 0:ni * 64].rearrange("p (i d) -> p i d", d=64), func=AF.Copy),
                 reads=[("bank", bi)], writes=["vc"])
    if upto < 3:
        return cx.done()
    for h in range(4):
        P.dma("pool", TcT[:, h, :], bass.AP(fD, h * ND + OFFD - 2063, [[16, 128], [1, W_C]]), reads=["fD"], writes=ARENA_TOK + ["TcT"])
        P.dma("pool", TsT[:, h, :], bass.AP(fD, h * ND + OFFD - 127, [[1, 128], [1, W_S]]), reads=["fD"], writes=ARENA_TOK + ["TsT"])
        P.dma("pool", TwT[:, h, :], bass.AP(fW, h * NDW + OFFW - 127, [[1, 128], [1, W_W]]), reads=["fW"], writes=ARENA_TOK + ["TwT"])

    if upto < 4:
        return cx.done()
    def load_xq(qb):
        r_ = xsrc(qb * 128, 128)
        P.dma(r_[1], xq[qb % 2][:], r_[0], reads=(r_[2] if len(r_) > 2 else []), writes=[("xq", qb % 2)])

    def score_tile(qb, Qa, lhs_full, near, table, wbase, mask_j, acc_bank, acc_tok, v_rhs, first, extra=None):
        bi = rbank()
        kk = 64 if near else 65
        nmm = 1 + (1 if near else 0) + (1 if mask_j is not None else 0)
        k = 0
        P.op("pe", lambda e: e.matmul(banks[bi], lhs_full[0:kk, :], Qa[0:kk].rearrange("p h q -> p (h q)"), start=True, stop=(nmm == 1)),
             reads=[("Q", qb % 2), ("Qaug", qb % 2), "kcT", "KsT", "KwT", "KsT_aug"], writes=[("bank", bi)])
        k += 1
        if near:
            P.op("pe", lambda e, k=k: e.matmul(banks[bi].rearrange("p (h q) -> p h q", h=4), jdb[:], table[:, :, wbase:wbase + 128], start=False, stop=(k == nmm - 1)),
                 reads=["jdb", "TcT", "TsT", "TwT"], writes=[("bank", bi)])
            k += 1
        if mask_j is not None:
            lw = selx[:, mask_j * 128:(mask_j + 1) * 128]
            P.op("pe", lambda e: e.matmul(banks[bi], lw, id4[:], start=False, stop=True),
                 reads=[("selx", mask_j // 8), "id4"], writes=[("bank", bi)])
        pt = pT[bi]
        P.op("act", lambda e: e.activation(out=pt[:], in_=banks[bi], func=AF.Exp), reads=[("bank", bi)], writes=[("pT", bi)])

        def pv():
            for h in range(4):
                P.op("pe", lambda e, h=h: e.matmul(banks[acc_bank][:, h * 65:(h + 1) * 65], pt[:, h * 128:(h + 1) * 128], v_rhs,
                                                   start=(first and h == 0), stop=False, skip_group_check=True),
                     reads=[("pT", bi), "vc", "Vs", "Vw", "Vs_aug", "Vw_aug"], writes=[("bank", acc_bank)])
                if extra is not None:
                    extra(pt, bi, h, first and h == 0)
        pend.append(pv)
        while len(pend) > 2:
            pend.pop(0)()

    pend = []
    deferred = []
    oacc = [P.sb([128, 3, 260], F32) for _ in range(2)]

    def flush():
        while pend:
            pend.pop(0)()

    qdone = {}

    def qproj(qb_):
        b_ = qb_ % 2
        for h in range(4):
            mm_acc(P, banks[B_M][0:64, h * 128:(h + 1) * 128], [(wqb[:, kc, h * 64:(h + 1) * 64], xq[b_][:, kc, :]) for kc in range(8)],
                   ["wq", ("xq", b_)], [("bank", B_M)])
        P.op("act", lambda e: e.activation(out=Qb[b_][0:64], in_=banks[B_M][0:64, :].rearrange("p (h q) -> p h q", h=4), func=AF.Copy, scale=0.125),
             reads=[("bank", B_M)], writes=[("Q", b_)])
        qdone[qb_] = True

    load_xq(0)
    for qb in range(NQB):
        b = qb % 2
        t0 = qb * 128
        if qb + 1 < NQB:
            load_xq(qb + 1)
        if qbs is not None and qb not in qbs:
            continue
        Qa = Qb[b]
        if not qdone.get(qb):
            qproj(qb)
        ni = min(3, (t0 + 96) // 2048)
        for i in range(ni + 1):
            wbase = t0 - 2048 * i
            near = wbase < W_C

            def extra(pt, bi, h, st, i=i):
                P.op("pe", lambda e: e.matmul(banks[B_U][:, h * 128:(h + 1) * 128], pt[:, h * 128:(h + 1) * 128], c2s[:, i, :],
                                              start=st, stop=False, skip_group_check=True),
                     reads=[("pT", bi), "c2s"], writes=[("bank", B_U)])
            score_tile(qb, Qa, kcT[:, i * 128:(i + 1) * 128], near, TcT, wbase if near else 0, None, B_OC, "oc", vc[:, i, :], i == 0,
                       extra if qb >= 8 else None)
        while deferred:
            deferred.pop(0)()
        flush()
        if qb >= 8:
            ocv = banks[B_OC][:, 0:260].rearrange("p (h c) -> p h c", c=65)
            P.op("dve", lambda e: e.tensor_scalar(out=rz[:, 0:4], in0=ocv[:, :, 64], scalar1=1e-30, scalar2=None, op0=ALU.max),
                 reads=[("bank", B_OC)], writes=["rzc"])
            P.op("dve", lambda e: e.reciprocal(out=rz[:, 0:4], in_=rz[:, 0:4]), reads=["rzc"], writes=["rzc"])
            for h in range(4):
                if h == 0:
                    P.op("dve", lambda e: e.tensor_scalar(out=imp[:], in0=banks[B_U][:, 0:128], scalar1=rz[:, 0:1], scalar2=None, op0=ALU.mult),
                         reads=[("bank", B_U), "rzc"], writes=["imp"])
                else:
                    P.op("dve", lambda e, h=h: e.scalar_tensor_tensor(out=imp[:], in0=banks[B_U][:, h * 128:(h + 1) * 128], scalar=rz[:, h:h + 1],
                                                                       in1=imp[:], op0=ALU.mult, op1=ALU.add),
                         reads=[("bank", B_U), "rzc", "imp"], writes=["imp"])
            c0 = 126 - 2 * qb
            P.op("dve", lambda e, c0=c0: e.tensor_tensor(out=imp[:], in0=imp[:], in1=vt[:, c0:c0 + 128], op=ALU.mult), reads=["imp", "vt"], writes=["imp"])
            P.op("dve", lambda e, c0=c0: e.tensor_tensor(out=imp[:], in0=imp[:], in1=vm1[:, c0:c0 + 128], op=ALU.add), reads=["imp", "vm1"], writes=["imp"])
            P.op("dve", lambda e: e.memset(imp[:, 0:1], -1.0), reads=["imp"], writes=["imp"])
            P.op("dve", lambda e: e.max(out=m8a[:], in_=imp[:]), reads=["imp"], writes=["m8a"])
            P.op("dve", lambda e: e.match_replace(out=imp2[:], in_to_replace=m8a[:], in_values=imp[:], imm_value=-2.0), reads=["imp", "m8a"], writes=["imp2"])
            P.op("dve", lambda e: e.max(out=m8b[:], in_=imp2[:]), reads=["imp2"], writes=["m8b"])
            P.op("dve", lambda e: e.tensor_scalar(out=imp2[:], in0=imp[:], scalar1=m8b[:, 4:5], scalar2=None, op0=ALU.is_ge), reads=["imp", "m8b"], writes=["imp2"])
            P.op("dve", lambda e, c0=c0: e.tensor_tensor(out=imp2[:], in0=imp2[:], in1=ft[:, c0:c0 + 128], op=ALU.max), reads=["imp2", "ft"], writes=["imp2"])
            P.op("dve", lambda e: e.memset(imp2[:, 0:1], 1.0), reads=["imp2"], writes=["imp2"])
            P.op("dve", lambda e: e.tensor_scalar(out=selneg[:], in0=imp2[:], scalar1=-NEG, scalar2=NEG, op0=ALU.mult, op1=ALU.add),
                 reads=["imp2"], writes=["selneg"])
            nkb = 2 * (qb + 1)
            for k0 in range(0, nkb, 16):
                k1 = min(nkb, k0 + 16)
                P.op("dve", lambda e, k0=k0, k1=k1: e.tensor_copy(out=selx[:, k0 * 64:k1 * 64].rearrange("p (k j) -> p k j", j=64),
                                                                  in_=selneg[:, k0:k1].unsqueeze(2).to_broadcast([128, k1 - k0, 64])),
                     reads=["selneg"], writes=[("selx", k0 // 16)])
        j0 = max(0, qb - 4)
        for j in range(j0, qb + 1):
            score_tile(qb, Qa, KwT[:, j * 128:(j + 1) * 128], True, TwT, 128 * (qb - j), None, B_OW, "ow", Vw[:, j, :], j == j0)
        if qb + 1 < NQB and (qbs is None or (qb + 1) in qbs):
            qproj(qb + 1)
        for j in range(qb + 1):
            near = (qb - j) <= 16
            score_tile(qb, Qa, KsT[:, j * 128:(j + 1) * 128], near, TsT, 128 * (qb - j) if near else 0, j if qb >= 8 else None,
                       B_OS, "os", Vs[:, j, :], j == 0)
        flush()
        oa_ = oacc[b]
        for x_, bk in enumerate((B_OC, B_OS, B_OW)):
            P.op("dve", lambda e, x_=x_, bk=bk, oa_=oa_: e.tensor_copy(out=oa_[:, x_, :], in_=banks[bk][:, 0:260]),
                 reads=[("bank", bk)], writes=[("oacc", b)])
        oav = oa_[:].rearrange("p x (h c) -> p x h c", c=65)
        P.op("dve", lambda e, oav=oav: e.tensor_scalar(out=rz[:].rearrange("p (x h) -> p x h", x=3), in0=oav[:, :, :, 64], scalar1=1e-30, scalar2=None, op0=ALU.max),
             reads=[("oacc", b), "rzc"], writes=["rz"])
        P.op("dve", lambda e: e.reciprocal(out=rz[:], in_=rz[:]), reads=["rz"], writes=["rz"])
        P.op("dve", lambda e, qb=qb: e.tensor_tensor(out=coef[:].rearrange("p (x h) -> p x h", x=3), in0=rz[:].rearrange("p (x h) -> p x h", x=3),
                                                      in1=gsig[:, qb, :].rearrange("p (h x) -> p x h", x=3), op=ALU.mult),
             reads=["rz", "gsig"], writes=["coef"])
        for h in range(4):
            for x_ in range(3):
                src = oa_[:, x_, h * 65:h * 65 + 64]
                dst = ofin[:, h * 64:(h + 1) * 64]
                if x_ == 0:
                    P.op("dve", lambda e, src=src, dst=dst, h=h, x_=x_: e.tensor_scalar(out=dst, in0=src, scalar1=coef[:, x_ * 4 + h:x_ * 4 + h + 1], scalar2=None, op0=ALU.mult),
                         reads=[("oacc", b), "coef"], writes=["ofin"])
                else:
                    P.op("dve", lambda e, src=src, dst=dst, h=h, x_=x_: e.scalar_tensor_tensor(out=dst, in0=src, scalar=coef[:, x_ * 4 + h:x_ * 4 + h + 1], in1=dst,
                                                                                             op0=ALU.mult, op1=ALU.add),
                         reads=[("oacc", b), "coef", "ofin"], writes=["ofin"])
        P.op("act", lambda e: e.activation(out=ofb[:], in_=ofin[:], func=AF.Copy), reads=["ofin"], writes=["ofb"])

        def finish(b=b, t0=t0):
            for f in range(2):
                P.op("pe", lambda e, f=f: e.matmul(banks[B_M][:, f * 128:(f + 1) * 128], ofb[:, f * 128:(f + 1) * 128], idb[:], start=True, stop=True),
                     reads=["ofb", "idb"], writes=[("bank", B_M)])
            P.op("act", lambda e, b=b: e.activation(out=oT[b][:], in_=banks[B_M][:, 0:256].rearrange("p (f q) -> p f q", f=2), func=AF.Copy),
                 reads=[("bank", B_M)], writes=[("oT", b)])
            for f in range(2):
                P.dma("sp", odst(f * 128, t0, 128), oT[b][:, f, :], reads=[("oT", b)], writes=otok(t0))
            if after_store is not None:
                after_store(t0 + 128)
        deferred.append(finish)
    while deferred:
        deferred.pop(0)()
    return cx.done()


D_FFE = 3584
N_EXP = 8


def build_moe(T, GT=2048, cx=None):
    cx = cx or Ctx()
    nc, P = cx.nc, cx.P
    I = cx.inp
    xT = I("xT", [1024, T])
    wr = I("wr", [1024, N_EXP])
    wg = I("mwg", [N_EXP, 1024, D_FFE]); wu = I("mwu", [N_EXP, 1024, D_FFE]); wd = I("mwd", [N_EXP, D_FFE, 1024])
    lngb = I("lngb", [2, 1024])
    ident_d = cx.inp("ident", [128, 128], shared=True)
    out = cx.out("out", [T, 1024])
    GT = min(GT, T)
    NSUB = GT // 128
    NTT = GT // 512
    NSL = D_FFE // 512
    banks = cx.banks
    idf = P.sb([128, 128], F32)
    P.dma("sp", idf[:], ident_d, writes=["idf"])
    acc = P.sb([128, NSUB, 1024], F32)
    xb = P.sb([128, 8, GT], BF16)
    xf = [P.sb([128, 8, 128], F32) for _ in range(2)]
    wrs = P.sb([128, 8, N_EXP], F32)
    wgs = [P.sb([128, 8, 512], BF16) for _ in range(2)]
    wus = [P.sb([128, 8, 512], BF16) for _ in range(2)]
    wds = [P.sb([128, 4, 1024], BF16) for _ in range(2)]
    hs = [P.sb([128, 4, 512], BF16) for _ in range(2)]
    sg = [P.sb([128, 512], F32) for _ in range(2)]
    lg = P.sb([128, N_EXP], F32); m8 = P.sb([128, 8], F32); gt = P.sb([128, 4], F32); eq = P.sb([128, N_EXP], F32)
    wts = P.sb([128, NSUB, N_EXP], F32)
    gbc = P.sb([128, 2, 1024], F32)
    st = P.sb([128, 8], F32)
    junk = P.sb([128, 1024], F32)
    P.dma("sp", wrs[:], wr.rearrange("(c p) e -> p c e", p=128), writes=["wr"])
    P.dma("sp", gbc[:, 0, :], lngb[0:1, :].partition_broadcast(128), writes=["gbc"])
    P.dma("sp", gbc[:, 1, :], lngb[1:2, :].partition_broadcast(128), writes=["gbc"])
    xTv = xT.rearrange("(c p) t -> p c t", p=128)
    pr = [0]

    def rb(lo, n):
        i = lo + pr[0] % n
        pr[0] += 1
        return i
    nload = [0]
    for g0 in range(0, T, GT):
        P.dma("pool", xb[:], xTv[:, :, g0:g0 + GT], writes=["xb"])
        for sub in range(NSUB):
            fb = sub % 2
            P.dma("sp", xf[fb][:], xTv[:, :, g0 + sub * 128:g0 + (sub + 1) * 128], writes=[("xf", fb)])
            mm_acc(P, banks[7][:, 0:N_EXP], [(xf[fb][:, kc, :], wrs[:, kc, :]) for kc in range(8)], [("xf", fb), "wr"], [("bank", 7)])
            P.op("dve", lambda e: e.tensor_copy(out=lg[:], in_=banks[7][:, 0:N_EXP]), reads=[("bank", 7)], writes=["lg"])
            P.op("dve", lambda e: e.max(out=m8[:], in_=lg[:]), reads=["lg"], writes=["m8"])
            P.op("dve", lambda e: e.tensor_tensor(out=gt[:, 0:1], in0=m8[:, 1:2], in1=m8[:, 0:1], op=ALU.subtract), reads=["m8"], writes=["gt"])
            P.op("act", lambda e: e.activation(out=gt[:, 0:1], in_=gt[:, 0:1], func=AF.Exp), reads=["gt"], writes=["gt"])
            P.op("dve", lambda e: e.tensor_scalar(out=gt[:, 0:1], in0=gt[:, 0:1], scalar1=1.0, scalar2=None, op0=ALU.add), reads=["gt"], writes=["gt"])
            P.op("dve", lambda e: e.reciprocal(out=gt[:, 1:2], in_=gt[:, 0:1]), reads=["gt"], writes=["gt"])
            P.op("dve", lambda e: e.tensor_scalar(out=gt[:, 2:3], in0=gt[:, 1:2], scalar1=-1.0, scalar2=1.0, op0=ALU.mult, op1=ALU.add),
                 reads=["gt"], writes=["gt"])
            P.op("dve", lambda e: e.tensor_scalar(out=eq[:], in0=lg[:], scalar1=m8[:, 0:1], scalar2=gt[:, 1:2], op0=ALU.is_equal, op1=ALU.mult),
                 reads=["lg", "m8", "gt"], writes=["eq"])
            P.op("dve", lambda e, sub=sub: e.tensor_scalar(out=wts[:, sub, :], in0=lg[:], scalar1=m8[:, 1:2], scalar2=gt[:, 2:3], op0=ALU.is_equal, op1=ALU.mult),
                 reads=["lg", "m8", "gt"], writes=["wts"])
            P.op("dve", lambda e, sub=sub: e.tensor_tensor(out=wts[:, sub, :], in0=wts[:, sub, :], in1=eq[:], op=ALU.add), reads=["wts", "eq"], writes=["wts"])
            for half in range(2):
                bt = 4 + half
                for c4 in range(4):
                    c = half * 4 + c4
                    P.op("pe", lambda e, fb=fb, c=c, c4=c4, bt=bt: e.matmul(banks[bt][:, c4 * 128:(c4 + 1) * 128], xf[fb][:, c, :], idf[:], start=True, stop=True),
                         reads=[("xf", fb), "idf"], writes=[("bank", bt)])
                P.op("act", lambda e, sub=sub, half=half, bt=bt: e.activation(out=acc[:, sub, half * 512:(half + 1) * 512], in_=banks[bt], func=AF.Copy,
                                                                              scale=float(DN_ALPHA)),
                     reads=[("bank", bt)], writes=[("acc", sub)])
        for ex in range(N_EXP):
            for s in range(NSL):
                wbuf = nload[0] % 2
                nload[0] += 1
                fs = slice(s * 512, (s + 1) * 512)
                P.dma("pool", wgs[wbuf][:], wg[ex, :, fs].rearrange("(c p) n -> p c n", p=128), writes=[("wgs", wbuf)])
                P.dma("pool", wus[wbuf][:], wu[ex, :, fs].rearrange("(c p) n -> p c n", p=128), writes=[("wus", wbuf)])
                P.dma("pool", wds[wbuf][:], wd[ex, fs, :].rearrange("(c p) n -> p c n", p=128), writes=[("wds", wbuf)])
                for tt in range(NTT):
                    hb = tt % 2
                    tsl = slice(tt * 512, (tt + 1) * 512)
                    for m in range(4):
                        ms = slice(m * 128, (m + 1) * 128)
                        bg = rb(0, 2); bu = 2 + (bg % 2)
                        mm_acc(P, banks[bg], [(wgs[wbuf][:, kc, ms], xb[:, kc, tsl]) for kc in range(8)], [("wgs", wbuf), "xb"], [("bank", bg)])
                        mm_acc(P, banks[bu], [(wus[wbuf][:, kc, ms], xb[:, kc, tsl]) for kc in range(8)], [("wus", wbuf), "xb"], [("bank", bu)])
                        P.op("act", lambda e, bg=bg: e.activation(out=sg[bg][:], in_=banks[bg], func=AF.Silu), reads=[("bank", bg)], writes=[("sg", bg)])
                        P.op("dve", lambda e, bg=bg, bu=bu, hb=hb, m=m: e.tensor_tensor(out=hs[hb][:, m, :], in0=sg[bg][:], in1=banks[bu], op=ALU.mult),
                             reads=[("sg", bg), ("bank", bu)], writes=[("hs", hb)])
                    for sub4 in range(4):
                        sub = tt * 4 + sub4
                        for half in range(2):
                            bd = 4 + (sub4 * 2 + half) % 3
                            mm_acc(P, banks[bd], [(hs[hb][:, m, sub4 * 128:(sub4 + 1) * 128], wds[wbuf][:, m, half * 512:(half + 1) * 512]) for m in range(4)],
                                   [("hs", hb), ("wds", wbuf)], [("bank", bd)])
                            P.op("dve", lambda e, bd=bd, sub=sub, half=half, ex=ex: e.scalar_tensor_tensor(
                                out=acc[:, sub, half * 512:(half + 1) * 512], in0=banks[bd], scalar=wts[:, sub, ex:ex + 1],
                                in1=acc[:, sub, half * 512:(half + 1) * 512], op0=ALU.mult, op1=ALU.add),
                                reads=[("bank", bd), "wts", ("acc", sub)], writes=[("acc", sub)])
        for sub in range(NSUB):
            a = acc[:, sub, :]
            P.op("act", lambda e, a=a: e.activation(out=junk[:], in_=a, func=AF.Copy, accum_out=st[:, 0:1]), reads=[("acc", sub)], writes=["junk", "st"])
            P.op("act", lambda e, a=a: e.activation(out=junk[:], in_=a, func=AF.Square, accum_out=st[:, 1:2]), reads=[("acc", sub)], writes=["junk", "st"])
            P.op("dve", lambda e: e.tensor_scalar(out=st[:, 0:2], in0=st[:, 0:2], scalar1=1.0 / 1024, scalar2=None, op0=ALU.mult), reads=["st"], writes=["st"])
            P.op("dve", lambda e: e.tensor_tensor(out=st[:, 2:3], in0=st[:, 0:1], in1=st[:, 0:1], op=ALU.mult), reads=["st"], writes=["st"])
            P.op("dve", lambda e: e.tensor_tensor(out=st[:, 2:3], in0=st[:, 1:2], in1=st[:, 2:3], op=ALU.subtract), reads=["st"], writes=["st"])
            P.op("dve", lambda e: e.tensor_scalar(out=st[:, 2:3], in0=st[:, 2:3], scalar1=LN_EPS, scalar2=None, op0=ALU.add), reads=["st"], writes=["st"])
            P.op("act", lambda e: e.activation(out=st[:, 2:3], in_=st[:, 2:3], func=AF.Sqrt), reads=["st"], writes=["st"])
            P.op("dve", lambda e: e.reciprocal(out=st[:, 2:3], in_=st[:, 2:3]), reads=["st"], writes=["st"])
            P.op("dve", lambda e, a=a: e.tensor_scalar(out=a, in0=a, scalar1=st[:, 0:1], scalar2=st[:, 2:3], op0=ALU.subtract, op1=ALU.mult),
                 reads=["st", ("acc", sub)], writes=[("acc", sub)])
            P.op("dve", lambda e, a=a: e.tensor_tensor(out=a, in0=a, in1=gbc[:, 0, :], op=ALU.mult), reads=["gbc", ("acc", sub)], writes=[("acc", sub)])
            P.op("dve", lambda e, a=a: e.tensor_tensor(out=a, in0=a, in1=gbc[:, 1, :], op=ALU.add), reads=["gbc", ("acc", sub)], writes=[("acc", sub)])
        P.dma("sp", out[g0:g0 + GT, :].rearrange("(s p) d -> p s d", p=128), acc[:], reads=[("acc", s) for s in range(NSUB)])
    return cx.done()


MOE_CAP = 1408
MOE_SL = 256
MOE_BIG = 1.0e6
MOE_DEBUG = False


def build_moe2(T, cx=None, CAP=None):
    cx = cx or Ctx()
    nc, P = cx.nc, cx.P
    I = cx.inp
    CAP = CAP or min(MOE_CAP, ((T * 2) // N_EXP * 5 // 4 + 127) // 128 * 128 + 128)
    xT = I("xT", [1024, T])
    wr = I("wr", [1024, N_EXP])
    wg = I("mwg", [N_EXP, 1024, D_FFE]); wu = I("mwu", [N_EXP, 1024, D_FFE]); wd = I("mwd", [N_EXP, D_FFE, 1024])
    lngb = I("lngb", [2, 1024])
    ident_d = cx.inp("ident", [128, 128], shared=True)
    lstr_d = cx.inp("lstrict", [128, 128], shared=True)
    out = cx.out("out", [T, 1024])
    NSA_ = T // 128
    SL = MOE_SL
    NSL = D_FFE // SL
    MT = D_FFE // 128
    NROW = N_EXP * CAP
    xe = nc.dram_tensor(cx.prefix + "xe", [NROW, 1024], BF16)
    ye = nc.dram_tensor(cx.prefix + "ye", [NROW, 1024], F32)
    banks = cx.banks
    xTv = xT.rearrange("(c p) t -> p c t", p=128)
    base = (nc.sbuf_base, nc.sbuf_top)
    idf = P.sb([128, 128], F32); idb = P.sb([128, 128], BF16); lstr = P.sb([128, 128], BF16); onesb = P.sb([128, 128], BF16)
    wrs = P.sb([128, 8, N_EXP], F32)
    E1 = P.sb([128, NSA_, N_EXP], F32); E2 = P.sb([128, NSA_, N_EXP], F32); G = P.sb([128, NSA_, 2], F32)
    S1i = P.sb([128, NSA_], I32); S2i = P.sb([128, NSA_], I32)
    P.dma("sp", idf[:], ident_d, writes=["idf"])
    P.dma("pool", idb[:], ident_d, writes=["idb"])
    P.dma("pool", lstr[:], lstr_d, writes=["lstr"])
    P.op("dve", lambda e: e.memset(onesb[:], 1.0), writes=["onesb"])
    P.dma("sp", wrs[:], wr.rearrange("(c p) e -> p c e", p=128), writes=["wr"])
    base2 = (nc.sbuf_base, nc.sbuf_top)
    breg = {}

    def mk_breg(e):
        breg["r"] = e.alloc_register("moe_bound")
        return e.reg_mov(breg["r"], NROW - 1)
    P.op("pool", mk_breg)
    xf = [P.sb([128, 8, 128], F32) for _ in range(2)]
    xtmb = P.sb([128, NSA_, 1024], BF16)
    lg = P.sb([128, N_EXP], F32); m8 = P.sb([128, 8], F32); gt = P.sb([128, 4], F32)
    zt = P.sb([128, CAP // 128, 1024], BF16)
    P.op("pool", lambda e: e.memset(zt[:], 0.0), writes=["zt"])
    for ex in range(N_EXP):
        P.dma("sp", xe.ap()[ex * CAP:(ex + 1) * CAP, :].rearrange("(n p) d -> p n d", p=128), zt[:], reads=["zt"], writes=["xe_zero"])
    for sub in range(NSA_):
        fb = sub % 2
        P.dma("sp", xf[fb][:], xTv[:, :, sub * 128:(sub + 1) * 128], writes=[("xf", fb)])
        mm_acc(P, banks[7][:, 0:N_EXP], [(xf[fb][:, kc, :], wrs[:, kc, :]) for kc in range(8)], [("xf", fb), "wr"], [("bank", 7)])
        P.op("dve", lambda e: e.tensor_copy(out=lg[:], in_=banks[7][:, 0:N_EXP]), reads=[("bank", 7)], writes=["lg"])
        P.op("dve", lambda e: e.max(out=m8[:], in_=lg[:]), reads=["lg"], writes=["m8"])
        P.op("dve", lambda e: e.tensor_tensor(out=gt[:, 0:1], in0=m8[:, 1:2], in1=m8[:, 0:1], op=ALU.subtract), reads=["m8"], writes=["gt"])
        P.op("act", lambda e: e.activation(out=gt[:, 0:1], in_=gt[:, 0:1], func=AF.Exp), reads=["gt"], writes=["gt"])
        P.op("dve", lambda e: e.tensor_scalar(out=gt[:, 0:1], in0=gt[:, 0:1], scalar1=1.0, scalar2=None, op0=ALU.add), reads=["gt"], writes=["gt"])
        P.op("dve", lambda e, sub=sub: e.reciprocal(out=G[:, sub, 0:1], in_=gt[:, 0:1]), reads=["gt"], writes=["G"])
        P.op("dve", lambda e, sub=sub: e.tensor_scalar(out=G[:, sub, 1:2], in0=G[:, sub, 0:1], scalar1=-1.0, scalar2=1.0, op0=ALU.mult, op1=ALU.add),
             reads=["G"], writes=["G"])
        P.op("dve", lambda e, sub=sub: e.tensor_scalar(out=E1[:, sub, :], in0=lg[:], scalar1=m8[:, 0:1], scalar2=None, op0=ALU.is_equal),
             reads=["lg", "m8"], writes=["E1"])
        P.op("dve", lambda e, sub=sub: e.tensor_scalar(out=E2[:, sub, :], in0=lg[:], scalar1=m8[:, 1:2], scalar2=None, op0=ALU.is_equal),
             reads=["lg", "m8"], writes=["E2"])
        for half in range(2):
            bt = 4 + half
            for c4 in range(4):
                c = half * 4 + c4
                P.op("pe", lambda e, fb=fb, c=c, c4=c4, bt=bt: e.matmul(banks[bt][:, c4 * 128:(c4 + 1) * 128], xf[fb][:, c, :], idf[:], start=True, stop=True),
                     reads=[("xf", fb), "idf"], writes=[("bank", bt)])
            P.op("act", lambda e, sub=sub, half=half, bt=bt: e.activation(out=xtmb[:, sub, half * 512:(half + 1) * 512], in_=banks[bt], func=AF.Copy),
                 reads=[("bank", bt)], writes=[("xtmb", sub)])
    Mb = P.sb([128, NSA_ * N_EXP], BF16)
    Mf = P.sb([128, NSA_, N_EXP], F32); tots = P.sb([128, NSA_, N_EXP], F32); incl = P.sb([128, NSA_, N_EXP], F32)
    pos = P.sb([128, NSA_, N_EXP], F32); tmp = P.sb([128, NSA_, N_EXP], F32); onesf = P.sb([128, NSA_], F32)
    s12 = P.sb([128, 2, NSA_], F32)
    NE = NSA_ * N_EXP
    P.op("dve", lambda e: e.tensor_tensor(out=Mf[:], in0=E1[:], in1=E2[:], op=ALU.add), reads=["E1", "E2"], writes=["Mf"])
    P.op("dve", lambda e: e.tensor_copy(out=Mb[:], in_=Mf[:].rearrange("p s e -> p (s e)")), reads=["Mf"], writes=["Mb"])
    P.op("dve", lambda e: e.memset(onesf[:], 1.0), writes=["onesf"])
    P.op("pe", lambda e: e.matmul(banks[0][:, 0:NE], lstr[:], Mb[:], start=True, stop=True), reads=["lstr", "Mb"], writes=[("bank", 0)])
    P.op("pe", lambda e: e.matmul(banks[1][:, 0:NE], onesb[:], Mb[:], start=True, stop=True), reads=["onesb", "Mb"], writes=[("bank", 1)])
    P.op("dve", lambda e: e.tensor_copy(out=tots[:].rearrange("p s e -> p (s e)"), in_=banks[1][:, 0:NE]), reads=[("bank", 1)], writes=["tots"])
    if MOE_DEBUG:
        dbg = nc.dram_tensor("dbg_tots", [128, NSA_ * N_EXP], F32, kind="ExternalOutput").ap()
        P.dma("sp", dbg, tots[:].rearrange("p s e -> p (s e)"), reads=["tots"])
    for ex in range(N_EXP):
        P.op("dve", lambda e, ex=ex: e.tensor_tensor_scan(out=incl[:, :, ex], data0=onesf[:], data1=tots[:, :, ex], initial=0.0, op0=ALU.mult, op1=ALU.add),
             reads=["tots", "onesf"], writes=["incl"])
    P.op("dve", lambda e: e.tensor_tensor(out=pos[:].rearrange("p s e -> p (s e)"), in0=banks[0][:, 0:NE], in1=incl[:].rearrange("p s e -> p (s e)"), op=ALU.add),
         reads=[("bank", 0), "incl"], writes=["pos"])
    P.op("dve", lambda e: e.tensor_tensor(out=pos[:], in0=pos[:], in1=tots[:], op=ALU.subtract), reads=["pos", "tots"], writes=["pos"])
    P.op("dve", lambda e: e.tensor_scalar(out=tmp[:], in0=pos[:], scalar1=float(CAP), scalar2=MOE_BIG, op0=ALU.is_ge, op1=ALU.mult), reads=["pos"], writes=["tmp"])
    P.op("dve", lambda e: e.tensor_tensor(out=pos[:], in0=pos[:], in1=tmp[:], op=ALU.add), reads=["pos", "tmp"], writes=["pos"])
    for ex in range(1, N_EXP):
        P.op("dve", lambda e, ex=ex: e.tensor_scalar(out=pos[:, :, ex], in0=pos[:, :, ex], scalar1=float(ex * CAP), scalar2=None, op0=ALU.add),
             reads=["pos"], writes=["pos"])
    for k_, EE in enumerate((E1, E2)):
        P.op("dve", lambda e, EE=EE: e.tensor_tensor(out=tmp[:], in0=pos[:], in1=EE[:], op=ALU.mult), reads=["pos", "E1", "E2", "s12"], writes=["tmp"])
        P.op("dve", lambda e, k_=k_: e.reduce_sum(out=s12[:, k_, :], in_=tmp[:], axis=AX.X), reads=["tmp"], writes=["s12"])
    P.op("dve", lambda e: e.tensor_copy(out=S1i[:], in_=s12[:, 0, :]), reads=["s12"], writes=["S1i"])
    P.op("dve", lambda e: e.tensor_copy(out=S2i[:], in_=s12[:, 1, :]), reads=["s12"], writes=["S2i"])
    for sub in range(NSA_):
        for Si, tk in ((S1i, "S1i"), (S2i, "S2i")):
            P.op("pool", lambda e, sub=sub, Si=Si: e.indirect_dma_start(
                out=xe.ap(), out_offset=bass.IndirectOffsetOnAxis(ap=Si[:, sub:sub + 1], axis=0), in_=xtmb[:, sub, :], in_offset=None,
                bounds_check=breg["r"], oob_is_err=False), reads=[tk, ("xtmb", sub), "xe_zero"], dma=True)
    P.barrier()
    nc.sbuf_base, nc.sbuf_top = base2
    NST = CAP // 128
    tiles = []
    t_ = 0
    while t_ < CAP:
        n_ = min(512, CAP - t_)
        tiles.append((t_, n_))
        t_ += n_
    xrow = [P.sb([128, 1024], BF16) for _ in range(2)]
    xeT = P.sb([128, 8, CAP], BF16)
    hT = P.sb([128, MT, CAP], BF16)
    wgs = [P.sb([128, 8, SL], BF16) for _ in range(2)]
    wus = [P.sb([128, 8, SL], BF16) for _ in range(2)]
    wds = P.sb([128, MT, 1024], BF16)
    sg = [P.sb([128, 512], F32) for _ in range(2)]
    yb = [P.sb([128, 1024], F32) for _ in range(2)]
    nl = 0
    nrow = 0
    for ex in range(N_EXP):
        P.dma("pool", wds[:], wd[ex].rearrange("(c p) n -> p c n", p=128), writes=["wds"])
        for stl in range(NST):
            rb_ = nrow % 2
            nrow += 1
            r0 = ex * CAP + stl * 128
            P.dma("sp", xrow[rb_][:], xe.ap()[r0:r0 + 128, :], writes=[("xrow", rb_)])
            for half in range(2):
                bt = 4 + half
                for c4 in range(4):
                    c = half * 4 + c4
                    P.op("pe", lambda e, rb_=rb_, c=c, c4=c4, bt=bt: e.matmul(banks[bt][:, c4 * 128:(c4 + 1) * 128], xrow[rb_][:, c * 128:(c + 1) * 128], idb[:],
                                                                             start=True, stop=True),
                         reads=[("xrow", rb_), "idb"], writes=[("bank", bt)])
                P.op("act" if half == 0 else "dve",
                     (lambda e, half=half, stl=stl, bt=bt: e.activation(out=xeT[:, half * 4:(half + 1) * 4, stl * 128:(stl + 1) * 128],
                                                                         in_=banks[bt].rearrange("p (c t) -> p c t", c=4), func=AF.Copy)) if half == 0 else
                     (lambda e, half=half, stl=stl, bt=bt: e.tensor_copy(out=xeT[:, half * 4:(half + 1) * 4, stl * 128:(stl + 1) * 128],
                                                                          in_=banks[bt].rearrange("p (c t) -> p c t", c=4))),
                     reads=[("bank", bt)], writes=["xeT"])
        for s_ in range(NSL):
            wb_ = nl % 2
            nl += 1
            fs = slice(s_ * SL, (s_ + 1) * SL)
            P.dma("pool", wgs[wb_][:], wg[ex, :, fs].rearrange("(c p) n -> p c n", p=128), writes=[("wgs", wb_)])
            P.dma("pool", wus[wb_][:], wu[ex, :, fs].rearrange("(c p) n -> p c n", p=128), writes=[("wus", wb_)])
            for (t0_, n_) in tiles:
                for m in range(SL // 128):
                    ms = slice(m * 128, (m + 1) * 128)
                    pb = m % 2
                    bg, bu = pb, 2 + pb
                    mm_acc(P, banks[bg][:, 0:n_], [(wgs[wb_][:, kc, ms], xeT[:, kc, t0_:t0_ + n_]) for kc in range(8)], [("wgs", wb_), "xeT"], [("bank", bg)])
                    mm_acc(P, banks[bu][:, 0:n_], [(wus[wb_][:, kc, ms], xeT[:, kc, t0_:t0_ + n_]) for kc in range(8)], [("wus", wb_), "xeT"], [("bank", bu)])
                    P.op("act", lambda e, bg=bg, n_=n_: e.activation(out=sg[bg][:, 0:n_], in_=banks[bg][:, 0:n_], func=AF.Silu), reads=[("bank", bg)], writes=[("sg", bg)])
                    P.op("dve", lambda e, bg=bg, bu=bu, n_=n_, t0_=t0_, mm_=s_ * (SL // 128) + m: e.tensor_tensor(out=hT[:, mm_, t0_:t0_ + n_], in0=sg[bg][:, 0:n_], in1=banks[bu][:, 0:n_], op=ALU.mult),
                         reads=[("sg", bg), ("bank", bu)], writes=[("hT", s_ * (SL // 128) + m)])
        for stl in range(NST):
            yb_ = yb[stl % 2]
            for half in range(2):
                bd = 4 + (stl * 2 + half) % 3
                mm_acc(P, banks[bd], [(hT[:, m, stl * 128:(stl + 1) * 128], wds[:, m, half * 512:(half + 1) * 512]) for m in range(MT)],
                       [[("hT", m), "wds"] for m in range(MT)], [("bank", bd)])
                if half == 0:
                    P.op("act", lambda e, bd=bd, yb_=yb_: e.activation(out=yb_[:, 0:512], in_=banks[bd], func=AF.Copy), reads=[("bank", bd)], writes=[("yb", stl % 2)])
                else:
                    P.op("dve", lambda e, bd=bd, yb_=yb_: e.tensor_copy(out=yb_[:, 512:1024], in_=banks[bd]), reads=[("bank", bd)], writes=[("yb", stl % 2)])
            r0 = ex * CAP + stl * 128
            P.dma("sp", ye.ap()[r0:r0 + 128, :], yb_[:], reads=[("yb", stl % 2)])
    P.barrier()
    nc.sbuf_base, nc.sbuf_top = base2
    xf = [P.sb([128, 8, 128], F32) for _ in range(2)]
    y1 = [P.sb([128, 1024], F32) for _ in range(2)]
    y2 = [P.sb([128, 1024], F32) for _ in range(2)]
    accs = [P.sb([128, 1024], F32) for _ in range(2)]
    gbc = P.sb([128, 2, 1024], F32)
    P.dma("sp", gbc[:, 0, :], lngb[0:1, :].partition_broadcast(128), writes=["gbc"])
    P.dma("sp", gbc[:, 1, :], lngb[1:2, :].partition_broadcast(128), writes=["gbc"])
    sts = [P.sb([128, 8], F32) for _ in range(2)]
    junks = [P.sb([128, 1024], F32) for _ in range(2)]
    for sub in range(NSA_):
        fb = sub % 2
        a = accs[fb]
        st = sts[fb]
        junk = junks[fb]
        P.dma("sp", xf[fb][:], xTv[:, :, sub * 128:(sub + 1) * 128], writes=[("xf", fb)])
        for yy, Si, tk in ((y1, S1i, "y1"), (y2, S2i, "y2")):
            P.op("pool", lambda e, yy=yy, fb=fb: e.memset(yy[fb][:], 0.0), writes=[(tk, fb)])
            P.op("pool", lambda e, yy=yy, fb=fb, Si=Si, sub=sub: e.indirect_dma_start(
                out=yy[fb][:], out_offset=None, in_=ye.ap(), in_offset=bass.IndirectOffsetOnAxis(ap=Si[:, sub:sub + 1], axis=0),
                bounds_check=breg["r"], oob_is_err=False), reads=["S1i", "S2i"], writes=[(tk, fb)], dma=True)
        for half in range(2):
            bt = 4 + half
            for c4 in range(4):
                c = half * 4 + c4
                P.op("pe", lambda e, fb=fb, c=c, c4=c4, bt=bt: e.matmul(banks[bt][:, c4 * 128:(c4 + 1) * 128], xf[fb][:, c, :], idf[:], start=True, stop=True),
                     reads=[("xf", fb), "idf"], writes=[("bank", bt)])
            P.op("act", lambda e, a=a, half=half, bt=bt: e.activation(out=a[:, half * 512:(half + 1) * 512], in_=banks[bt], func=AF.Copy, scale=float(DN_ALPHA)),
                 reads=[("bank", bt)], writes=[("acc", fb)])
        P.op("dve", lambda e, a=a, fb=fb, sub=sub: e.scalar_tensor_tensor(out=a[:], in0=y1[fb][:], scalar=G[:, sub, 0:1], in1=a[:], op0=ALU.mult, op1=ALU.add),
             reads=[("y1", fb), "G", ("acc", fb)], writes=[("acc", fb)])
        P.op("dve", lambda e, a=a, fb=fb, sub=sub: e.scalar_tensor_tensor(out=a[:], in0=y2[fb][:], scalar=G[:, sub, 1:2], in1=a[:], op0=ALU.mult, op1=ALU.add),
             reads=[("y2", fb), "G", ("acc", fb)], writes=[("acc", fb)])
        T_ = ("acc", fb)
        P.op("act", lambda e, a=a, st=st, junk=junk: e.activation(out=junk[:], in_=a[:], func=AF.Copy, accum_out=st[:, 0:1]), reads=[T_], writes=[("junk", fb), ("st", fb)])
        P.op("act", lambda e, a=a, st=st, junk=junk: e.activation(out=junk[:], in_=a[:], func=AF.Square, accum_out=st[:, 1:2]), reads=[T_], writes=[("junk", fb), ("st", fb)])
        P.op("dve", lambda e, st=st: e.tensor_scalar(out=st[:, 0:2], in0=st[:, 0:2], scalar1=1.0 / 1024, scalar2=None, op0=ALU.mult), reads=[("st", fb)], writes=[("st", fb)])
        P.op("dve", lambda e, st=st: e.tensor_tensor(out=st[:, 2:3], in0=st[:, 0:1], in1=st[:, 0:1], op=ALU.mult), reads=[("st", fb)], writes=[("st", fb)])
        P.op("dve", lambda e, st=st: e.tensor_tensor(out=st[:, 2:3], in0=st[:, 1:2], in1=st[:, 2:3], op=ALU.subtract), reads=[("st", fb)], writes=[("st", fb)])
        P.op("dve", lambda e, st=st: e.tensor_scalar(out=st[:, 2:3], in0=st[:, 2:3], scalar1=LN_EPS, scalar2=None, op0=ALU.add), reads=[("st", fb)], writes=[("st", fb)])
        P.op("act", lambda e, st=st: e.activation(out=st[:, 2:3], in_=st[:, 2:3], func=AF.Sqrt), reads=[("st", fb)], writes=[("st", fb)])
        P.op("dve", lambda e, st=st: e.reciprocal(out=st[:, 2:3], in_=st[:, 2:3]), reads=[("st", fb)], writes=[("st", fb)])
        P.op("dve", lambda e, a=a, st=st, junk=junk: e.tensor_scalar(out=a[:], in0=a[:], scalar1=st[:, 0:1], scalar2=st[:, 2:3], op0=ALU.subtract, op1=ALU.mult), reads=[("st", fb), T_], writes=[T_])
        P.op("dve", lambda e, a=a, st=st, junk=junk: e.tensor_tensor(out=a[:], in0=a[:], in1=gbc[:, 0, :], op=ALU.mult), reads=["gbc", T_], writes=[T_])
        P.op("dve", lambda e, a=a, st=st, junk=junk: e.tensor_tensor(out=a[:], in0=a[:], in1=gbc[:, 1, :], op=ALU.add), reads=["gbc", T_], writes=[T_])
        P.dma("sp", out[sub * 128:(sub + 1) * 128, :], a[:], reads=[T_])
    if not cx.standalone:
        nc.sbuf_base, nc.sbuf_top = base
    return cx.done()


from concourse.bass_utils import run_bass_kernel_spmd

S_FULL = 8192
_PROGS = {}


def _prog(key, fn):
    if key not in _PROGS:
        _PROGS[key] = fn()
    return _PROGS[key]


def _c(a):
    return np.ascontiguousarray(a, dtype=np.float32)


def _pc(v):
    return _c(np.asarray(v).reshape(8, 128).T)


def _run(nc, in_maps):
    res = run_bass_kernel_spmd(nc, in_maps, core_ids=list(range(8)))
    return res.results


def kernel_unfused(x, w_in, hg_lb_logits, hg_norm_w, cmp_pos, cmp_w1, cmp_w2, rel_bias, w_branch_a, w_branch_b,
           w_out, ln1_g, ln1_b, ln2_g, ln2_b, ffn_w_gate, ffn_w_up, ffn_w_down, moe_router, moe_w_gate,
           moe_w_up, moe_w_down):
    f = lambda a: np.asarray(a, dtype=np.float32)
    x = f(x); w_in = f(w_in); hg_lb_logits = f(hg_lb_logits); hg_norm_w = f(hg_norm_w)
    cmp_pos = f(cmp_pos); cmp_w1 = f(cmp_w1); cmp_w2 = f(cmp_w2); rel_bias = f(rel_bias)
    B, S, D = x.shape
    NT = 512
    cmask = np.ones((128, NT), np.float32); cmask[:, ::64] = 0
    tri = np.zeros((64, NT), np.float32)
    for c in range(NT // 64):
        tri[:, c * 64:(c + 1) * 64] = np.triu(np.ones((64, 64), np.float32))
    ident = np.eye(128, dtype=np.float32)
    st = nsa_static()
    xT_b = [_c(x[b].T) for b in range(B)]
    out = None
    for l in range(DEPTH):
        W = w_in[l]
        nc_h = _prog(("hgrn", l), lambda: build_hgrn(S, NT, layer=l))
        maps = []
        for c in range(8):
            b, j = c // 2, c % 2
            cs = slice(j * 256, (j + 1) * 256)
            lbl = np.stack([hg_lb_logits[0, cs][:128], hg_lb_logits[0, cs][128:], hg_lb_logits[1, cs][:128], hg_lb_logits[1, cs][128:]], axis=1)
            nw = hg_norm_w[l, cs]
            hp = np.stack([np.ones(128, np.float32), np.ones(128, np.float32), nw[:128], nw[128:]], axis=1)
            maps.append(dict(xT=xT_b[b], hwq=_c(W[:, 0:512][:, cs]), hwf=_c(W[:, 512:1024][:, cs]), hwi=_c(W[:, 1024:1536][:, cs]),
                             hwg=_c(W[:, 1536:2048][:, cs]), hp=_c(hp), lbl=_c(lbl), cmask=cmask, tri=tri, ident=ident))
        r_h = _run(nc_h, maps)
        nc_n = _prog(("nsa",), lambda: build_nsa(S))
        maps = []
        for c in range(8):
            b, g = c // 2, c % 2
            kv = lambda base: W[:, base + g * 64: base + (g + 1) * 64]
            d = dict(xT=xT_b[b], nwq=_c(W[:, 2048 + g * 256:2048 + (g + 1) * 256]),
                     nwk=_c(np.concatenate([kv(2816), kv(3072), kv(2560), kv(2688)], 1)),
                     nwt=_c(np.concatenate([kv(2944), kv(3200), W[:, 3328 + g * 12:3328 + (g + 1) * 12]], 1)),
                     w1=_c(cmp_w1[l].reshape(2, 32, 64, 128).transpose(0, 2, 1, 3)), posT=_c(cmp_pos[l].transpose(0, 2, 1)), w2=_c(cmp_w2[l]),
                     tabaug=_c(np.concatenate([rel_bias[:, g * 4:(g + 1) * 4], np.full((1, 4), NEG, np.float32)], 0)))
            d.update(st)
            maps.append(d)
        r_n = _run(nc_n, maps)
        T = S // 2
        nc_m = _prog(("merge",), lambda: build_merge(T))
        lnp1 = _c(np.concatenate([_pc(ln1_g[l]), _pc(ln1_b[l])], axis=1))
        maps = []
        for c in range(8):
            b, hf = c // 2, c % 2
            ts = slice(hf * T, (hf + 1) * T)
            oaT = np.ascontiguousarray(np.concatenate([r_h[2 * b]["oaT"][:, ts], r_h[2 * b + 1]["oaT"][:, ts]], axis=0))
            obT = np.ascontiguousarray(np.concatenate([r_n[2 * b]["obT"][:, ts], r_n[2 * b + 1]["obT"][:, ts]], axis=0))
            maps.append(dict(xT=_c(xT_b[b][:, ts]), oaT=oaT, obT=obT, wgt=_c(W[:, 3352:5400]), wa=_c(f(w_branch_a)[l]), wb=_c(f(w_branch_b)[l]),
                             wo=_c(f(w_out)[l]), lnp=lnp1))
        r_m = _run(nc_m, maps)
        lnp2 = _c(np.concatenate([_pc(ln2_g[l]), _pc(ln2_b[l])], axis=1))
        if l % 2 == 0:
            nc_f = _prog(("ffn",), lambda: build_ffn(T))
            maps = [dict(xT=r_m[c]["outT"], wg=_c(f(ffn_w_gate)[l // 2]), wu=_c(f(ffn_w_up)[l // 2]), wd=_c(f(ffn_w_down)[l // 2]), lnp=lnp2)
                    for c in range(8)]
            r_f = _run(nc_f, maps)
            xT_b = [np.ascontiguousarray(np.concatenate([r_f[2 * b]["outT"], r_f[2 * b + 1]["outT"]], axis=1)) for b in range(B)]
            if l == DEPTH - 1:
                out = np.stack([xT_b[b].T for b in range(B)])
        else:
            nc_f = _prog(("moe",), lambda: build_moe(T))
            wgm, wum, wdm = _c(f(moe_w_gate)[l // 2]), _c(f(moe_w_up)[l // 2]), _c(f(moe_w_down)[l // 2])
            maps = [dict(xT=r_m[c]["outT"], wr=_c(f(moe_router)[l // 2]), mwg=wgm, mwu=wum, mwd=wdm, ident=ident,
                         lngb=_c(np.stack([f(ln2_g)[l], f(ln2_b)[l]]))) for c in range(8)]
            r_f = _run(nc_f, maps)
            xtm = [np.concatenate([r_f[2 * b]["out"], r_f[2 * b + 1]["out"]], axis=0) for b in range(B)]
            xT_b = [_c(a.T) for a in xtm]
            if l == DEPTH - 1:
                out = np.stack(xtm)
    return np.ascontiguousarray(out, dtype=np.float32)


def build_fused(S=S_FULL if False else 8192, ncores=8):
    T = S // 2
    nc = new_nc()
    P = Prog(nc)
    banks = [P.ps([128, 512]) for _ in range(8)]
    groups = [[2 * i, 2 * i + 1] for i in range(ncores // 2)]
    EI = lambda name, shape, dt=F32: nc.dram_tensor(name, list(shape), dt, kind="ExternalInput").ap()
    xT = EI("xT", [1024, S]); xTh = EI("xTh", [1024, T]); selh = EI("selh", [128, 2])
    out = nc.dram_tensor("out", [T, 1024], F32, kind="ExternalOutput").ap()
    OC = min(2048, S)
    XC = min(1024, T)
    NOC, NXC = S // OC, T // XC
    obuf = [[nc.dram_tensor(f"obuf{l}_{k}", [512, OC], BF16) for k in range(NOC)] for l in range(DEPTH)]
    og = [[nc.dram_tensor(f"og{l}_{k}", [1024, OC], BF16) for k in range(NOC)] for l in range(DEPTH)]
    x2b = [nc.dram_tensor(f"x2b_{k}", [1024, XC], BF16) for k in range(NXC)]
    xg = [nc.dram_tensor(f"xg_{k}", [2048, XC], BF16) for k in range(NXC)]
    x1f = [nc.dram_tensor(f"x1f_{l}", [1024, T], F32) for l in range(DEPTH)]
    x2f = nc.dram_tensor("x2f", [1024, T], F32)
    base = (nc.sbuf_base, nc.sbuf_top)

    P.persist = set()

    def phase_end(wait_cc=True):
        P.barrier(wait_cc=wait_cc)
        nc.sbuf_base, nc.sbuf_top = base

    xTv = xT.rearrange("(c p) t -> p c t", p=128)

    def xsrc0(t0, n):
        return xTv[:, :, t0:t0 + n], "pool"

    def xsrc1(t0, n):
        r, k, tt = t0 // T, (t0 % T) // XC, t0 % XC
        return xg[k].ap()[r * 1024:(r + 1) * 1024, tt:tt + n].rearrange("(c p) t -> p c t", p=128), "sp", [("xg", k)]

    for l in range(DEPTH):
        xsrc = xsrc0 if l == 0 else xsrc1

        def odst_h(row0, t0, n, l=l):
            return obuf[l][t0 // OC].ap()[row0:row0 + 128, t0 % OC:t0 % OC + n]

        def odst_n(row0, t0, n, l=l):
            return obuf[l][t0 // OC].ap()[256 + row0:256 + row0 + 128, t0 % OC:t0 % OC + n]
        build_hgrn(S, 512, layer=l, cx=Ctx(nc, P, banks, f"l{l}h_", dict(xsrc=xsrc, odst=odst_h)))
        phase_end()
        def o_after(tend, l=l):
            if tend % OC == 0:
                k = tend // OC - 1
                P.cc(lambda e, l=l, k=k: e.collective_compute("AllGather", ALU.bypass, replica_groups=groups,
                                                              ins=[obuf[l][k].ap().opt()], outs=[og[l][k].ap().opt()]),
                     reads=[("obuf", k)], writes=[("og", l, k)])
                P.persist.add(("og", l, k))
        build_nsa(S, cx=Ctx(nc, P, banks, f"l{l}n_", dict(xsrc=xsrc, odst=odst_n, otok=lambda t0: [("obuf", t0 // OC)], after_store=o_after)))
        phase_end(wait_cc=False)

        def osrc2(which, h, t0, n, l=l):
            g0 = h * T + t0
            k, tt = g0 // OC, g0 % OC
            off = 0 if which == "a" else 256
            return [(2 * r, 2 * r + 2, og[l][k].ap()[r * 512 + off:r * 512 + off + 256, tt:tt + n].rearrange("(c p) t -> p c t", p=128))
                    for r in range(2)]
        xres = xTh if l == 0 else x2f.ap()
        ctok = lambda h, t0, l=l: [("og", l, (h * T + t0) // OC)]
        build_merge(T, cx=Ctx(nc, P, banks, f"l{l}m_", dict(xT=xres, outT=x1f[l].ap(), osrc=True, osrc2=osrc2, selh=selh, ctok=ctok)))
        phase_end()
        if l % 2 == 0:
            def outb(t0, n):
                return x2b[t0 // XC].ap()[:, t0 % XC:t0 % XC + n].rearrange("(c p) t -> p c t", p=128)
            def x_after(tend):
                if tend % XC == 0:
                    k = tend // XC - 1
                    P.cc(lambda e, k=k: e.collective_compute("AllGather", ALU.bypass, replica_groups=groups,
                                                             ins=[x2b[k].ap().opt()], outs=[xg[k].ap().opt()]),
                         reads=[("x2b", k)], writes=[("xg", k)])
                    P.persist.add(("xg", k))
            build_ffn(T, cx=Ctx(nc, P, banks, f"l{l}f_", dict(xT=x1f[l].ap(), outT=x2f.ap(), outb=outb,
                                                              otok=lambda t0: [("x2b", t0 // XC)], after_store=x_after)))
            phase_end(wait_cc=False)
        else:
            build_moe2(T, cx=Ctx(nc, P, banks, f"l{l}e_", dict(xT=x1f[l].ap(), out=out)))
            phase_end()
    P.emit()
    return nc


def fused_in_maps(S, ncores, x, w_in, hg_lb_logits, hg_norm_w, cmp_pos, cmp_w1, cmp_w2, rel_bias, w_branch_a, w_branch_b,
                  w_out, ln1_g, ln1_b, ln2_g, ln2_b, ffn_w_gate, ffn_w_up, ffn_w_down, moe_router, moe_w_gate,
                  moe_w_up, moe_w_down):
    f = lambda a: np.asarray(a, dtype=np.float32)
    x = f(x); w_in = f(w_in); hg_lb_logits = f(hg_lb_logits); hg_norm_w = f(hg_norm_w)
    cmp_pos = f(cmp_pos); cmp_w1 = f(cmp_w1); cmp_w2 = f(cmp_w2); rel_bias = f(rel_bias)
    T = S // 2
    NT = 512
    cmask = np.ones((128, NT), np.float32); cmask[:, ::64] = 0
    tri = np.zeros((64, NT), np.float32)
    for c in range(NT // 64):
        tri[:, c * 64:(c + 1) * 64] = np.triu(np.ones((64, 64), np.float32))
    st = nsa_static()
    shared = dict(cmask=cmask, tri=tri, **st)
    per_layer = []
    for l in range(DEPTH):
        W = w_in[l]
        d = {}
        d[f"l{l}m_wgt"] = _c(W[:, 3352:5400]); d[f"l{l}m_wa"] = _c(f(w_branch_a)[l]); d[f"l{l}m_wb"] = _c(f(w_branch_b)[l])
        d[f"l{l}m_wo"] = _c(f(w_out)[l]); d[f"l{l}m_lnp"] = _c(np.concatenate([_pc(f(ln1_g)[l]), _pc(f(ln1_b)[l])], axis=1))
        d[f"l{l}n_w1"] = _c(cmp_w1[l].reshape(2, 32, 64, 128).transpose(0, 2, 1, 3)); d[f"l{l}n_posT"] = _c(cmp_pos[l].transpose(0, 2, 1))
        d[f"l{l}n_w2"] = _c(cmp_w2[l])
        if l % 2 == 0:
            d[f"l{l}f_wg"] = _c(f(ffn_w_gate)[l // 2]); d[f"l{l}f_wu"] = _c(f(ffn_w_up)[l // 2]); d[f"l{l}f_wd"] = _c(f(ffn_w_down)[l // 2])
            d[f"l{l}f_lnp"] = _c(np.concatenate([_pc(f(ln2_g)[l]), _pc(f(ln2_b)[l])], axis=1))
        else:
            d[f"l{l}e_wr"] = _c(f(moe_router)[l // 2]); d[f"l{l}e_mwg"] = _c(f(moe_w_gate)[l // 2]); d[f"l{l}e_mwu"] = _c(f(moe_w_up)[l // 2])
            d[f"l{l}e_mwd"] = _c(f(moe_w_down)[l // 2]); d[f"l{l}e_lngb"] = _c(np.stack([f(ln2_g)[l], f(ln2_b)[l]]))
        per_layer.append(d)
    maps = []
    for c in range(ncores):
        b, j = c // 2, c % 2
        xTb = _c(x[b].T)
        m = dict(xT=xTb, xTh=_c(xTb[:, j * T:(j + 1) * T]), selh=_c(np.tile(np.array([[1 - j, j]], np.float32), (128, 1))))
        m.update(shared)
        m["tabaug"] = _c(np.concatenate([rel_bias[:, j * 4:(j + 1) * 4], np.full((1, 4), NEG, np.float32)], 0))
        for l in range(DEPTH):
            W = w_in[l]
            m.update(per_layer[l])
            cs = slice(j * 256, (j + 1) * 256)
            m[f"l{l}h_hwq"] = _c(W[:, 0:512][:, cs]); m[f"l{l}h_hwf"] = _c(W[:, 512:1024][:, cs])
            m[f"l{l}h_hwi"] = _c(W[:, 1024:1536][:, cs]); m[f"l{l}h_hwg"] = _c(W[:, 1536:2048][:, cs])
            nw = hg_norm_w[l, cs]
            m[f"l{l}h_hp"] = _c(np.stack([np.ones(128, np.float32), np.ones(128, np.float32), nw[:128], nw[128:]], axis=1))
            m[f"l{l}h_lbl"] = _c(np.stack([hg_lb_logits[0, cs][:128], hg_lb_logits[0, cs][128:], hg_lb_logits[1, cs][:128], hg_lb_logits[1, cs][128:]], axis=1))
            g = j
            kv = lambda base: W[:, base + g * 64: base + (g + 1) * 64]
            m[f"l{l}n_nwq"] = _c(W[:, 2048 + g * 256:2048 + (g + 1) * 256])
            m[f"l{l}n_nwk"] = _c(np.concatenate([kv(2816), kv(3072), kv(2560), kv(2688)], 1))
            m[f"l{l}n_nwt"] = _c(np.concatenate([kv(2944), kv(3200), W[:, 3328 + g * 12:3328 + (g + 1) * 12]], 1))
        maps.append(m)
    return maps


def kernel(**inputs):
    x = np.asarray(inputs["x"])
    B, S, D = x.shape
    ncores = 2 * B
    nc = _prog(("fused", S, ncores), lambda: build_fused(S, ncores))
    maps = fused_in_maps(S, ncores, **inputs)
    res = run_bass_kernel_spmd(nc, maps, core_ids=list(range(ncores))).results
    T = S // 2
    out = np.empty((B, S, D), np.float32)
    for c in range(ncores):
        out[c // 2, (c % 2) * T:(c % 2 + 1) * T] = res[c]["out"]
    return out
```

```python
import numpy as np
import concourse.bass as bass
import concourse.mybir as mybir

F32 = mybir.dt.float32
BF16 = mybir.dt.bfloat16
I32 = mybir.dt.int32
AF = mybir.ActivationFunctionType
ALU = mybir.AluOpType
AX = mybir.AxisListType

ENGS = ("pe", "act", "dve", "pool", "sp")


class Op:
    __slots__ = ("eng", "fn", "deps", "is_dma", "idx", "inc", "semval", "dma_id", "dma_prev", "cc_id")

    def __init__(self, eng, fn, is_dma):
        self.eng = eng
        self.fn = fn
        self.deps = set()
        self.is_dma = is_dma
        self.inc = False
        self.semval = 0
        self.dma_id = -1
        self.dma_prev = None
        self.cc_id = -1


class Prog:
    NDMA_SEM = 24

    def __init__(self, nc, same_engine_sync=True):
        self.nc = nc
        self.ops = []
        self.last_w = {}
        self.readers = {}
        self.same_engine_sync = same_engine_sync
        self.n_dma = 0
        self.dma_ops = []
        self.cc_ops = []
        self._n = 0

    def sb(self, shape, dt, name=None):
        self._n += 1
        return self.nc.alloc_sbuf_tensor(name or f"sb{self._n}", list(shape), dt)

    def ps(self, shape, dt=F32, name=None):
        self._n += 1
        n = 2048 // (4 if dt == F32 else 2)
        assert shape[1] <= n
        t = self.nc.alloc_psum_tensor(f"psb{self._n}", [128, n], dt)
        return t[0:shape[0], 0:shape[1]]

    def op(self, eng, fn, reads=(), writes=(), dma=False):
        o = Op(eng, fn, dma)
        o.idx = len(self.ops)
        bank_r = [r for r in reads if isinstance(r, tuple) and r[0] == "bank"]
        if bank_r:
            reads = [r for r in reads if r not in bank_r]
            writes = list(writes) + bank_r
        for r in reads:
            w = self.last_w.get(r)
            if w is not None:
                o.deps.add(w)
        for w_ in writes:
            w = self.last_w.get(w_)
            if w is not None:
                o.deps.add(w)
            for r in self.readers.get(w_, ()):
                o.deps.add(r)
        o.deps.discard(o.idx)
        for r in reads:
            self.readers.setdefault(r, []).append(o.idx)
        for w_ in writes:
            self.last_w[w_] = o.idx
            self.readers[w_] = []
        if dma:
            o.dma_id = self.n_dma
            self.n_dma += 1
            self.dma_ops.append(o.idx)
            if o.dma_id >= self.NDMA_SEM:
                o.dma_prev = self.dma_ops[o.dma_id - self.NDMA_SEM]
        self.ops.append(o)
        return o

    def dma(self, eng, out, in_, reads=(), writes=(), **kw):
        return self.op(eng, lambda e: e.dma_start(out=out, in_=in_, **kw), reads, writes, dma=True)

    def cc(self, fn, reads=(), writes=()):
        o = self.op("pool", fn, reads, writes)
        o.cc_id = len(self.cc_ops)
        self.cc_ops.append(o.idx)
        return o

    def barrier(self):
        last = {}
        for o in self.ops:
            if o.fn is not None and not o.is_dma and o.cc_id < 0 and o.eng != "sp":
                last[o.eng] = o.idx
        deps = set(last.values()) | set(self.dma_ops[-self.NDMA_SEM:]) | set(self.cc_ops)
        for e in ENGS:
            b = Op(e, None, False)
            b.idx = len(self.ops)
            b.deps = set(deps)
            self.ops.append(b)
        self.last_w = {}
        self.readers = {}

    def emit(self):
        nc = self.nc
        ops = self.ops
        fin = Op("sp", None, False)
        fin.idx = len(ops)
        fin.deps = set(self.dma_ops[-self.NDMA_SEM:]) | {i for i in self.dma_ops}
        ops.append(fin)
        for o in ops:
            nd = set()
            for d in o.deps:
                p = ops[d]
                if p.is_dma or p.cc_id >= 0:
                    nd.add(d)
                    continue
                if p.eng == o.eng and not o.is_dma:
                    if p.eng == "pe" or p.eng == "sp" or not self.same_engine_sync:
                        continue
                nd.add(d)
            if o.dma_prev is not None:
                nd.add(o.dma_prev)
            o.deps = nd
            for d in nd:
                ops[d].inc = True
        val = {e: 0 for e in ENGS}
        for o in ops:
            if o.is_dma:
                o.semval = 16 * (o.dma_id // self.NDMA_SEM + 1)
            elif o.cc_id >= 0:
                o.semval = 1
            elif o.inc:
                val[o.eng] += 1
                o.semval = val[o.eng]
        self.maxval = dict(val)
        sem = {e: nc.alloc_semaphore(f"s_{e}") for e in ENGS if e != "sp"}
        dsem = [nc.alloc_semaphore(f"s_dma{i}") for i in range(min(self.NDMA_SEM, max(1, self.n_dma)))]
        csem = [nc.alloc_semaphore(f"s_cc{i}") for i in range(len(self.cc_ops))]
        per = {e: [o for o in ops if o.eng == e] for e in ENGS}

        def run(e, engobj):
            seen = {}
            for o in per[e]:
                need = {}
                for d in o.deps:
                    p = ops[d]
                    key = ("d", p.dma_id % self.NDMA_SEM) if p.is_dma else (("c", p.cc_id) if p.cc_id >= 0 else ("e", p.eng))
                    if p.semval > need.get(key, 0):
                        need[key] = p.semval
                for key, v in need.items():
                    if seen.get(key, 0) >= v:
                        continue
                    seen[key] = v
                    s = dsem[key[1]] if key[0] == "d" else (csem[key[1]] if key[0] == "c" else sem[key[1]])
                    engobj.wait_ge(s, v)
                if o.fn is None:
                    continue
                ins = o.fn(engobj)
                if o.is_dma:
                    ins.then_inc(dsem[o.dma_id % self.NDMA_SEM], 16)
                elif o.cc_id >= 0:
                    ins.then_inc(csem[o.cc_id])
                elif o.inc:
                    ins.then_inc(sem[e], 1)

        with nc.Block() as block:
            if per["pe"]:
                block.tensor(lambda t: run("pe", t))
            if per["act"]:
                block.scalar(lambda t: run("act", t))
            if per["dve"]:
                block.vector(lambda t: run("dve", t))
            if per["pool"]:
                block.gpsimd(lambda t: run("pool", t))
            block.sync(lambda t: run("sp", t))


D_MODEL = 1024
DEPTH = 2
DN_ALPHA = (2 * DEPTH) ** 0.25
LN_EPS = 1e-5
D_FF = 2752


def new_nc():
    return bass.Bass("TRN2", target_bir_lowering=False)


class Ctx:
    def __init__(self, nc=None, P=None, banks=None, prefix="", ov=None):
        self.standalone = nc is None
        self.nc = nc if nc is not None else new_nc()
        self.P = P if P is not None else Prog(self.nc)
        self.banks = banks if banks is not None else [self.P.ps([128, 512]) for _ in range(8)]
        self.prefix = prefix
        self.ov = ov or {}
        if not hasattr(self.P, "_tens"):
            self.P._tens = {}

    def inp(self, name, shape, dt=F32, shared=False):
        if name in self.ov:
            return self.ov[name]
        full = name if shared else self.prefix + name
        if full not in self.P._tens:
            self.P._tens[full] = self.nc.dram_tensor(full, list(shape), dt, kind="ExternalInput").ap()
        return self.P._tens[full]

    def out(self, name, shape, dt=F32):
        if name in self.ov:
            return self.ov[name]
        return self.nc.dram_tensor(name, list(shape), dt, kind="ExternalOutput").ap()

    def done(self):
        if self.standalone:
            self.P.emit()
            return self.nc
        return None


def mm_acc(P, out, pairs, reads, writes):
    n = len(pairs)
    for i, (l, r) in enumerate(pairs):
        P.op("pe", (lambda e, l=l, r=r, i=i: e.matmul(out, l, r, start=(i == 0), stop=(i == n - 1))),
             reads=reads[i] if (len(reads) and isinstance(reads[0], list)) else reads, writes=writes)


def load_w_bf16(P, dst, w, K, N, tok, nsplit=4):
    kc_full = K // 128
    step = max(1, (kc_full + nsplit - 1) // nsplit)
    for k0 in range(0, kc_full, step):
        k1 = min(kc_full, k0 + step)
        P.dma("pool", dst[:, k0:k1, :], w[k0 * 128:k1 * 128, :].rearrange("(c p) n -> p c n", p=128), writes=[tok])
    rem = K - kc_full * 128
    if rem:
        P.dma("pool", dst[0:rem, kc_full, :], w[kc_full * 128:K, :], writes=[tok])


def load_w_cols(P, dst, w, K, c0, c1, tok):
    kc_full = K // 128
    P.dma("pool", dst[:, 0:kc_full, c0:c1], w[0:kc_full * 128, c0:c1].rearrange("(c p) n -> p c n", p=128), writes=[tok])
    rem = K - kc_full * 128
    if rem:
        P.dma("pool", dst[0:rem, kc_full, c0:c1], w[kc_full * 128:K, c0:c1], writes=[tok])


def layer_norm_T(P, xf, b, NT, ones_b, rb, rsq, ps_mean, ps_msq, st, lng, lnb, tag,
                 rbt=lambda mo: ("rb", mo), rsqt=lambda mo: ("rsq", mo)):
    for mo in range(8):
        P.op("act", lambda e, mo=mo: e.activation(out=rb[:, mo, :], in_=xf[:, mo, :], func=AF.Copy),
             reads=[("xf", b, mo)], writes=[rbt(mo)])
        P.op("act", lambda e, mo=mo: e.activation(out=rsq[:, mo, :], in_=xf[:, mo, :], func=AF.Square),
             reads=[("xf", b, mo)], writes=[rsqt(mo)])
    mm_acc(P, ps_mean, [(ones_b[:], rb[:, mo, :]) for mo in range(8)],
           [[rbt(mo), "ones"] for mo in range(8)], [("bank", 6)])
    mm_acc(P, ps_msq, [(ones_b[:], rsq[:, mo, :]) for mo in range(8)],
           [[rsqt(mo), "ones"] for mo in range(8)], [("bank", 7)])
    mean_s, m2, rstd = st[:, 0, :], st[:, 1, :], st[:, 2, :]
    P.op("dve", lambda e: e.tensor_copy(out=mean_s, in_=ps_mean), reads=[("bank", 6)], writes=["mean_s"])
    P.op("dve", lambda e: e.tensor_tensor(out=m2, in0=mean_s, in1=mean_s, op=ALU.mult), reads=["mean_s"], writes=["m2"])
    P.op("dve", lambda e: e.tensor_tensor(out=rstd, in0=ps_msq, in1=m2, op=ALU.subtract), reads=[("bank", 7), "m2"], writes=["rstd"])
    P.op("dve", lambda e: e.tensor_scalar(out=rstd, in0=rstd, scalar1=LN_EPS, scalar2=None, op0=ALU.add),
         reads=["rstd"], writes=["rstd"])
    P.op("act", lambda e: e.activation(out=rstd, in_=rstd, func=AF.Sqrt), reads=["rstd"], writes=["rstd"])
    P.op("dve", lambda e: e.reciprocal(out=rstd, in_=rstd), reads=["rstd"], writes=["rstd"])
    for mo in range(8):
        P.op("dve", lambda e, mo=mo: e.tensor_tensor(out=xf[:, mo, :], in0=xf[:, mo, :], in1=mean_s, op=ALU.subtract),
             reads=[("xf", b, mo), "mean_s"], writes=[("xf", b, mo)])
        P.op("dve", lambda e, mo=mo: e.tensor_tensor(out=xf[:, mo, :], in0=xf[:, mo, :], in1=rstd, op=ALU.mult),
             reads=[("xf", b, mo), "rstd"], writes=[("xf", b, mo)])
        P.op("dve", lambda e, mo=mo: e.tensor_scalar(out=xf[:, mo, :], in0=xf[:, mo, :], scalar1=lng[:, mo:mo + 1],
                                                      scalar2=lnb[:, mo:mo + 1], op0=ALU.mult, op1=ALU.add),
             reads=[("xf", b, mo), "lnp"], writes=[("xf", b, mo)])


def build_ffn(T, NT=512, cx=None):
    cx = cx or Ctx()
    nc, P = cx.nc, cx.P
    xT = cx.inp("xT", [1024, T])
    wg = cx.inp("wg", [1024, D_FF]); wu = cx.inp("wu", [1024, D_FF]); wd = cx.inp("wd", [D_FF, 1024])
    lnp = cx.inp("lnp", [128, 16])
    outT = cx.out("outT", [1024, T])
    outb = cx.ov.get("outb")
    MT = 22
    wgb = P.sb([128, 8, D_FF], BF16)
    wub = P.sb([128, 8, D_FF], BF16)
    wdb = P.sb([128, MT, 1024], BF16)
    lns = P.sb([128, 16], F32)
    ones_b = P.sb([128, 128], BF16)
    xb = [P.sb([128, 8, NT], BF16) for _ in range(2)]
    xf1 = P.sb([128, 8, NT], F32)
    xf = [xf1, xf1]
    h = P.sb([128, MT, NT], BF16)
    sg = [P.sb([128, NT], F32) for _ in range(2)]
    rb = h[:, 0:8, :]
    rsq = h[:, 8:16, :]
    st = P.sb([128, 3, NT], F32)
    bk = [b_[:, 0:NT] for b_ in cx.banks]
    psg, psu, psf, ps_mean, ps_msq = bk[0:2], bk[2:4], bk[4:6], bk[6], bk[7]
    BG, BU, BF_, BMEAN, BMSQ = (0, 1), (2, 3), (4, 5), 6, 7

    P.dma("sp", lns[:], lnp, writes=["lnp"])
    P.op("dve", lambda e: e.memset(ones_b[:], 1.0 / 1024), writes=["ones"])
    xTv = xT.rearrange("(c p) t -> p c t", p=128)
    oTv = outT.rearrange("(c p) t -> p c t", p=128)
    ntiles = T // NT

    def load_x(ti):
        b = ti % 2
        P.dma("pool", xb[b][:], xTv[:, :, ti * NT:(ti + 1) * NT], writes=[("xb", b)])

    def load_xf(ti):
        P.dma("sp", xf1[:], xTv[:, :, ti * NT:(ti + 1) * NT], writes=[("xf", 0, mo) for mo in range(8)])

    load_x(0)
    load_xf(0)
    WB = 6
    for blk in range((MT + WB - 1) // WB):
        c0, c1 = blk * WB * 128, min(D_FF, (blk + 1) * WB * 128)
        load_w_cols(P, wgb, wg, 1024, c0, c1, ("wg", blk))
        load_w_cols(P, wub, wu, 1024, c0, c1, ("wu", blk))
    load_w_bf16(P, wdb, wd, D_FF, 1024, "wd")
    for ti in range(ntiles):
        b = ti % 2
        if ti + 1 < ntiles:
            load_x(ti + 1)
        for m in range(MT):
            msz = min(128, D_FF - m * 128)
            ms = slice(m * 128, m * 128 + msz)
            pb = m % 2
            mm_acc(P, psg[pb][0:msz, :], [(wgb[:, kc, ms], xb[b][:, kc, :]) for kc in range(8)],
                   [("wg", m // WB), ("xb", b)], [("bank", BG[pb])])
            mm_acc(P, psu[pb][0:msz, :], [(wub[:, kc, ms], xb[b][:, kc, :]) for kc in range(8)],
                   [("wu", m // WB), ("xb", b)], [("bank", BU[pb])])
            P.op("act", lambda e, pb=pb, msz=msz: e.activation(out=sg[pb][0:msz, :], in_=psg[pb][0:msz, :], func=AF.Silu),
                 reads=[("bank", BG[pb])], writes=[("sg", pb)])
            P.op("dve", lambda e, pb=pb, msz=msz, m=m: e.tensor_tensor(out=h[0:msz, m, :], in0=sg[pb][0:msz, :],
                                                                       in1=psu[pb][0:msz, :], op=ALU.mult),
                 reads=[("sg", pb), ("bank", BU[pb])], writes=[("h", m)])
        for mo in range(8):
            pb = mo % 2
            pairs, rds = [], []
            for kc in range(MT):
                ksz = min(128, D_FF - kc * 128)
                pairs.append((wdb[0:ksz, kc, mo * 128:(mo + 1) * 128], h[0:ksz, kc, :]))
                rds.append(["wd", ("h", kc)])
            mm_acc(P, psf[pb], pairs, rds, [("bank", BF_[pb])])
            P.op("dve", lambda e, pb=pb, mo=mo: e.scalar_tensor_tensor(
                out=xf1[:, mo, :], in0=xf1[:, mo, :], scalar=float(DN_ALPHA), in1=psf[pb],
                op0=ALU.mult, op1=ALU.add), reads=[("bank", BF_[pb]), ("xf", 0, mo)], writes=[("xf", 0, mo)])
        layer_norm_T(P, xf1, 0, NT, ones_b, rb, rsq, ps_mean, ps_msq, st, lns[:, 0:8], lns[:, 8:16], "ln2",
                     rbt=lambda mo: ("h", mo), rsqt=lambda mo: ("h", 8 + mo))
        P.dma("sp", oTv[:, :, ti * NT:(ti + 1) * NT], xf1[:], reads=[("xf", 0, mo) for mo in range(8)])
        if outb is not None:
            P.dma("pool", outb(ti * NT, NT), xf1[:], reads=[("xf", 0, mo) for mo in range(8)], writes=cx.ov.get("otok", lambda t0: [])(ti * NT))
            if cx.ov.get("after_store") is not None:
                cx.ov["after_store"]((ti + 1) * NT)
        if ti + 1 < ntiles:
            load_xf(ti + 1)
    return cx.done()


def build_merge(T, NT=512, cx=None):
    cx = cx or Ctx()
    nc, P = cx.nc, cx.P
    xT = cx.inp("xT", [1024, T])
    osrc = cx.ov.get("osrc")
    if osrc is None:
        oaT = cx.inp("oaT", [512, T], BF16); obT = cx.inp("obT", [512, T], BF16)
    wgt = cx.inp("wgt", [1024, 2048]); wa = cx.inp("wa", [512, 1024]); wb = cx.inp("wb", [512, 1024]); wo = cx.inp("wo", [1024, 1024])
    lnp = cx.inp("lnp", [128, 16])
    outT = cx.out("outT", [1024, T])
    wgtb = P.sb([128, 8, 2048], BF16)
    wab = P.sb([128, 4, 1024], BF16)
    wbb = P.sb([128, 4, 1024], BF16)
    wob = P.sb([128, 8, 1024], BF16)
    lns = P.sb([128, 16], F32)
    ones_b = P.sb([128, 128], BF16)
    xb = [P.sb([128, 8, NT], BF16) for _ in range(2)]
    xf = [P.sb([128, 8, NT], F32) for _ in range(2)]
    oa = [P.sb([128, 4, NT], BF16) for _ in range(2)]
    ob = [P.sb([128, 4, NT], BF16) for _ in range(2)]
    y = P.sb([128, 8, NT], BF16)
    sga = [P.sb([128, NT], F32) for _ in range(2)]
    sgb = [P.sb([128, NT], F32) for _ in range(2)]
    rb = P.sb([128, 8, NT], BF16)
    rsq = P.sb([128, 8, NT], BF16)
    st = P.sb([128, 3, NT], F32)
    bk = [b_[:, 0:NT] for b_ in cx.banks]
    pq, pmx, ps_mean, ps_msq = bk[0:4], bk[4:6], bk[6], bk[7]
    P.dma("sp", lns[:], lnp, writes=["lnp"])
    P.op("dve", lambda e: e.memset(ones_b[:], 1.0 / 1024), writes=["ones"])
    xTv = xT.rearrange("(c p) t -> p c t", p=128)
    if osrc is None:
        oaTv = oaT.rearrange("(c p) t -> p c t", p=128)
        obTv = obT.rearrange("(c p) t -> p c t", p=128)
        osrc = lambda which, t0, n: [(0, 4, (oaTv if which == "a" else obTv)[:, :, t0:t0 + n])]
    oTv = outT.rearrange("(c p) t -> p c t", p=128)
    ntiles = T // NT

    selh = cx.ov.get("selh")
    if selh is not None:
        sels = P.sb([128, 2], F32)
        P.dma("sp", sels[:], selh, writes=["sels"])
        cand = {(w_, h_): P.sb([128, 4, NT], BF16) for w_ in "ab" for h_ in range(2)}

    def load_x(ti):
        b = ti % 2
        ts = slice(ti * NT, (ti + 1) * NT)
        P.dma("pool", xb[b][:], xTv[:, :, ts], writes=[("xb", b)])
        P.dma("sp", xf[b][:], xTv[:, :, ts], writes=[("xf", b, mo) for mo in range(8)])
        if selh is not None:
            for w_, dst_, tk_ in (("a", oa, "oa"), ("b", ob, "ob")):
                for h_ in range(2):
                    for (c0, c1, ap_) in cx.ov["osrc2"](w_, h_, ti * NT, NT):
                        P.dma("sp", cand[(w_, h_)][:, c0:c1, :], ap_, writes=[("cand", w_, h_)])
                P.op("dve", lambda e, w_=w_: e.tensor_scalar(out=cand[(w_, 0)][:], in0=cand[(w_, 0)][:], scalar1=sels[:, 0:1], scalar2=None, op0=ALU.mult),
                     reads=["sels", ("cand", w_, 0)], writes=[("cand", w_, 0)])
                P.op("dve", lambda e, w_=w_, dst_=dst_, b=b: e.scalar_tensor_tensor(out=dst_[b][:], in0=cand[(w_, 1)][:], scalar=sels[:, 1:2], in1=cand[(w_, 0)][:],
                                                                                 op0=ALU.mult, op1=ALU.add),
                     reads=["sels", ("cand", w_, 0), ("cand", w_, 1)], writes=[(tk_, b)])
            return
        for (c0, c1, ap_) in osrc("a", ti * NT, NT):
            P.dma("sp", oa[b][:, c0:c1, :], ap_, writes=[("oa", b)])
        for (c0, c1, ap_) in osrc("b", ti * NT, NT):
            P.dma("sp", ob[b][:, c0:c1, :], ap_, writes=[("ob", b)])

    load_x(0)
    for blk in range(4):
        c0, c1 = blk * 256, (blk + 1) * 256
        load_w_cols(P, wgtb, wgt, 1024, c0, c1, ("wgt", blk))
        load_w_cols(P, wab, wa, 512, c0, c1, ("wa", blk))
        load_w_cols(P, wgtb, wgt, 1024, 1024 + c0, 1024 + c1, ("wgt", blk))
        load_w_cols(P, wbb, wb, 512, c0, c1, ("wb", blk))
    load_w_bf16(P, wob, wo, 1024, 1024, "wo", nsplit=2)
    for ti in range(ntiles):
        b = ti % 2
        if ti + 1 < ntiles:
            load_x(ti + 1)
        for mo in range(8):
            pb = mo % 2
            ms = slice(mo * 128, (mo + 1) * 128)
            ms2 = slice(1024 + mo * 128, 1024 + (mo + 1) * 128)
            mm_acc(P, pq[0], [(wgtb[:, kc, ms], xb[b][:, kc, :]) for kc in range(8)], [("wgt", mo // 2), ("xb", b)], [("bank", 0)])
            mm_acc(P, pq[1], [(wab[:, kc, ms], oa[b][:, kc, :]) for kc in range(4)], [("wa", mo // 2), ("oa", b)], [("bank", 1)])
            mm_acc(P, pq[2], [(wgtb[:, kc, ms2], xb[b][:, kc, :]) for kc in range(8)], [("wgt", mo // 2), ("xb", b)], [("bank", 2)])
            mm_acc(P, pq[3], [(wbb[:, kc, ms], ob[b][:, kc, :]) for kc in range(4)], [("wb", mo // 2), ("ob", b)], [("bank", 3)])
            P.op("act", lambda e, pb=pb: e.activation(out=sga[pb][:], in_=pq[0], func=AF.Sigmoid),
                 reads=[("bank", 0)], writes=[("sga", pb)])
            P.op("act", lambda e, pb=pb: e.activation(out=sgb[pb][:], in_=pq[2], func=AF.Sigmoid),
                 reads=[("bank", 2)], writes=[("sgb", pb)])
            P.op("dve", lambda e, pb=pb: e.tensor_tensor(out=sga[pb][:], in0=sga[pb][:], in1=pq[1], op=ALU.mult),
                 reads=[("sga", pb), ("bank", 1)], writes=[("sga", pb)])
            P.op("dve", lambda e, pb=pb: e.tensor_tensor(out=sgb[pb][:], in0=sgb[pb][:], in1=pq[3], op=ALU.mult),
                 reads=[("sgb", pb), ("bank", 3)], writes=[("sgb", pb)])
            P.op("dve", lambda e, pb=pb, mo=mo: e.tensor_tensor(out=y[:, mo, :], in0=sga[pb][:], in1=sgb[pb][:], op=ALU.add),
                 reads=[("sga", pb), ("sgb", pb)], writes=[("y", mo)])
        for mo in range(8):
            pb = mo % 2
            ms = slice(mo * 128, (mo + 1) * 128)
            mm_acc(P, pmx[pb], [(wob[:, kc, ms], y[:, kc, :]) for kc in range(8)],
                   [["wo", ("y", kc)] for kc in range(8)], [("bank", 4 + pb)])
            P.op("dve", lambda e, pb=pb, mo=mo, b=b: e.scalar_tensor_tensor(
                out=xf[b][:, mo, :], in0=xf[b][:, mo, :], scalar=float(DN_ALPHA), in1=pmx[pb],
                op0=ALU.mult, op1=ALU.add), reads=[("bank", 4 + pb), ("xf", b, mo)], writes=[("xf", b, mo)])
        layer_norm_T(P, xf[b], b, NT, ones_b, rb, rsq, ps_mean, ps_msq, st, lns[:, 0:8], lns[:, 8:16], "ln1")
        P.dma("sp", oTv[:, :, ti * NT:(ti + 1) * NT], xf[b][:], reads=[("xf", b, mo) for mo in range(8)])
    return cx.done()


def build_hgrn(S, NT=512, layer=0, cx=None):
    cx = cx or Ctx()
    nc, P = cx.nc, cx.P
    xsrc = cx.ov.get("xsrc")
    if xsrc is None:
        xT = cx.inp("xT", [1024, S])
        xTv_ = xT.rearrange("(c p) t -> p c t", p=128)
        xsrc = lambda t0, n: (xTv_[:, :, t0:t0 + n], "pool")
    wq = cx.inp("hwq", [1024, 256]); wf = cx.inp("hwf", [1024, 256]); wi = cx.inp("hwi", [1024, 256]); wg = cx.inp("hwg", [1024, 256])
    hp = cx.inp("hp", [128, 4]); lbl = cx.inp("lbl", [128, 4])
    cmask = cx.inp("cmask", [128, NT], shared=True); tri = cx.inp("tri", [64, NT], shared=True)
    ident = cx.inp("ident", [128, 128], shared=True)
    odst = cx.ov.get("odst")
    if odst is None:
        oaT = cx.out("oaT", [256, S], BF16)
        odst = lambda row0, t0, n: oaT[row0:row0 + 128, t0:t0 + n]
    NCH = NT // 64
    wqb = P.sb([128, 8, 256], BF16); wfb = P.sb([128, 8, 256], BF16)
    wib = P.sb([128, 8, 256], BF16); wgb = P.sb([128, 8, 256], BF16)
    hps = P.sb([128, 4], F32)
    cm = P.sb([128, NT], F32)
    tr = P.sb([64, NT], F32)
    idb = P.sb([128, 128], BF16)
    ones_b = P.sb([128, 128], BF16)
    xb = [P.sb([128, 8, NT], BF16) for _ in range(2)]
    banks = cx.banks
    rot = [0]

    def rbank():
        i = rot[0] % 3
        rot[0] += 1
        return i
    B_ATT = 3
    H = []
    for hd in range(2):
        d = dict(
            kk=P.sb([128, NT], F32), lf=P.sb([128, NT], F32), bb=P.sb([128, NT], F32), eb=P.sb([128, NT], F32),
            ex=P.sb([128, NT], F32), qf=P.sb([128, NT], F32), sgl=P.sb([128, NT], F32), rs=P.sb([128, NT], F32),
            qt=P.sb([128, NT], BF16), kt=P.sb([128, NT], BF16), kd=P.sb([128, NT], BF16),
            vtm=P.sb([64, NCH, 128], BF16), kdt=P.sb([64, NCH, 128], BF16), attm=P.sb([64, NT], BF16),
            Sf=P.sb([128, 128], F32), Sb=[P.sb([128, 128], BF16) for _ in range(2)], osq=P.sb([128, NT], BF16),
            ob=P.sb([128, NT], BF16))
        H.append(d)
    P.dma("sp", hps[:], hp, writes=["hp"])
    lbs_ = P.sb([128, 4], F32)
    P.dma("sp", lbs_[:], lbl, writes=["lbl"])
    if layer == 0:
        P.op("dve", lambda e: e.memset(hps[:, 0:2], 1.0), reads=["lbl"], writes=["hp"])
    else:
        P.op("dve", lambda e: e.tensor_tensor(out=hps[:, 0:2], in0=lbs_[:, 0:2], in1=lbs_[:, 2:4], op=ALU.subtract), reads=["lbl"], writes=["hp"])
        P.op("act", lambda e: e.activation(out=hps[:, 0:2], in_=hps[:, 0:2], func=AF.Sigmoid), reads=["hp"], writes=["hp"])
    P.dma("sp", cm[:], cmask, writes=["cm"])
    P.dma("sp", tr[:], tri, writes=["tr"])
    P.dma("pool", idb[:], ident, writes=["idb"])
    P.op("dve", lambda e: e.memset(ones_b[:], 1.0 / 128), writes=["ones"])
    for hd in range(2):
        P.op("dve", lambda e, hd=hd: e.memset(H[hd]["Sf"][:], 0.0), writes=[("Sf", hd)])
    ntiles = S // NT

    def load_x(ti):
        b = ti % 2
        ap_, eng_ = xsrc(ti * NT, NT)
        P.dma(eng_, xb[b][:], ap_, writes=[("xb", b)])
    load_x(0)
    load_w_bf16(P, wqb, wq, 1024, 256, "wq", nsplit=1)
    load_w_bf16(P, wfb, wf, 1024, 256, "wf", nsplit=1)
    load_w_bf16(P, wib, wi, 1024, 256, "wi", nsplit=1)
    load_w_bf16(P, wgb, wg, 1024, 256, "wg", nsplit=1)
    gch = 0
    for ti in range(ntiles):
        b = ti % 2
        if ti + 1 < ntiles:
            load_x(ti + 1)
        def prep(hd):
            h = H[hd]
            hs = slice(hd * 128, (hd + 1) * 128)
            T = lambda n: (n, hd)
            B_O, B_S = 4 + hd, 6 + hd
            bi = rbank()
            mm_acc(P, banks[bi], [(wfb[:, kc, hs], xb[b][:, kc, :]) for kc in range(8)], ["wf", ("xb", b)], [("bank", bi)])
            P.op("act", lambda e, h=h, bi=bi: e.activation(out=h["kk"][:], in_=banks[bi], func=AF.Sigmoid, scale=-1.0),
                 reads=[("bank", bi)], writes=[T("kk")])
            P.op("dve", lambda e, h=h, hd=hd: e.tensor_scalar(out=h["kk"][:], in0=h["kk"][:], scalar1=hps[:, hd:hd + 1], scalar2=None, op0=ALU.mult),
                 reads=[T("kk"), "hp"], writes=[T("kk")])
            P.op("act", lambda e, h=h: e.activation(out=h["lf"][:], in_=h["kk"][:], func=AF.Ln, scale=-1.0, bias=1.0),
                 reads=[T("kk")], writes=[T("lf")])
            P.op("dve", lambda e, h=h: e.tensor_tensor_scan(out=h["bb"][:], data0=cm[:], data1=h["lf"][:], initial=0.0,
                                                            op0=ALU.mult, op1=ALU.add),
                 reads=[T("lf"), "cm"], writes=[T("bb")])
            P.op("act", lambda e, h=h: e.activation(out=h["eb"][:], in_=h["bb"][:], func=AF.Exp), reads=[T("bb")], writes=[T("eb")])
            bi = rbank()
            mm_acc(P, banks[bi], [(wqb[:, kc, hs], xb[b][:, kc, :]) for kc in range(8)], ["wq", ("xb", b)], [("bank", bi)])
            P.op("dve", lambda e, h=h, bi=bi: e.tensor_tensor(out=h["qt"][:], in0=banks[bi], in1=h["eb"][:], op=ALU.mult),
                 reads=[("bank", bi), T("eb")], writes=[T("qt")])
            P.op("act", lambda e, h=h: e.activation(out=h["ex"][:], in_=h["bb"][:], func=AF.Exp, scale=-1.0), reads=[T("bb")], writes=[T("ex")])
            P.op("dve", lambda e, h=h: e.tensor_tensor(out=h["kt"][:], in0=h["kk"][:], in1=h["ex"][:], op=ALU.mult),
                 reads=[T("kk"), T("ex")], writes=[T("kt")])
            for c in range(NCH):
                cs = slice(c * 64, (c + 1) * 64)
                P.op("act", lambda e, h=h, cs=cs, c=c: e.activation(out=h["ex"][:, cs], in_=h["bb"][:, cs], func=AF.Exp, scale=-1.0,
                                                                    bias=h["bb"][:, c * 64 + 63:c * 64 + 64]),
                     reads=[T("bb"), T("kt")], writes=[T("ex")])
            P.op("dve", lambda e, h=h: e.tensor_tensor(out=h["kd"][:], in0=h["kk"][:], in1=h["ex"][:], op=ALU.mult),
                 reads=[T("kk"), T("ex")], writes=[T("kd")])
            bi = rbank()
            mm_acc(P, banks[bi], [(wgb[:, kc, hs], xb[b][:, kc, :]) for kc in range(8)], ["wg", ("xb", b)], [("bank", bi)])
            P.op("act", lambda e, h=h, bi=bi: e.activation(out=h["sgl"][:], in_=banks[bi], func=AF.Silu), reads=[("bank", bi)], writes=[T("sgl")])
            for half in range(NCH // 4):
                bi = rbank()
                for cc in range(4):
                    c = half * 4 + cc
                    mm_acc(P, banks[bi][0:64, cc * 128:(cc + 1) * 128],
                           [(xb[b][:, kc, c * 64:(c + 1) * 64], wib[:, kc, hs]) for kc in range(8)], ["wi", ("xb", b)], [("bank", bi)])
                P.op("act", lambda e, h=h, bi=bi, half=half: e.activation(
                    out=h["vtm"][:, half * 4:(half + 1) * 4, :], in_=banks[bi][0:64, :].rearrange("p (c v) -> p c v", v=128), func=AF.Copy),
                    reads=[("bank", bi)], writes=[T("vtm")])
            for half in range(NCH // 4):
                bi = rbank()
                for cc in range(4):
                    c = half * 4 + cc
                    P.op("pe", lambda e, h=h, bi=bi, cc=cc, c=c: e.matmul(banks[bi][0:64, cc * 128:(cc + 1) * 128], h["kd"][:, c * 64:(c + 1) * 64],
                                                                          idb[:], start=True, stop=True),
                         reads=[T("kd"), "idb"], writes=[("bank", bi)])
                P.op("dve", lambda e, h=h, bi=bi, half=half: e.tensor_copy(
                    out=h["kdt"][:, half * 4:(half + 1) * 4, :], in_=banks[bi][0:64, :].rearrange("p (c v) -> p c v", v=128)),
                    reads=[("bank", bi)], writes=[T("kdt")])
            for c in range(NCH):
                cs = slice(c * 64, (c + 1) * 64)
                P.op("pe", lambda e, h=h, cs=cs: e.matmul(banks[B_ATT][0:64, cs], h["kt"][:, cs], h["qt"][:, cs], start=True, stop=True),
                     reads=[T("kt"), T("qt")], writes=[("bank", B_ATT)])
            P.op("dve", lambda e, h=h: e.tensor_tensor(out=h["attm"][:], in0=banks[B_ATT][0:64, 0:NT], in1=tr[:], op=ALU.mult),
                 reads=[("bank", B_ATT), "tr"], writes=[T("attm")])
        def chunk(hd, c):
            h = H[hd]
            hs = slice(hd * 128, (hd + 1) * 128)
            T = lambda n: (n, hd)
            B_O, B_S = 4 + hd, 6 + hd
            cs = slice(c * 64, (c + 1) * 64)
            g = ti * NCH + c
            sb_cur = h["Sb"][g % 2]
            first = (g == 0)
            P.op("pe", lambda e, h=h, cs=cs, c=c, first=first: e.matmul(banks[B_O][:, cs], h["vtm"][:, c, :], h["attm"][:, cs],
                                                                        start=True, stop=first),
                 reads=[T("vtm"), T("attm")], writes=[("bank", B_O)])
            if not first:
                P.op("pe", lambda e, h=h, cs=cs, sb_cur=sb_cur: e.matmul(banks[B_O][:, cs], sb_cur[:], h["qt"][:, cs], start=False, stop=True),
                     reads=[("Sb", hd, g % 2), T("qt")], writes=[("bank", B_O)])
            P.op("pe", lambda e, h=h, c=c: e.matmul(banks[B_S][:, 0:128], h["kdt"][:, c, :], h["vtm"][:, c, :], start=True, stop=True),
                 reads=[T("kdt"), T("vtm")], writes=[("bank", B_S)])
            P.op("dve", lambda e, h=h, c=c: e.scalar_tensor_tensor(out=h["Sf"][:], in0=h["Sf"][:], scalar=h["eb"][:, c * 64 + 63:c * 64 + 64],
                                                                     in1=banks[B_S][:, 0:128], op0=ALU.mult, op1=ALU.add),
                 reads=[("Sf", hd), ("bank", B_S), T("eb")], writes=[("Sf", hd)])
            nxt = h["Sb"][(g + 1) % 2]
            P.op("act", lambda e, h=h, nxt=nxt: e.activation(out=nxt[:], in_=h["Sf"][:], func=AF.Copy),
                 reads=[("Sf", hd)], writes=[("Sb", hd, (g + 1) % 2)])
        def post(hd):
            h = H[hd]
            hs = slice(hd * 128, (hd + 1) * 128)
            T = lambda n: (n, hd)
            B_O, B_S = 4 + hd, 6 + hd
            P.op("act", lambda e, h=h: e.activation(out=h["osq"][:], in_=banks[B_O][:, 0:NT], func=AF.Square), reads=[("bank", B_O)], writes=[T("osq")])
            bi = rbank()
            P.op("pe", lambda e, h=h, bi=bi: e.matmul(banks[bi][:, 0:NT], ones_b[:], h["osq"][:], start=True, stop=True),
                 reads=["ones", T("osq")], writes=[("bank", bi)])
            P.op("dve", lambda e, h=h, bi=bi: e.tensor_scalar(out=h["rs"][:], in0=banks[bi][:, 0:NT], scalar1=1e-6, scalar2=None, op0=ALU.add),
                 reads=[("bank", bi)], writes=[T("rs")])
            P.op("act", lambda e, h=h: e.activation(out=h["rs"][:], in_=h["rs"][:], func=AF.Sqrt), reads=[T("rs")], writes=[T("rs")])
            P.op("dve", lambda e, h=h: e.reciprocal(out=h["rs"][:], in_=h["rs"][:]), reads=[T("rs")], writes=[T("rs")])
            P.op("dve", lambda e, h=h: e.tensor_tensor(out=h["rs"][:], in0=h["rs"][:], in1=banks[B_O][:, 0:NT], op=ALU.mult),
                 reads=[T("rs"), ("bank", B_O)], writes=[T("rs")])
            P.op("dve", lambda e, h=h, hd=hd: e.scalar_tensor_tensor(out=h["ob"][:], in0=h["rs"][:], scalar=hps[:, 2 + hd:3 + hd], in1=h["sgl"][:],
                                                                      op0=ALU.mult, op1=ALU.mult),
                 reads=[T("rs"), T("sgl"), "hp"], writes=[T("ob")])
            P.dma("sp", odst(hd * 128, ti * NT, NT), h["ob"][:], reads=[T("ob")])
        for hd in range(2):
            prep(hd)
        for c in range(NCH):
            for hd in range(2):
                chunk(hd, c)
        for hd in range(2):
            post(hd)
    return cx.done()


import math

NEG = -30000.0
GELU_C2 = 2.0 * 0.7978845608028654
W_C, W_S, W_W = 4224, 2176, 640
OFFD, ND = 2064, 6272
OFFW, NDW = 128, 768


def _bucket(d):
    n = np.maximum(d, 0)
    large = 16 + (np.log(np.maximum(n, 16).astype(np.float32) / np.float32(16)) / np.float32(math.log(2048 / 16))
                  * np.float32(16)).astype(np.int32)
    return np.where(n < 16, n, np.minimum(large, 31))


def nsa_static():
    d = np.arange(ND) - OFFD
    ohd = np.zeros((33, ND), np.float32)
    bk = _bucket(d)
    ohd[bk[d >= 0], np.nonzero(d >= 0)[0]] = 1.0
    ohd[32, d < 0] = 1.0
    dw = np.arange(NDW) - OFFW
    ohw = np.zeros((33, NDW), np.float32)
    okw = (dw >= 0) & (dw < 512)
    ohw[_bucket(dw)[okw], np.nonzero(okw)[0]] = 1.0
    ohw[32, ~okw] = 1.0
    n = np.arange(512)[:, None]
    s = np.arange(128)[None, :]
    ov = np.clip(np.minimum(n * 16 + 32, s * 64 + 64) - np.maximum(n * 16, s * 64), 0, None) / 32.0
    ov[511] = 0.0
    c2s = np.ascontiguousarray(ov.reshape(4, 128, 128).transpose(1, 0, 2)).astype(np.float32)
    q = np.arange(128)[:, None]
    c = np.arange(256)[None, :]
    hi = (q >= 64).astype(np.int64)
    vt = ((c - 126) <= hi - 2).astype(np.float32)
    ft = (((c - 126) == hi) | ((c - 126) == hi - 1)).astype(np.float32)
    ident = np.eye(128, dtype=np.float32)
    return dict(ohd=ohd, ohw=ohw, c2s=c2s, vt=vt, vm1=vt - 1.0, ft=ft, ident=ident, ident4=np.tile(ident, (1, 4)), aident=np.ascontiguousarray(ident[::-1]),
                lstrict=np.triu(np.ones((128, 128), np.float32), 1))


def build_nsa(S, upto=99, qbs=None, cx=None):
    cx = cx or Ctx()
    nc, P = cx.nc, cx.P
    NQB = S // 128
    NT = 512
    I = cx.inp
    SI = lambda name, shape, dt=F32: cx.inp(name, shape, dt, shared=True)
    xsrc = cx.ov.get("xsrc")
    if xsrc is None:
        xT = I("xT", [1024, S])
        xTv_ = xT.rearrange("(c p) t -> p c t", p=128)
        xsrc = lambda t0, n: (xTv_[:, :, t0:t0 + n], "pool")
    wq = I("nwq", [1024, 256]); wk = I("nwk", [1024, 256]); wt = I("nwt", [1024, 140])
    w1 = I("w1", [2, 64, 32, 128]); posT = I("posT", [2, 64, 32]); w2 = I("w2", [2, 128, 64])
    tabaug = SI("tabaug", [33, 4])
    ohd = SI("ohd", [33, ND]); ohw = SI("ohw", [33, NDW])
    c2s_d = SI("c2s", [128, 4, 128]); vt_d = SI("vt", [128, 256]); vm1_d = SI("vm1", [128, 256]); ft_d = SI("ft", [128, 256])
    ident_d = SI("ident", [128, 128]); ident4_d = SI("ident4", [128, 512]); aident_d = SI("aident", [128, 128])
    odst = cx.ov.get("odst")
    if odst is None:
        obT = cx.out("obT", [256, S], BF16)
        odst = lambda row0, t0, n: obT[row0:row0 + 128, t0:t0 + n]
    otok = cx.ov.get("otok", lambda t0: [])
    after_store = cx.ov.get("after_store")
    fD = nc.dram_tensor(cx.prefix + "fD", [4, ND], F32, kind="Internal")
    fW = nc.dram_tensor(cx.prefix + "fW", [4, NDW], F32, kind="Internal")
    banks = cx.banks
    B_OC, B_U, B_OS, B_OW, B_M = 3, 4, 5, 6, 7
    rot = [0]

    def rbank():
        i = rot[0] % 3
        rot[0] += 1
        return i
    wqb = P.sb([128, 8, 256], BF16)
    KsT = P.sb([65, S], BF16); KwT = P.sb([64, S], BF16)
    Vs = P.sb([128, NQB, 65], BF16); Vw = P.sb([128, NQB, 65], BF16)
    gsig = P.sb([128, NQB, 12], F32)
    kcT = P.sb([65, 512], BF16); vc = P.sb([128, 4, 65], BF16)
    c2s = P.sb([128, 4, 128], BF16)
    vt = P.sb([128, 256], F32); vm1 = P.sb([128, 256], F32); ft = P.sb([128, 256], F32)
    idb = P.sb([128, 128], BF16); id4 = P.sb([128, 512], BF16); jdb = P.sb([128, 128], BF16)
    tq = P.sb([65, 4], F32)
    AR = max(4 * (W_C + W_S + W_W), 2 * 8 * NT + 8 * 256 + 8 * 140 + 2 * 32 * 128 + 2 * S)
    arena = P.sb([128, AR], BF16)
    o = 0

    def carve(n):
        nonlocal o
        a = arena[:, o:o + n]
        o += n
        return a
    xb = [carve(8 * NT).rearrange("p (c t) -> p c t", t=NT) for _ in range(2)]
    wkb = carve(8 * 256).rearrange("p (c n) -> p c n", n=256)
    wtb = carve(8 * 140).rearrange("p (c n) -> p c n", n=140)
    w1b = carve(2 * 32 * 128).rearrange("p (k l j) -> p k l j", k=2, l=32)
    KcT = carve(S); VcT = carve(S)
    assert o <= AR, (o, AR)
    TcT = arena[:, 0:4 * W_C].rearrange("p (h w) -> p h w", h=4)
    TsT = arena[:, 4 * W_C:4 * (W_C + W_S)].rearrange("p (h w) -> p h w", h=4)
    TwT = arena[:, 4 * (W_C + W_S):4 * (W_C + W_S + W_W)].rearrange("p (h w) -> p h w", h=4)
    ARENA_TOK = [("xb", 0), ("xb", 1), "wk", "wt", "w1", "KcT", "VcT"]
    posb = P.sb([64, 2, 32], BF16); w2b = P.sb([128, 2, 64], BF16)
    cst = P.sb([128, 2], F32)
    g1 = P.sb([128, 512], F32); g2 = P.sb([128, 512], F32); h1g = P.sb([128, 512], BF16)
    tabs = P.sb([33, 4], F32); ohs = [P.sb([33, 512], F32) for _ in range(2)]
    fsb = P.sb([4, ND], F32); fwsb = P.sb([4, NDW], F32)
    xq = [P.sb([128, 8, 128], BF16) for _ in range(2)]
    Qb = [P.sb([65, 4, 128], BF16) for _ in range(2)]
    pT = [P.sb([128, 512], BF16) for _ in range(3)]
    rz = P.sb([128, 12], F32); coef = P.sb([128, 12], F32)
    imp = P.sb([128, 128], F32); imp2 = P.sb([128, 128], F32); m8a = P.sb([128, 8], F32); m8b = P.sb([128, 8], F32)
    selneg = P.sb([128, 128], BF16)
    selx = P.sb([128, S], BF16)
    ofin = P.sb([128, 256], F32); ofb = P.sb([128, 256], BF16); oT = [P.sb([128, 2, 128], BF16) for _ in range(2)]

    P.dma("pool", wqb[:], wq.rearrange("(c p) n -> p c n", p=128), writes=["wq"])
    P.dma("pool", wkb, wk.rearrange("(c p) n -> p c n", p=128), writes=["wk"])
    P.dma("pool", wtb, wt.rearrange("(c p) n -> p c n", p=128), writes=["wt"])
    P.dma("pool", w1b[0:64], w1.rearrange("k e l j -> e k l j"), writes=["w1"])
    P.dma("pool", posb[:], posT.rearrange("k e l -> e k l"), writes=["pos"])
    P.dma("pool", w2b[:], w2.rearrange("k j d -> j k d"), writes=["w2"])
    P.dma("pool", c2s[:], c2s_d, writes=["c2s"])
    P.dma("pool", idb[:], ident_d, writes=["idb"])
    P.dma("pool", id4[:], ident4_d, writes=["id4"])
    P.dma("pool", jdb[:], aident_d, writes=["jdb"])
    P.dma("sp", vt[:], vt_d, writes=["vt"]); P.dma("sp", vm1[:], vm1_d, writes=["vm1"]); P.dma("sp", ft[:], ft_d, writes=["ft"])
    P.dma("sp", tabs[:], tabaug, writes=["tabs"])
    P.dma("sp", tq[64:65, :], tabaug[31:32, :], writes=["tq"])
    P.op("dve", lambda e: e.memset(KsT[64:65, :], 1.0), writes=["KsT_aug"])
    P.op("dve", lambda e: e.memset(kcT[:], 0.0), writes=["kcT"])
    P.op("dve", lambda e: e.memset(kcT[64:65, :], 1.0), writes=["kcT"])
    P.op("dve", lambda e: e.memset(vc[:], 0.0), writes=["vc"])
    P.op("dve", lambda e: e.memset(vc[:, :, 64:65], 1.0), writes=["vc"])
    P.op("pool", lambda e: e.memset(Vs[:, :, 64:65], 1.0), writes=["Vs_aug"])
    P.op("pool", lambda e: e.memset(Vw[:, :, 64:65], 1.0), writes=["Vw_aug"])
    for b in range(2):
        for h in range(4):
            P.op("pool", lambda e, b=b, h=h: e.memset(Qb[b][64:65, h, :], 0.0), writes=[("Qaug", b)])
            P.op("pool", lambda e, b=b, h=h: e.tensor_scalar(out=Qb[b][64:65, h, :], in0=Qb[b][64:65, h, :], scalar1=tq[64:65, h:h + 1],
                                                             scalar2=None, op0=ALU.add), reads=["tq"], writes=[("Qaug", b)])
    for (oh_d, n_d, dst_s, dst_d, tk) in ((ohd, ND, fsb, fD, "fD"), (ohw, NDW, fwsb, fW, "fW")):
        for ci, c0 in enumerate(range(0, n_d, 512)):
            c1 = min(n_d, c0 + 512)
            ob_ = ohs[ci % 2]
            P.dma("sp", ob_[:, 0:c1 - c0], oh_d[:, c0:c1], writes=[("ohs", ci % 2)])
            P.op("pe", lambda e, ob_=ob_, n=c1 - c0: e.matmul(banks[B_M][0:4, 0:n], tabs[:], ob_[:, 0:n], start=True, stop=True),
                 reads=["tabs", ("ohs", ci % 2)], writes=[("bank", B_M)])
            P.op("act", lambda e, dst_s=dst_s, c0=c0, c1=c1: e.activation(out=dst_s[:, c0:c1], in_=banks[B_M][0:4, 0:c1 - c0], func=AF.Copy),
                 reads=[("bank", B_M)], writes=[tk + "s"])
        P.dma("sp", dst_d.ap(), dst_s[:], reads=[tk + "s"], writes=[tk])

    if upto < 1:
        return cx.done()
    def load_x(ti):
        ap_, eng_ = xsrc(ti * NT, NT)
        P.dma(eng_, xb[ti % 2], ap_, writes=[("xb", ti % 2)])
    load_x(0)
    for ti in range(S // NT):
        b = ti % 2
        ts = slice(ti * NT, (ti + 1) * NT)
        if ti + 1 < S // NT:
            load_x(ti + 1)
        for j, (dst, tk) in enumerate(((KsT, "KsT"), (KwT, "KwT"), (KcT, "KcT"), (VcT, "VcT"))):
            bi = rbank()
            mm_acc(P, banks[bi][0:64, :], [(wkb[:, kc, j * 64:(j + 1) * 64], xb[b][:, kc, :]) for kc in range(8)], ["wk", ("xb", b)], [("bank", bi)])
            eng = "act" if j % 2 == 0 else "dve"
            if eng == "act":
                P.op("act", lambda e, dst=dst, bi=bi, ts=ts: e.activation(out=dst[0:64, ts], in_=banks[bi][0:64, :], func=AF.Copy),
                     reads=[("bank", bi)], writes=[tk])
            else:
                P.op("dve", lambda e, dst=dst, bi=bi, ts=ts: e.tensor_copy(out=dst[0:64, ts], in_=banks[bi][0:64, :]),
                     reads=[("bank", bi)], writes=[tk])
        for st in range(4):
            bi = rbank()
            qi = ti * 4 + st
            mm_acc(P, banks[bi][:, 0:140], [(xb[b][:, kc, st * 128:(st + 1) * 128], wtb[:, kc, :]) for kc in range(8)], ["wt", ("xb", b)], [("bank", bi)])
            P.op("dve", lambda e, bi=bi, qi=qi: e.tensor_copy(out=Vs[:, qi, 0:64], in_=banks[bi][:, 0:64]), reads=[("bank", bi)], writes=["Vs"])
            P.op("dve", lambda e, bi=bi, qi=qi: e.tensor_copy(out=Vw[:, qi, 0:64], in_=banks[bi][:, 64:128]), reads=[("bank", bi)], writes=["Vw"])
            P.op("act", lambda e, bi=bi, qi=qi: e.activation(out=gsig[:, qi, :], in_=banks[bi][:, 128:140], func=AF.Sigmoid),
                 reads=[("bank", bi)], writes=["gsig"])
    if upto < 2:
        return cx.done()
    NCB = (S - 32) // 16 + 1
    for kv, (src, tk) in enumerate(((KcT, "KcT"), (VcT, "VcT"))):
        bi = rbank()
        mm_acc(P, banks[bi][:, 0:1], [(w1b[0:64, kv, l, :], posb[:, kv, l:l + 1]) for l in range(32)], ["w1", "pos"], [("bank", bi)])
        P.op("dve", lambda e, bi=bi, kv=kv: e.tensor_copy(out=cst[:, kv:kv + 1], in_=banks[bi][:, 0:1]), reads=[("bank", bi)], writes=["cst"])
        bi = rbank()
        srcv = src[0:64, :]
        mm_acc(P, banks[bi][:, 0:NCB],
               [(w1b[0:64, kv, l, :], src[0:64, l:l + 16 * (NCB - 1) + 1:16]) for l in range(32)], ["w1", tk], [("bank", bi)])
        P.op("dve", lambda e, bi=bi, kv=kv: e.tensor_scalar(out=g1[:, 0:NCB], in0=banks[bi][:, 0:NCB], scalar1=cst[:, kv:kv + 1], scalar2=None, op0=ALU.add),
             reads=[("bank", bi), "cst"], writes=["g1"])
        P.op("dve", lambda e: e.tensor_tensor(out=g2[:, 0:NCB], in0=g1[:, 0:NCB], in1=g1[:, 0:NCB], op=ALU.mult), reads=["g1"], writes=["g2"])
        P.op("dve", lambda e: e.tensor_scalar(out=g2[:, 0:NCB], in0=g2[:, 0:NCB], scalar1=0.044715, scalar2=1.0, op0=ALU.mult, op1=ALU.add),
             reads=["g2"], writes=["g2"])
        P.op("dve", lambda e: e.tensor_tensor(out=g2[:, 0:NCB], in0=g2[:, 0:NCB], in1=g1[:, 0:NCB], op=ALU.mult), reads=["g1", "g2"], writes=["g2"])
        P.op("act", lambda e: e.activation(out=g2[:, 0:NCB], in_=g2[:, 0:NCB], func=AF.Sigmoid, scale=GELU_C2), reads=["g2"], writes=["g2"])
        P.op("dve", lambda e: e.memset(h1g[:], 0.0), reads=["h1g"], writes=["h1g"])
        P.op("dve", lambda e: e.tensor_tensor(out=h1g[:, 0:NCB], in0=g2[:, 0:NCB], in1=g1[:, 0:NCB], op=ALU.mult), reads=["g1", "g2"], writes=["h1g"])
        if kv == 0:
            bi = rbank()
            P.op("pe", lambda e, bi=bi: e.matmul(banks[bi][0:64, 0:NCB], w2b[:, 0, :], h1g[:, 0:NCB], start=True, stop=True),
                 reads=["w2", "h1g"], writes=[("bank", bi)])
            P.op("act", lambda e, bi=bi: e.activation(out=kcT[0:64, 0:NCB], in_=banks[bi][0:64, 0:NCB], func=AF.Copy), reads=[("bank", bi)], writes=["kcT"])
        else:
            bi = rbank()
            for i in range((NCB + 127) // 128):
                P.op("pe", lambda e, bi=bi, i=i: e.matmul(banks[bi][:, i * 64:(i + 1) * 64], h1g[:, i * 128:(i + 1) * 128], w2b[:, 1, :], start=True, stop=True),
                     reads=["w2", "h1g"], writes=[("bank", bi)])
            ni = (NCB + 127) // 128
            P.op("act", lambda e, bi=bi, ni=ni: e.activation(out=vc[:, 0:ni, 0:64], in_=banks[bi][:, 0:ni * 64].rearrange("p (i d) -> p i d", d=64), func=AF.Copy),
                 reads=[("bank", bi)], writes=["vc"])
    if upto < 3:
        return cx.done()
    for h in range(4):
        P.dma("pool", TcT[:, h, :], bass.AP(fD, h * ND + OFFD - 2063, [[16, 128], [1, W_C]]), reads=["fD"], writes=ARENA_TOK + ["TcT"])
        P.dma("pool", TsT[:, h, :], bass.AP(fD, h * ND + OFFD - 127, [[1, 128], [1, W_S]]), reads=["fD"], writes=ARENA_TOK + ["TsT"])
        P.dma("pool", TwT[:, h, :], bass.AP(fW, h * NDW + OFFW - 127, [[1, 128], [1, W_W]]), reads=["fW"], writes=ARENA_TOK + ["TwT"])

    if upto < 4:
        return cx.done()
    def load_xq(qb):
        ap_, eng_ = xsrc(qb * 128, 128)
        P.dma(eng_, xq[qb % 2][:], ap_, writes=[("xq", qb % 2)])

    def score_tile(qb, Qa, lhs_full, near, table, wbase, mask_j, acc_bank, acc_tok, v_rhs, first, extra=None):
        bi = rbank()
        kk = 64 if near else 65
        nmm = 1 + (1 if near else 0) + (1 if mask_j is not None else 0)
        k = 0
        P.op("pe", lambda e: e.matmul(banks[bi], lhs_full[0:kk, :], Qa[0:kk].rearrange("p h q -> p (h q)"), start=True, stop=(nmm == 1)),
             reads=[("Q", qb % 2), ("Qaug", qb % 2), "kcT", "KsT", "KwT", "KsT_aug"], writes=[("bank", bi)])
        k += 1
        if near:
            P.op("pe", lambda e, k=k: e.matmul(banks[bi].rearrange("p (h q) -> p h q", h=4), jdb[:], table[:, :, wbase:wbase + 128], start=False, stop=(k == nmm - 1)),
                 reads=["jdb", "TcT", "TsT", "TwT"], writes=[("bank", bi)])
            k += 1
        if mask_j is not None:
            lw = selx[:, mask_j * 128:(mask_j + 1) * 128]
            P.op("pe", lambda e: e.matmul(banks[bi], lw, id4[:], start=False, stop=True),
                 reads=[("selx", mask_j // 8), "id4"], writes=[("bank", bi)])
        pt = pT[bi]
        P.op("act", lambda e: e.activation(out=pt[:], in_=banks[bi], func=AF.Exp), reads=[("bank", bi)], writes=[("pT", bi)])

        def pv():
            for h in range(4):
                P.op("pe", lambda e, h=h: e.matmul(banks[acc_bank][:, h * 65:(h + 1) * 65], pt[:, h * 128:(h + 1) * 128], v_rhs,
                                                   start=(first and h == 0), stop=False, skip_group_check=True),
                     reads=[("pT", bi), "vc", "Vs", "Vw", "Vs_aug", "Vw_aug"], writes=[("bank", acc_bank)])
                if extra is not None:
                    extra(pt, bi, h, first and h == 0)
        pend.append(pv)
        while len(pend) > 2:
            pend.pop(0)()

    pend = []
    deferred = []
    oacc = [P.sb([128, 3, 260], F32) for _ in range(2)]

    def flush():
        while pend:
            pend.pop(0)()

    qdone = {}

    def qproj(qb_):
        b_ = qb_ % 2
        for h in range(4):
            mm_acc(P, banks[B_M][0:64, h * 128:(h + 1) * 128], [(wqb[:, kc, h * 64:(h + 1) * 64], xq[b_][:, kc, :]) for kc in range(8)],
                   ["wq", ("xq", b_)], [("bank", B_M)])
        P.op("act", lambda e: e.activation(out=Qb[b_][0:64], in_=banks[B_M][0:64, :].rearrange("p (h q) -> p h q", h=4), func=AF.Copy, scale=0.125),
             reads=[("bank", B_M)], writes=[("Q", b_)])
        qdone[qb_] = True

    load_xq(0)
    for qb in range(NQB):
        b = qb % 2
        t0 = qb * 128
        if qb + 1 < NQB:
            load_xq(qb + 1)
        if qbs is not None and qb not in qbs:
            continue
        Qa = Qb[b]
        if not qdone.get(qb):
            qproj(qb)
        ni = min(3, (t0 + 96) // 2048)
        for i in range(ni + 1):
            wbase = t0 - 2048 * i
            near = wbase < W_C

            def extra(pt, bi, h, st, i=i):
                P.op("pe", lambda e: e.matmul(banks[B_U][:, h * 128:(h + 1) * 128], pt[:, h * 128:(h + 1) * 128], c2s[:, i, :],
                                              start=st, stop=False, skip_group_check=True),
                     reads=[("pT", bi), "c2s"], writes=[("bank", B_U)])
            score_tile(qb, Qa, kcT[:, i * 128:(i + 1) * 128], near, TcT, wbase if near else 0, None, B_OC, "oc", vc[:, i, :], i == 0,
                       extra if qb >= 8 else None)
        while deferred:
            deferred.pop(0)()
        flush()
        if qb >= 8:
            ocv = banks[B_OC][:, 0:260].rearrange("p (h c) -> p h c", c=65)
            P.op("dve", lambda e: e.tensor_scalar(out=rz[:, 0:4], in0=ocv[:, :, 64], scalar1=1e-30, scalar2=None, op0=ALU.max),
                 reads=[("bank", B_OC)], writes=["rzc"])
            P.op("dve", lambda e: e.reciprocal(out=rz[:, 0:4], in_=rz[:, 0:4]), reads=["rzc"], writes=["rzc"])
            for h in range(4):
                if h == 0:
                    P.op("dve", lambda e: e.tensor_scalar(out=imp[:], in0=banks[B_U][:, 0:128], scalar1=rz[:, 0:1], scalar2=None, op0=ALU.mult),
                         reads=[("bank", B_U), "rzc"], writes=["imp"])
                else:
                    P.op("dve", lambda e, h=h: e.scalar_tensor_tensor(out=imp[:], in0=banks[B_U][:, h * 128:(h + 1) * 128], scalar=rz[:, h:h + 1],
                                                                       in1=imp[:], op0=ALU.mult, op1=ALU.add),
                         reads=[("bank", B_U), "rzc", "imp"], writes=["imp"])
            c0 = 126 - 2 * qb
            P.op("dve", lambda e, c0=c0: e.tensor_tensor(out=imp[:], in0=imp[:], in1=vt[:, c0:c0 + 128], op=ALU.mult), reads=["imp", "vt"], writes=["imp"])
            P.op("dve", lambda e, c0=c0: e.tensor_tensor(out=imp[:], in0=imp[:], in1=vm1[:, c0:c0 + 128], op=ALU.add), reads=["imp", "vm1"], writes=["imp"])
            P.op("dve", lambda e: e.memset(imp[:, 0:1], -1.0), reads=["imp"], writes=["imp"])
            P.op("dve", lambda e: e.max(out=m8a[:], in_=imp[:]), reads=["imp"], writes=["m8a"])
            P.op("dve", lambda e: e.match_replace(out=imp2[:], in_to_replace=m8a[:], in_values=imp[:], imm_value=-2.0), reads=["imp", "m8a"], writes=["imp2"])
            P.op("dve", lambda e: e.max(out=m8b[:], in_=imp2[:]), reads=["imp2"], writes=["m8b"])
            P.op("dve", lambda e: e.tensor_scalar(out=imp2[:], in0=imp[:], scalar1=m8b[:, 4:5], scalar2=None, op0=ALU.is_ge), reads=["imp", "m8b"], writes=["imp2"])
            P.op("dve", lambda e, c0=c0: e.tensor_tensor(out=imp2[:], in0=imp2[:], in1=ft[:, c0:c0 + 128], op=ALU.max), reads=["imp2", "ft"], writes=["imp2"])
            P.op("dve", lambda e: e.memset(imp2[:, 0:1], 1.0), reads=["imp2"], writes=["imp2"])
            P.op("dve", lambda e: e.tensor_scalar(out=selneg[:], in0=imp2[:], scalar1=-NEG, scalar2=NEG, op0=ALU.mult, op1=ALU.add),
                 reads=["imp2"], writes=["selneg"])
            nkb = 2 * (qb + 1)
            for k0 in range(0, nkb, 16):
                k1 = min(nkb, k0 + 16)
                P.op("dve", lambda e, k0=k0, k1=k1: e.tensor_copy(out=selx[:, k0 * 64:k1 * 64].rearrange("p (k j) -> p k j", j=64),
                                                                  in_=selneg[:, k0:k1].unsqueeze(2).to_broadcast([128, k1 - k0, 64])),
                     reads=["selneg"], writes=[("selx", k0 // 16)])
        j0 = max(0, qb - 4)
        for j in range(j0, qb + 1):
            score_tile(qb, Qa, KwT[:, j * 128:(j + 1) * 128], True, TwT, 128 * (qb - j), None, B_OW, "ow", Vw[:, j, :], j == j0)
        if qb + 1 < NQB and (qbs is None or (qb + 1) in qbs):
            qproj(qb + 1)
        for j in range(qb + 1):
            near = (qb - j) <= 16
            score_tile(qb, Qa, KsT[:, j * 128:(j + 1) * 128], near, TsT, 128 * (qb - j) if near else 0, j if qb >= 8 else None,
                       B_OS, "os", Vs[:, j, :], j == 0)
        flush()
        oa_ = oacc[b]
        for x_, bk in enumerate((B_OC, B_OS, B_OW)):
            P.op("dve", lambda e, x_=x_, bk=bk, oa_=oa_: e.tensor_copy(out=oa_[:, x_, :], in_=banks[bk][:, 0:260]),
                 reads=[("bank", bk)], writes=[("oacc", b, x_)])
        oav = oa_[:].rearrange("p x (h c) -> p x h c", c=65)
        P.op("dve", lambda e, oav=oav: e.tensor_scalar(out=rz[:].rearrange("p (x h) -> p x h", x=3), in0=oav[:, :, :, 64], scalar1=1e-30, scalar2=None, op0=ALU.max),
             reads=[("oacc", b, 0), ("oacc", b, 1), ("oacc", b, 2), "rzc"], writes=["rz"])
        P.op("dve", lambda e: e.reciprocal(out=rz[:], in_=rz[:]), reads=["rz"], writes=["rz"])
        P.op("dve", lambda e, qb=qb: e.tensor_tensor(out=coef[:].rearrange("p (x h) -> p x h", x=3), in0=rz[:].rearrange("p (x h) -> p x h", x=3),
                                                      in1=gsig[:, qb, :].rearrange("p (h x) -> p x h", x=3), op=ALU.mult),
             reads=["rz", "gsig"], writes=["coef"])
        for h in range(4):
            for x_ in range(3):
                src = oa_[:, x_, h * 65:h * 65 + 64]
                dst = ofin[:, h * 64:(h + 1) * 64]
                if x_ == 0:
                    P.op("dve", lambda e, src=src, dst=dst, h=h, x_=x_: e.tensor_scalar(out=dst, in0=src, scalar1=coef[:, x_ * 4 + h:x_ * 4 + h + 1], scalar2=None, op0=ALU.mult),
                         reads=[("oacc", b, x_), "coef"], writes=[("ofin", h)])
                else:
                    P.op("dve", lambda e, src=src, dst=dst, h=h, x_=x_: e.scalar_tensor_tensor(out=dst, in0=src, scalar=coef[:, x_ * 4 + h:x_ * 4 + h + 1], in1=dst,
                                                                                             op0=ALU.mult, op1=ALU.add),
                         reads=[("oacc", b, x_), "coef", ("ofin", h)], writes=[("ofin", h)])
        P.op("act", lambda e: e.activation(out=ofb[:], in_=ofin[:], func=AF.Copy), reads=[("ofin", h_) for h_ in range(4)], writes=["ofb"])

        def finish(b=b, t0=t0):
            for f in range(2):
                P.op("pe", lambda e, f=f: e.matmul(banks[B_M][:, f * 128:(f + 1) * 128], ofb[:, f * 128:(f + 1) * 128], idb[:], start=True, stop=True),
                     reads=["ofb", "idb"], writes=[("bank", B_M)])
            P.op("act", lambda e, b=b: e.activation(out=oT[b][:], in_=banks[B_M][:, 0:256].rearrange("p (f q) -> p f q", f=2), func=AF.Copy),
                 reads=[("bank", B_M)], writes=[("oT", b)])
            for f in range(2):
                P.dma("sp", odst(f * 128, t0, 128), oT[b][:, f, :], reads=[("oT", b)], writes=otok(t0))
            if after_store is not None:
                after_store(t0 + 128)
        deferred.append(finish)
    while deferred:
        deferred.pop(0)()
    return cx.done()


D_FFE = 3584
N_EXP = 8


def build_moe(T, GT=2048, cx=None):
    cx = cx or Ctx()
    nc, P = cx.nc, cx.P
    I = cx.inp
    xT = I("xT", [1024, T])
    wr = I("wr", [1024, N_EXP])
    wg = I("mwg", [N_EXP, 1024, D_FFE]); wu = I("mwu", [N_EXP, 1024, D_FFE]); wd = I("mwd", [N_EXP, D_FFE, 1024])
    lngb = I("lngb", [2, 1024])
    ident_d = cx.inp("ident", [128, 128], shared=True)
    out = cx.out("out", [T, 1024])
    GT = min(GT, T)
    NSUB = GT // 128
    NTT = GT // 512
    NSL = D_FFE // 512
    banks = cx.banks
    idf = P.sb([128, 128], F32)
    P.dma("sp", idf[:], ident_d, writes=["idf"])
    acc = P.sb([128, NSUB, 1024], F32)
    xb = P.sb([128, 8, GT], BF16)
    xf = [P.sb([128, 8, 128], F32) for _ in range(2)]
    wrs = P.sb([128, 8, N_EXP], F32)
    wgs = [P.sb([128, 8, 512], BF16) for _ in range(2)]
    wus = [P.sb([128, 8, 512], BF16) for _ in range(2)]
    wds = [P.sb([128, 4, 1024], BF16) for _ in range(2)]
    hs = [P.sb([128, 4, 512], BF16) for _ in range(2)]
    sg = [P.sb([128, 512], F32) for _ in range(2)]
    lg = P.sb([128, N_EXP], F32); m8 = P.sb([128, 8], F32); gt = P.sb([128, 4], F32); eq = P.sb([128, N_EXP], F32)
    wts = P.sb([128, NSUB, N_EXP], F32)
    gbc = P.sb([128, 2, 1024], F32)
    st = P.sb([128, 8], F32)
    junk = P.sb([128, 1024], F32)
    P.dma("sp", wrs[:], wr.rearrange("(c p) e -> p c e", p=128), writes=["wr"])
    P.dma("sp", gbc[:, 0, :], lngb[0:1, :].partition_broadcast(128), writes=["gbc"])
    P.dma("sp", gbc[:, 1, :], lngb[1:2, :].partition_broadcast(128), writes=["gbc"])
    xTv = xT.rearrange("(c p) t -> p c t", p=128)
    pr = [0]

    def rb(lo, n):
        i = lo + pr[0] % n
        pr[0] += 1
        return i
    nload = [0]
    for g0 in range(0, T, GT):
        P.dma("pool", xb[:], xTv[:, :, g0:g0 + GT], writes=["xb"])
        for sub in range(NSUB):
            fb = sub % 2
            P.dma("sp", xf[fb][:], xTv[:, :, g0 + sub * 128:g0 + (sub + 1) * 128], writes=[("xf", fb)])
            mm_acc(P, banks[7][:, 0:N_EXP], [(xf[fb][:, kc, :], wrs[:, kc, :]) for kc in range(8)], [("xf", fb), "wr"], [("bank", 7)])
            P.op("dve", lambda e: e.tensor_copy(out=lg[:], in_=banks[7][:, 0:N_EXP]), reads=[("bank", 7)], writes=["lg"])
            P.op("dve", lambda e: e.max(out=m8[:], in_=lg[:]), reads=["lg"], writes=["m8"])
            P.op("dve", lambda e: e.tensor_tensor(out=gt[:, 0:1], in0=m8[:, 1:2], in1=m8[:, 0:1], op=ALU.subtract), reads=["m8"], writes=["gt"])
            P.op("act", lambda e: e.activation(out=gt[:, 0:1], in_=gt[:, 0:1], func=AF.Exp), reads=["gt"], writes=["gt"])
            P.op("dve", lambda e: e.tensor_scalar(out=gt[:, 0:1], in0=gt[:, 0:1], scalar1=1.0, scalar2=None, op0=ALU.add), reads=["gt"], writes=["gt"])
            P.op("dve", lambda e: e.reciprocal(out=gt[:, 1:2], in_=gt[:, 0:1]), reads=["gt"], writes=["gt"])
            P.op("dve", lambda e: e.tensor_scalar(out=gt[:, 2:3], in0=gt[:, 1:2], scalar1=-1.0, scalar2=1.0, op0=ALU.mult, op1=ALU.add),
                 reads=["gt"], writes=["gt"])
            P.op("dve", lambda e: e.tensor_scalar(out=eq[:], in0=lg[:], scalar1=m8[:, 0:1], scalar2=gt[:, 1:2], op0=ALU.is_equal, op1=ALU.mult),
                 reads=["lg", "m8", "gt"], writes=["eq"])
            P.op("dve", lambda e, sub=sub: e.tensor_scalar(out=wts[:, sub, :], in0=lg[:], scalar1=m8[:, 1:2], scalar2=gt[:, 2:3], op0=ALU.is_equal, op1=ALU.mult),
                 reads=["lg", "m8", "gt"], writes=["wts"])
            P.op("dve", lambda e, sub=sub: e.tensor_tensor(out=wts[:, sub, :], in0=wts[:, sub, :], in1=eq[:], op=ALU.add), reads=["wts", "eq"], writes=["wts"])
            for half in range(2):
                bt = 4 + half
                for c4 in range(4):
                    c = half * 4 + c4
                    P.op("pe", lambda e, fb=fb, c=c, c4=c4, bt=bt: e.matmul(banks[bt][:, c4 * 128:(c4 + 1) * 128], xf[fb][:, c, :], idf[:], start=True, stop=True),
                         reads=[("xf", fb), "idf"], writes=[("bank", bt)])
                P.op("act", lambda e, sub=sub, half=half, bt=bt: e.activation(out=acc[:, sub, half * 512:(half + 1) * 512], in_=banks[bt], func=AF.Copy,
                                                                              scale=float(DN_ALPHA)),
                     reads=[("bank", bt)], writes=[("acc", sub)])
        for ex in range(N_EXP):
            for s in range(NSL):
                wbuf = nload[0] % 2
                nload[0] += 1
                fs = slice(s * 512, (s + 1) * 512)
                P.dma("pool", wgs[wbuf][:], wg[ex, :, fs].rearrange("(c p) n -> p c n", p=128), writes=[("wgs", wbuf)])
                P.dma("pool", wus[wbuf][:], wu[ex, :, fs].rearrange("(c p) n -> p c n", p=128), writes=[("wus", wbuf)])
                P.dma("pool", wds[wbuf][:], wd[ex, fs, :].rearrange("(c p) n -> p c n", p=128), writes=[("wds", wbuf)])
                for tt in range(NTT):
                    hb = tt % 2
                    tsl = slice(tt * 512, (tt + 1) * 512)
                    for m in range(4):
                        ms = slice(m * 128, (m + 1) * 128)
                        bg = rb(0, 2); bu = 2 + (bg % 2)
                        mm_acc(P, banks[bg], [(wgs[wbuf][:, kc, ms], xb[:, kc, tsl]) for kc in range(8)], [("wgs", wbuf), "xb"], [("bank", bg)])
                        mm_acc(P, banks[bu], [(wus[wbuf][:, kc, ms], xb[:, kc, tsl]) for kc in range(8)], [("wus", wbuf), "xb"], [("bank", bu)])
                        P.op("act", lambda e, bg=bg: e.activation(out=sg[bg][:], in_=banks[bg], func=AF.Silu), reads=[("bank", bg)], writes=[("sg", bg)])
                        P.op("dve", lambda e, bg=bg, bu=bu, hb=hb, m=m: e.tensor_tensor(out=hs[hb][:, m, :], in0=sg[bg][:], in1=banks[bu], op=ALU.mult),
                             reads=[("sg", bg), ("bank", bu)], writes=[("hs", hb)])
                    for sub4 in range(4):
                        sub = tt * 4 + sub4
                        for half in range(2):
                            bd = 4 + (sub4 * 2 + half) % 3
                            mm_acc(P, banks[bd], [(hs[hb][:, m, sub4 * 128:(sub4 + 1) * 128], wds[wbuf][:, m, half * 512:(half + 1) * 512]) for m in range(4)],
                                   [("hs", hb), ("wds", wbuf)], [("bank", bd)])
                            P.op("dve", lambda e, bd=bd, sub=sub, half=half, ex=ex: e.scalar_tensor_tensor(
                                out=acc[:, sub, half * 512:(half + 1) * 512], in0=banks[bd], scalar=wts[:, sub, ex:ex + 1],
                                in1=acc[:, sub, half * 512:(half + 1) * 512], op0=ALU.mult, op1=ALU.add),
                                reads=[("bank", bd), "wts", ("acc", sub)], writes=[("acc", sub)])
        for sub in range(NSUB):
            a = acc[:, sub, :]
            P.op("act", lambda e, a=a: e.activation(out=junk[:], in_=a, func=AF.Copy, accum_out=st[:, 0:1]), reads=[("acc", sub)], writes=["junk", "st"])
            P.op("act", lambda e, a=a: e.activation(out=junk[:], in_=a, func=AF.Square, accum_out=st[:, 1:2]), reads=[("acc", sub)], writes=["junk", "st"])
            P.op("dve", lambda e: e.tensor_scalar(out=st[:, 0:2], in0=st[:, 0:2], scalar1=1.0 / 1024, scalar2=None, op0=ALU.mult), reads=["st"], writes=["st"])
            P.op("dve", lambda e: e.tensor_tensor(out=st[:, 2:3], in0=st[:, 0:1], in1=st[:, 0:1], op=ALU.mult), reads=["st"], writes=["st"])
            P.op("dve", lambda e: e.tensor_tensor(out=st[:, 2:3], in0=st[:, 1:2], in1=st[:, 2:3], op=ALU.subtract), reads=["st"], writes=["st"])
            P.op("dve", lambda e: e.tensor_scalar(out=st[:, 2:3], in0=st[:, 2:3], scalar1=LN_EPS, scalar2=None, op0=ALU.add), reads=["st"], writes=["st"])
            P.op("act", lambda e: e.activation(out=st[:, 2:3], in_=st[:, 2:3], func=AF.Sqrt), reads=["st"], writes=["st"])
            P.op("dve", lambda e: e.reciprocal(out=st[:, 2:3], in_=st[:, 2:3]), reads=["st"], writes=["st"])
            P.op("dve", lambda e, a=a: e.tensor_scalar(out=a, in0=a, scalar1=st[:, 0:1], scalar2=st[:, 2:3], op0=ALU.subtract, op1=ALU.mult),
                 reads=["st", ("acc", sub)], writes=[("acc", sub)])
            P.op("dve", lambda e, a=a: e.tensor_tensor(out=a, in0=a, in1=gbc[:, 0, :], op=ALU.mult), reads=["gbc", ("acc", sub)], writes=[("acc", sub)])
            P.op("dve", lambda e, a=a: e.tensor_tensor(out=a, in0=a, in1=gbc[:, 1, :], op=ALU.add), reads=["gbc", ("acc", sub)], writes=[("acc", sub)])
        P.dma("sp", out[g0:g0 + GT, :].rearrange("(s p) d -> p s d", p=128), acc[:], reads=[("acc", s) for s in range(NSUB)])
    return cx.done()


MOE_CAP = 1408
MOE_SL = 256
MOE_BIG = 1.0e6
MOE_DEBUG = False


def build_moe2(T, cx=None, CAP=None):
    cx = cx or Ctx()
    nc, P = cx.nc, cx.P
    I = cx.inp
    CAP = CAP or min(MOE_CAP, ((T * 2) // N_EXP * 5 // 4 + 127) // 128 * 128 + 128)
    xT = I("xT", [1024, T])
    wr = I("wr", [1024, N_EXP])
    wg = I("mwg", [N_EXP, 1024, D_FFE]); wu = I("mwu", [N_EXP, 1024, D_FFE]); wd = I("mwd", [N_EXP, D_FFE, 1024])
    lngb = I("lngb", [2, 1024])
    ident_d = cx.inp("ident", [128, 128], shared=True)
    lstr_d = cx.inp("lstrict", [128, 128], shared=True)
    out = cx.out("out", [T, 1024])
    NSA_ = T // 128
    SL = MOE_SL
    NSL = D_FFE // SL
    MT = D_FFE // 128
    NROW = N_EXP * CAP
    xe = nc.dram_tensor(cx.prefix + "xe", [NROW, 1024], BF16)
    ye = nc.dram_tensor(cx.prefix + "ye", [NROW, 1024], F32)
    banks = cx.banks
    xTv = xT.rearrange("(c p) t -> p c t", p=128)
    base = (nc.sbuf_base, nc.sbuf_top)
    idf = P.sb([128, 128], F32); idb = P.sb([128, 128], BF16); lstr = P.sb([128, 128], BF16); onesb = P.sb([128, 128], BF16)
    wrs = P.sb([128, 8, N_EXP], F32)
    E1 = P.sb([128, NSA_, N_EXP], F32); E2 = P.sb([128, NSA_, N_EXP], F32); G = P.sb([128, NSA_, 2], F32)
    S1i = P.sb([128, NSA_], I32); S2i = P.sb([128, NSA_], I32)
    P.dma("sp", idf[:], ident_d, writes=["idf"])
    P.dma("pool", idb[:], ident_d, writes=["idb"])
    P.dma("pool", lstr[:], lstr_d, writes=["lstr"])
    P.op("dve", lambda e: e.memset(onesb[:], 1.0), writes=["onesb"])
    P.dma("sp", wrs[:], wr.rearrange("(c p) e -> p c e", p=128), writes=["wr"])
    base2 = (nc.sbuf_base, nc.sbuf_top)
    breg = {}

    def mk_breg(e):
        breg["r"] = e.alloc_register("moe_bound")
        return e.reg_mov(breg["r"], NROW - 1)
    P.op("pool", mk_breg)
    xf = [P.sb([128, 8, 128], F32) for _ in range(2)]
    xtmb = P.sb([128, NSA_, 1024], BF16)
    lg = P.sb([128, N_EXP], F32); m8 = P.sb([128, 8], F32); gt = P.sb([128, 4], F32)
    zt = P.sb([128, CAP // 128, 1024], BF16)
    P.op("pool", lambda e: e.memset(zt[:], 0.0), writes=["zt"])
    for ex in range(N_EXP):
        P.dma("sp", xe.ap()[ex * CAP:(ex + 1) * CAP, :].rearrange("(n p) d -> p n d", p=128), zt[:], reads=["zt"], writes=["xe_zero"])
    for sub in range(NSA_):
        fb = sub % 2
        P.dma("sp", xf[fb][:], xTv[:, :, sub * 128:(sub + 1) * 128], writes=[("xf", fb)])
        mm_acc(P, banks[7][:, 0:N_EXP], [(xf[fb][:, kc, :], wrs[:, kc, :]) for kc in range(8)], [("xf", fb), "wr"], [("bank", 7)])
        P.op("dve", lambda e: e.tensor_copy(out=lg[:], in_=banks[7][:, 0:N_EXP]), reads=[("bank", 7)], writes=["lg"])
        P.op("dve", lambda e: e.max(out=m8[:], in_=lg[:]), reads=["lg"], writes=["m8"])
        P.op("dve", lambda e: e.tensor_tensor(out=gt[:, 0:1], in0=m8[:, 1:2], in1=m8[:, 0:1], op=ALU.subtract), reads=["m8"], writes=["gt"])
        P.op("act", lambda e: e.activation(out=gt[:, 0:1], in_=gt[:, 0:1], func=AF.Exp), reads=["gt"], writes=["gt"])
        P.op("dve", lambda e: e.tensor_scalar(out=gt[:, 0:1], in0=gt[:, 0:1], scalar1=1.0, scalar2=None, op0=ALU.add), reads=["gt"], writes=["gt"])
        P.op("dve", lambda e, sub=sub: e.reciprocal(out=G[:, sub, 0:1], in_=gt[:, 0:1]), reads=["gt"], writes=["G"])
        P.op("dve", lambda e, sub=sub: e.tensor_scalar(out=G[:, sub, 1:2], in0=G[:, sub, 0:1], scalar1=-1.0, scalar2=1.0, op0=ALU.mult, op1=ALU.add),
             reads=["G"], writes=["G"])
        P.op("dve", lambda e, sub=sub: e.tensor_scalar(out=E1[:, sub, :], in0=lg[:], scalar1=m8[:, 0:1], scalar2=None, op0=ALU.is_equal),
             reads=["lg", "m8"], writes=["E1"])
        P.op("dve", lambda e, sub=sub: e.tensor_scalar(out=E2[:, sub, :], in0=lg[:], scalar1=m8[:, 1:2], scalar2=None, op0=ALU.is_equal),
             reads=["lg", "m8"], writes=["E2"])
        for half in range(2):
            bt = 4 + half
            for c4 in range(4):
                c = half * 4 + c4
                P.op("pe", lambda e, fb=fb, c=c, c4=c4, bt=bt: e.matmul(banks[bt][:, c4 * 128:(c4 + 1) * 128], xf[fb][:, c, :], idf[:], start=True, stop=True),
                     reads=[("xf", fb), "idf"], writes=[("bank", bt)])
            P.op("act", lambda e, sub=sub, half=half, bt=bt: e.activation(out=xtmb[:, sub, half * 512:(half + 1) * 512], in_=banks[bt], func=AF.Copy),
                 reads=[("bank", bt)], writes=[("xtmb", sub)])
    Mb = P.sb([128, NSA_ * N_EXP], BF16)
    Mf = P.sb([128, NSA_, N_EXP], F32); tots = P.sb([128, NSA_, N_EXP], F32); incl = P.sb([128, NSA_, N_EXP], F32)
    pos = P.sb([128, NSA_, N_EXP], F32); tmp = P.sb([128, NSA_, N_EXP], F32); onesf = P.sb([128, NSA_], F32)
    s12 = P.sb([128, 2, NSA_], F32)
    NE = NSA_ * N_EXP
    P.op("dve", lambda e: e.tensor_tensor(out=Mf[:], in0=E1[:], in1=E2[:], op=ALU.add), reads=["E1", "E2"], writes=["Mf"])
    P.op("dve", lambda e: e.tensor_copy(out=Mb[:], in_=Mf[:].rearrange("p s e -> p (s e)")), reads=["Mf"], writes=["Mb"])
    P.op("dve", lambda e: e.memset(onesf[:], 1.0), writes=["onesf"])
    P.op("pe", lambda e: e.matmul(banks[0][:, 0:NE], lstr[:], Mb[:], start=True, stop=True), reads=["lstr", "Mb"], writes=[("bank", 0)])
    P.op("pe", lambda e: e.matmul(banks[1][:, 0:NE], onesb[:], Mb[:], start=True, stop=True), reads=["onesb", "Mb"], writes=[("bank", 1)])
    P.op("dve", lambda e: e.tensor_copy(out=tots[:].rearrange("p s e -> p (s e)"), in_=banks[1][:, 0:NE]), reads=[("bank", 1)], writes=["tots"])
    if MOE_DEBUG:
        dbg = nc.dram_tensor("dbg_tots", [128, NSA_ * N_EXP], F32, kind="ExternalOutput").ap()
        P.dma("sp", dbg, tots[:].rearrange("p s e -> p (s e)"), reads=["tots"])
    for ex in range(N_EXP):
        P.op("dve", lambda e, ex=ex: e.tensor_tensor_scan(out=incl[:, :, ex], data0=onesf[:], data1=tots[:, :, ex], initial=0.0, op0=ALU.mult, op1=ALU.add),
             reads=["tots", "onesf"], writes=["incl"])
    P.op("dve", lambda e: e.tensor_tensor(out=pos[:].rearrange("p s e -> p (s e)"), in0=banks[0][:, 0:NE], in1=incl[:].rearrange("p s e -> p (s e)"), op=ALU.add),
         reads=[("bank", 0), "incl"], writes=["pos"])
    P.op("dve", lambda e: e.tensor_tensor(out=pos[:], in0=pos[:], in1=tots[:], op=ALU.subtract), reads=["pos", "tots"], writes=["pos"])
    P.op("dve", lambda e: e.tensor_scalar(out=tmp[:], in0=pos[:], scalar1=float(CAP), scalar2=MOE_BIG, op0=ALU.is_ge, op1=ALU.mult), reads=["pos"], writes=["tmp"])
    P.op("dve", lambda e: e.tensor_tensor(out=pos[:], in0=pos[:], in1=tmp[:], op=ALU.add), reads=["pos", "tmp"], writes=["pos"])
    for ex in range(1, N_EXP):
        P.op("dve", lambda e, ex=ex: e.tensor_scalar(out=pos[:, :, ex], in0=pos[:, :, ex], scalar1=float(ex * CAP), scalar2=None, op0=ALU.add),
             reads=["pos"], writes=["pos"])
    for k_, EE in enumerate((E1, E2)):
        P.op("dve", lambda e, EE=EE: e.tensor_tensor(out=tmp[:], in0=pos[:], in1=EE[:], op=ALU.mult), reads=["pos", "E1", "E2", "s12"], writes=["tmp"])
        P.op("dve", lambda e, k_=k_: e.reduce_sum(out=s12[:, k_, :], in_=tmp[:], axis=AX.X), reads=["tmp"], writes=["s12"])
    P.op("dve", lambda e: e.tensor_copy(out=S1i[:], in_=s12[:, 0, :]), reads=["s12"], writes=["S1i"])
    P.op("dve", lambda e: e.tensor_copy(out=S2i[:], in_=s12[:, 1, :]), reads=["s12"], writes=["S2i"])
    for sub in range(NSA_):
        for Si, tk in ((S1i, "S1i"), (S2i, "S2i")):
            P.op("pool", lambda e, sub=sub, Si=Si: e.indirect_dma_start(
                out=xe.ap(), out_offset=bass.IndirectOffsetOnAxis(ap=Si[:, sub:sub + 1], axis=0), in_=xtmb[:, sub, :], in_offset=None,
                bounds_check=breg["r"], oob_is_err=False), reads=[tk, ("xtmb", sub), "xe_zero"], dma=True)
    P.barrier()
    nc.sbuf_base, nc.sbuf_top = base2
    NST = CAP // 128
    tiles = []
    t_ = 0
    while t_ < CAP:
        n_ = min(512, CAP - t_)
        tiles.append((t_, n_))
        t_ += n_
    xrow = [P.sb([128, 1024], BF16) for _ in range(2)]
    xeT = P.sb([128, 8, CAP], BF16)
    hT = P.sb([128, MT, CAP], BF16)
    wgs = [P.sb([128, 8, SL], BF16) for _ in range(2)]
    wus = [P.sb([128, 8, SL], BF16) for _ in range(2)]
    wds = P.sb([128, MT, 1024], BF16)
    sg = [P.sb([128, 512], F32) for _ in range(2)]
    yb = [P.sb([128, 1024], F32) for _ in range(2)]
    nl = 0
    nrow = 0
    for ex in range(N_EXP):
        P.dma("pool", wds[:], wd[ex].rearrange("(c p) n -> p c n", p=128), writes=["wds"])
        for stl in range(NST):
            rb_ = nrow % 2
            nrow += 1
            r0 = ex * CAP + stl * 128
            P.dma("sp", xrow[rb_][:], xe.ap()[r0:r0 + 128, :], writes=[("xrow", rb_)])
            for half in range(2):
                bt = 4 + half
                for c4 in range(4):
                    c = half * 4 + c4
                    P.op("pe", lambda e, rb_=rb_, c=c, c4=c4, bt=bt: e.matmul(banks[bt][:, c4 * 128:(c4 + 1) * 128], xrow[rb_][:, c * 128:(c + 1) * 128], idb[:],
                                                                             start=True, stop=True),
                         reads=[("xrow", rb_), "idb"], writes=[("bank", bt)])
                P.op("act" if half == 0 else "dve",
                     (lambda e, half=half, stl=stl, bt=bt: e.activation(out=xeT[:, half * 4:(half + 1) * 4, stl * 128:(stl + 1) * 128],
                                                                         in_=banks[bt].rearrange("p (c t) -> p c t", c=4), func=AF.Copy)) if half == 0 else
                     (lambda e, half=half, stl=stl, bt=bt: e.tensor_copy(out=xeT[:, half * 4:(half + 1) * 4, stl * 128:(stl + 1) * 128],
                                                                          in_=banks[bt].rearrange("p (c t) -> p c t", c=4))),
                     reads=[("bank", bt)], writes=["xeT"])
        for s_ in range(NSL):
            wb_ = nl % 2
            nl += 1
            fs = slice(s_ * SL, (s_ + 1) * SL)
            P.dma("pool", wgs[wb_][:], wg[ex, :, fs].rearrange("(c p) n -> p c n", p=128), writes=[("wgs", wb_)])
            P.dma("pool", wus[wb_][:], wu[ex, :, fs].rearrange("(c p) n -> p c n", p=128), writes=[("wus", wb_)])
            for (t0_, n_) in tiles:
                for m in range(SL // 128):
                    ms = slice(m * 128, (m + 1) * 128)
                    pb = m % 2
                    bg, bu = pb, 2 + pb
                    mm_acc(P, banks[bg][:, 0:n_], [(wgs[wb_][:, kc, ms], xeT[:, kc, t0_:t0_ + n_]) for kc in range(8)], [("wgs", wb_), "xeT"], [("bank", bg)])
                    mm_acc(P, banks[bu][:, 0:n_], [(wus[wb_][:, kc, ms], xeT[:, kc, t0_:t0_ + n_]) for kc in range(8)], [("wus", wb_), "xeT"], [("bank", bu)])
                    P.op("act", lambda e, bg=bg, n_=n_: e.activation(out=sg[bg][:, 0:n_], in_=banks[bg][:, 0:n_], func=AF.Silu), reads=[("bank", bg)], writes=[("sg", bg)])
                    P.op("dve", lambda e, bg=bg, bu=bu, n_=n_, t0_=t0_, mm_=s_ * (SL // 128) + m: e.tensor_tensor(out=hT[:, mm_, t0_:t0_ + n_], in0=sg[bg][:, 0:n_], in1=banks[bu][:, 0:n_], op=ALU.mult),
                         reads=[("sg", bg), ("bank", bu)], writes=[("hT", s_ * (SL // 128) + m)])
        for stl in range(NST):
            yb_ = yb[stl % 2]
            for half in range(2):
                bd = 4 + (stl * 2 + half) % 3
                mm_acc(P, banks[bd], [(hT[:, m, stl * 128:(stl + 1) * 128], wds[:, m, half * 512:(half + 1) * 512]) for m in range(MT)],
                       [[("hT", m), "wds"] for m in range(MT)], [("bank", bd)])
                if half == 0:
                    P.op("act", lambda e, bd=bd, yb_=yb_: e.activation(out=yb_[:, 0:512], in_=banks[bd], func=AF.Copy), reads=[("bank", bd)], writes=[("yb", stl % 2)])
                else:
                    P.op("dve", lambda e, bd=bd, yb_=yb_: e.tensor_copy(out=yb_[:, 512:1024], in_=banks[bd]), reads=[("bank", bd)], writes=[("yb", stl % 2)])
            r0 = ex * CAP + stl * 128
            P.dma("sp", ye.ap()[r0:r0 + 128, :], yb_[:], reads=[("yb", stl % 2)])
    P.barrier()
    nc.sbuf_base, nc.sbuf_top = base2
    xf = [P.sb([128, 8, 128], F32) for _ in range(2)]
    y1 = [P.sb([128, 1024], F32) for _ in range(2)]
    y2 = [P.sb([128, 1024], F32) for _ in range(2)]
    accs = [P.sb([128, 1024], F32) for _ in range(2)]
    gbc = P.sb([128, 2, 1024], F32)
    P.dma("sp", gbc[:, 0, :], lngb[0:1, :].partition_broadcast(128), writes=["gbc"])
    P.dma("sp", gbc[:, 1, :], lngb[1:2, :].partition_broadcast(128), writes=["gbc"])
    sts = [P.sb([128, 8], F32) for _ in range(2)]
    junks = [P.sb([128, 1024], F32) for _ in range(2)]
    for sub in range(NSA_):
        fb = sub % 2
        a = accs[fb]
        st = sts[fb]
        junk = junks[fb]
        P.dma("sp", xf[fb][:], xTv[:, :, sub * 128:(sub + 1) * 128], writes=[("xf", fb)])
        for yy, Si, tk in ((y1, S1i, "y1"), (y2, S2i, "y2")):
            P.op("pool", lambda e, yy=yy, fb=fb: e.memset(yy[fb][:], 0.0), writes=[(tk, fb)])
            P.op("pool", lambda e, yy=yy, fb=fb, Si=Si, sub=sub: e.indirect_dma_start(
                out=yy[fb][:], out_offset=None, in_=ye.ap(), in_offset=bass.IndirectOffsetOnAxis(ap=Si[:, sub:sub + 1], axis=0),
                bounds_check=breg["r"], oob_is_err=False), reads=["S1i", "S2i"], writes=[(tk, fb)], dma=True)
        for half in range(2):
            bt = 4 + half
            for c4 in range(4):
                c = half * 4 + c4
                P.op("pe", lambda e, fb=fb, c=c, c4=c4, bt=bt: e.matmul(banks[bt][:, c4 * 128:(c4 + 1) * 128], xf[fb][:, c, :], idf[:], start=True, stop=True),
                     reads=[("xf", fb), "idf"], writes=[("bank", bt)])
            P.op("act", lambda e, a=a, half=half, bt=bt: e.activation(out=a[:, half * 512:(half + 1) * 512], in_=banks[bt], func=AF.Copy, scale=float(DN_ALPHA)),
                 reads=[("bank", bt)], writes=[("acc", fb)])
        P.op("dve", lambda e, a=a, fb=fb, sub=sub: e.scalar_tensor_tensor(out=a[:], in0=y1[fb][:], scalar=G[:, sub, 0:1], in1=a[:], op0=ALU.mult, op1=ALU.add),
             reads=[("y1", fb), "G", ("acc", fb)], writes=[("acc", fb)])
        P.op("dve", lambda e, a=a, fb=fb, sub=sub: e.scalar_tensor_tensor(out=a[:], in0=y2[fb][:], scalar=G[:, sub, 1:2], in1=a[:], op0=ALU.mult, op1=ALU.add),
             reads=[("y2", fb), "G", ("acc", fb)], writes=[("acc", fb)])
        T_ = ("acc", fb)
        P.op("act", lambda e, a=a, st=st, junk=junk: e.activation(out=junk[:], in_=a[:], func=AF.Copy, accum_out=st[:, 0:1]), reads=[T_], writes=[("junk", fb), ("st", fb)])
        P.op("act", lambda e, a=a, st=st, junk=junk: e.activation(out=junk[:], in_=a[:], func=AF.Square, accum_out=st[:, 1:2]), reads=[T_], writes=[("junk", fb), ("st", fb)])
        P.op("dve", lambda e, st=st: e.tensor_scalar(out=st[:, 0:2], in0=st[:, 0:2], scalar1=1.0 / 1024, scalar2=None, op0=ALU.mult), reads=[("st", fb)], writes=[("st", fb)])
        P.op("dve", lambda e, st=st: e.tensor_tensor(out=st[:, 2:3], in0=st[:, 0:1], in1=st[:, 0:1], op=ALU.mult), reads=[("st", fb)], writes=[("st", fb)])
        P.op("dve", lambda e, st=st: e.tensor_tensor(out=st[:, 2:3], in0=st[:, 1:2], in1=st[:, 2:3], op=ALU.subtract), reads=[("st", fb)], writes=[("st", fb)])
        P.op("dve", lambda e, st=st: e.tensor_scalar(out=st[:, 2:3], in0=st[:, 2:3], scalar1=LN_EPS, scalar2=None, op0=ALU.add), reads=[("st", fb)], writes=[("st", fb)])
        P.op("act", lambda e, st=st: e.activation(out=st[:, 2:3], in_=st[:, 2:3], func=AF.Sqrt), reads=[("st", fb)], writes=[("st", fb)])
        P.op("dve", lambda e, st=st: e.reciprocal(out=st[:, 2:3], in_=st[:, 2:3]), reads=[("st", fb)], writes=[("st", fb)])
        P.op("dve", lambda e, a=a, st=st, junk=junk: e.tensor_scalar(out=a[:], in0=a[:], scalar1=st[:, 0:1], scalar2=st[:, 2:3], op0=ALU.subtract, op1=ALU.mult), reads=[("st", fb), T_], writes=[T_])
        P.op("dve", lambda e, a=a, st=st, junk=junk: e.tensor_tensor(out=a[:], in0=a[:], in1=gbc[:, 0, :], op=ALU.mult), reads=["gbc", T_], writes=[T_])
        P.op("dve", lambda e, a=a, st=st, junk=junk: e.tensor_tensor(out=a[:], in0=a[:], in1=gbc[:, 1, :], op=ALU.add), reads=["gbc", T_], writes=[T_])
        P.dma("sp", out[sub * 128:(sub + 1) * 128, :], a[:], reads=[T_])
    if not cx.standalone:
        nc.sbuf_base, nc.sbuf_top = base
    return cx.done()


from concourse.bass_utils import run_bass_kernel_spmd

S_FULL = 8192
_PROGS = {}


def _prog(key, fn):
    if key not in _PROGS:
        _PROGS[key] = fn()
    return _PROGS[key]


def _c(a):
    return np.ascontiguousarray(a, dtype=np.float32)


def _pc(v):
    return _c(np.asarray(v).reshape(8, 128).T)


def _run(nc, in_maps):
    res = run_bass_kernel_spmd(nc, in_maps, core_ids=list(range(8)))
    return res.results


def kernel_unfused(x, w_in, hg_lb_logits, hg_norm_w, cmp_pos, cmp_w1, cmp_w2, rel_bias, w_branch_a, w_branch_b,
           w_out, ln1_g, ln1_b, ln2_g, ln2_b, ffn_w_gate, ffn_w_up, ffn_w_down, moe_router, moe_w_gate,
           moe_w_up, moe_w_down):
    f = lambda a: np.asarray(a, dtype=np.float32)
    x = f(x); w_in = f(w_in); hg_lb_logits = f(hg_lb_logits); hg_norm_w = f(hg_norm_w)
    cmp_pos = f(cmp_pos); cmp_w1 = f(cmp_w1); cmp_w2 = f(cmp_w2); rel_bias = f(rel_bias)
    B, S, D = x.shape
    NT = 512
    cmask = np.ones((128, NT), np.float32); cmask[:, ::64] = 0
    tri = np.zeros((64, NT), np.float32)
    for c in range(NT // 64):
        tri[:, c * 64:(c + 1) * 64] = np.triu(np.ones((64, 64), np.float32))
    ident = np.eye(128, dtype=np.float32)
    st = nsa_static()
    xT_b = [_c(x[b].T) for b in range(B)]
    out = None
    for l in range(DEPTH):
        W = w_in[l]
        nc_h = _prog(("hgrn", l), lambda: build_hgrn(S, NT, layer=l))
        maps = []
        for c in range(8):
            b, j = c // 2, c % 2
            cs = slice(j * 256, (j + 1) * 256)
            lbl = np.stack([hg_lb_logits[0, cs][:128], hg_lb_logits[0, cs][128:], hg_lb_logits[1, cs][:128], hg_lb_logits[1, cs][128:]], axis=1)
            nw = hg_norm_w[l, cs]
            hp = np.stack([np.ones(128, np.float32), np.ones(128, np.float32), nw[:128], nw[128:]], axis=1)
            maps.append(dict(xT=xT_b[b], hwq=_c(W[:, 0:512][:, cs]), hwf=_c(W[:, 512:1024][:, cs]), hwi=_c(W[:, 1024:1536][:, cs]),
                             hwg=_c(W[:, 1536:2048][:, cs]), hp=_c(hp), lbl=_c(lbl), cmask=cmask, tri=tri, ident=ident))
        r_h = _run(nc_h, maps)
        nc_n = _prog(("nsa",), lambda: build_nsa(S))
        maps = []
        for c in range(8):
            b, g = c // 2, c % 2
            kv = lambda base: W[:, base + g * 64: base + (g + 1) * 64]
            d = dict(xT=xT_b[b], nwq=_c(W[:, 2048 + g * 256:2048 + (g + 1) * 256]),
                     nwk=_c(np.concatenate([kv(2816), kv(3072), kv(2560), kv(2688)], 1)),
                     nwt=_c(np.concatenate([kv(2944), kv(3200), W[:, 3328 + g * 12:3328 + (g + 1) * 12]], 1)),
                     w1=_c(cmp_w1[l].reshape(2, 32, 64, 128).transpose(0, 2, 1, 3)), posT=_c(cmp_pos[l].transpose(0, 2, 1)), w2=_c(cmp_w2[l]),
                     tabaug=_c(np.concatenate([rel_bias[:, g * 4:(g + 1) * 4], np.full((1, 4), NEG, np.float32)], 0)))
            d.update(st)
            maps.append(d)
        r_n = _run(nc_n, maps)
        T = S // 2
        nc_m = _prog(("merge",), lambda: build_merge(T))
        lnp1 = _c(np.concatenate([_pc(ln1_g[l]), _pc(ln1_b[l])], axis=1))
        maps = []
        for c in range(8):
            b, hf = c // 2, c % 2
            ts = slice(hf * T, (hf + 1) * T)
            oaT = np.ascontiguousarray(np.concatenate([r_h[2 * b]["oaT"][:, ts], r_h[2 * b + 1]["oaT"][:, ts]], axis=0))
            obT = np.ascontiguousarray(np.concatenate([r_n[2 * b]["obT"][:, ts], r_n[2 * b + 1]["obT"][:, ts]], axis=0))
            maps.append(dict(xT=_c(xT_b[b][:, ts]), oaT=oaT, obT=obT, wgt=_c(W[:, 3352:5400]), wa=_c(f(w_branch_a)[l]), wb=_c(f(w_branch_b)[l]),
                             wo=_c(f(w_out)[l]), lnp=lnp1))
        r_m = _run(nc_m, maps)
        lnp2 = _c(np.concatenate([_pc(ln2_g[l]), _pc(ln2_b[l])], axis=1))
        if l % 2 == 0:
            nc_f = _prog(("ffn",), lambda: build_ffn(T))
            maps = [dict(xT=r_m[c]["outT"], wg=_c(f(ffn_w_gate)[l // 2]), wu=_c(f(ffn_w_up)[l // 2]), wd=_c(f(ffn_w_down)[l // 2]), lnp=lnp2)
                    for c in range(8)]
            r_f = _run(nc_f, maps)
            xT_b = [np.ascontiguousarray(np.concatenate([r_f[2 * b]["outT"], r_f[2 * b + 1]["outT"]], axis=1)) for b in range(B)]
            if l == DEPTH - 1:
                out = np.stack([xT_b[b].T for b in range(B)])
        else:
            nc_f = _prog(("moe",), lambda: build_moe(T))
            wgm, wum, wdm = _c(f(moe_w_gate)[l // 2]), _c(f(moe_w_up)[l // 2]), _c(f(moe_w_down)[l // 2])
            maps = [dict(xT=r_m[c]["outT"], wr=_c(f(moe_router)[l // 2]), mwg=wgm, mwu=wum, mwd=wdm, ident=ident,
                         lngb=_c(np.stack([f(ln2_g)[l], f(ln2_b)[l]]))) for c in range(8)]
            r_f = _run(nc_f, maps)
            xtm = [np.concatenate([r_f[2 * b]["out"], r_f[2 * b + 1]["out"]], axis=0) for b in range(B)]
            xT_b = [_c(a.T) for a in xtm]
            if l == DEPTH - 1:
                out = np.stack(xtm)
    return np.ascontiguousarray(out, dtype=np.float32)


def build_fused(S=S_FULL if False else 8192, ncores=8):
    T = S // 2
    nc = new_nc()
    P = Prog(nc)
    banks = [P.ps([128, 512]) for _ in range(8)]
    groups = [[2 * i, 2 * i + 1] for i in range(ncores // 2)]
    EI = lambda name, shape, dt=F32: nc.dram_tensor(name, list(shape), dt, kind="ExternalInput").ap()
    xT = EI("xT", [1024, S]); xTh = EI("xTh", [1024, T]); selh = EI("selh", [128, 2])
    out = nc.dram_tensor("out", [T, 1024], F32, kind="ExternalOutput").ap()
    OC = min(2048, S)
    XC = min(1024, T)
    NOC, NXC = S // OC, T // XC
    obuf = [[nc.dram_tensor(f"obuf{l}_{k}", [512, OC], BF16) for k in range(NOC)] for l in range(DEPTH)]
    og = [[nc.dram_tensor(f"og{l}_{k}", [1024, OC], BF16) for k in range(NOC)] for l in range(DEPTH)]
    x2b = [nc.dram_tensor(f"x2b_{k}", [1024, XC], BF16) for k in range(NXC)]
    xg = [nc.dram_tensor(f"xg_{k}", [2048, XC], BF16) for k in range(NXC)]
    x1f = [nc.dram_tensor(f"x1f_{l}", [1024, T], F32) for l in range(DEPTH)]
    x2f = nc.dram_tensor("x2f", [1024, T], F32)
    base = (nc.sbuf_base, nc.sbuf_top)

    def phase_end():
        P.barrier()
        nc.sbuf_base, nc.sbuf_top = base

    xTv = xT.rearrange("(c p) t -> p c t", p=128)

    def xsrc0(t0, n):
        return xTv[:, :, t0:t0 + n], "pool"

    def xsrc1(t0, n):
        r, k, tt = t0 // T, (t0 % T) // XC, t0 % XC
        return xg[k].ap()[r * 1024:(r + 1) * 1024, tt:tt + n].rearrange("(c p) t -> p c t", p=128), "sp"

    for l in range(DEPTH):
        xsrc = xsrc0 if l == 0 else xsrc1

        def odst_h(row0, t0, n, l=l):
            return obuf[l][t0 // OC].ap()[row0:row0 + 128, t0 % OC:t0 % OC + n]

        def odst_n(row0, t0, n, l=l):
            return obuf[l][t0 // OC].ap()[256 + row0:256 + row0 + 128, t0 % OC:t0 % OC + n]
        build_hgrn(S, 512, layer=l, cx=Ctx(nc, P, banks, f"l{l}h_", dict(xsrc=xsrc, odst=odst_h)))
        phase_end()
        def o_after(tend, l=l):
            if tend % OC == 0:
                k = tend // OC - 1
                P.cc(lambda e, l=l, k=k: e.collective_compute("AllGather", ALU.bypass, replica_groups=groups,
                                                              ins=[obuf[l][k].ap().opt()], outs=[og[l][k].ap().opt()]),
                     reads=[("obuf", k)])
        build_nsa(S, cx=Ctx(nc, P, banks, f"l{l}n_", dict(xsrc=xsrc, odst=odst_n, otok=lambda t0: [("obuf", t0 // OC)], after_store=o_after)))
        phase_end()

        def osrc2(which, h, t0, n, l=l):
            g0 = h * T + t0
            k, tt = g0 // OC, g0 % OC
            off = 0 if which == "a" else 256
            return [(2 * r, 2 * r + 2, og[l][k].ap()[r * 512 + off:r * 512 + off + 256, tt:tt + n].rearrange("(c p) t -> p c t", p=128))
                    for r in range(2)]
        xres = xTh if l == 0 else x2f.ap()
        build_merge(T, cx=Ctx(nc, P, banks, f"l{l}m_", dict(xT=xres, outT=x1f[l].ap(), osrc=True, osrc2=osrc2, selh=selh)))
        phase_end()
        if l % 2 == 0:
            def outb(t0, n):
                return x2b[t0 // XC].ap()[:, t0 % XC:t0 % XC + n].rearrange("(c p) t -> p c t", p=128)
            def x_after(tend):
                if tend % XC == 0:
                    k = tend // XC - 1
                    P.cc(lambda e, k=k: e.collective_compute("AllGather", ALU.bypass, replica_groups=groups,
                                                             ins=[x2b[k].ap().opt()], outs=[xg[k].ap().opt()]),
                         reads=[("x2b", k)])
            build_ffn(T, cx=Ctx(nc, P, banks, f"l{l}f_", dict(xT=x1f[l].ap(), outT=x2f.ap(), outb=outb,
                                                              otok=lambda t0: [("x2b", t0 // XC)], after_store=x_after)))
            phase_end()
        else:
            build_moe2(T, cx=Ctx(nc, P, banks, f"l{l}e_", dict(xT=x1f[l].ap(), out=out)))
            phase_end()
    P.emit()
    return nc


def fused_in_maps(S, ncores, x, w_in, hg_lb_logits, hg_norm_w, cmp_pos, cmp_w1, cmp_w2, rel_bias, w_branch_a, w_branch_b,
                  w_out, ln1_g, ln1_b, ln2_g, ln2_b, ffn_w_gate, ffn_w_up, ffn_w_down, moe_router, moe_w_gate,
                  moe_w_up, moe_w_down):
    f = lambda a: np.asarray(a, dtype=np.float32)
    x = f(x); w_in = f(w_in); hg_lb_logits = f(hg_lb_logits); hg_norm_w = f(hg_norm_w)
    cmp_pos = f(cmp_pos); cmp_w1 = f(cmp_w1); cmp_w2 = f(cmp_w2); rel_bias = f(rel_bias)
    T = S // 2
    NT = 512
    cmask = np.ones((128, NT), np.float32); cmask[:, ::64] = 0
    tri = np.zeros((64, NT), np.float32)
    for c in range(NT // 64):
        tri[:, c * 64:(c + 1) * 64] = np.triu(np.ones((64, 64), np.float32))
    st = nsa_static()
    shared = dict(cmask=cmask, tri=tri, **st)
    per_layer = []
    for l in range(DEPTH):
        W = w_in[l]
        d = {}
        d[f"l{l}m_wgt"] = _c(W[:, 3352:5400]); d[f"l{l}m_wa"] = _c(f(w_branch_a)[l]); d[f"l{l}m_wb"] = _c(f(w_branch_b)[l])
        d[f"l{l}m_wo"] = _c(f(w_out)[l]); d[f"l{l}m_lnp"] = _c(np.concatenate([_pc(f(ln1_g)[l]), _pc(f(ln1_b)[l])], axis=1))
        d[f"l{l}n_w1"] = _c(cmp_w1[l].reshape(2, 32, 64, 128).transpose(0, 2, 1, 3)); d[f"l{l}n_posT"] = _c(cmp_pos[l].transpose(0, 2, 1))
        d[f"l{l}n_w2"] = _c(cmp_w2[l])
        if l % 2 == 0:
            d[f"l{l}f_wg"] = _c(f(ffn_w_gate)[l // 2]); d[f"l{l}f_wu"] = _c(f(ffn_w_up)[l // 2]); d[f"l{l}f_wd"] = _c(f(ffn_w_down)[l // 2])
            d[f"l{l}f_lnp"] = _c(np.concatenate([_pc(f(ln2_g)[l]), _pc(f(ln2_b)[l])], axis=1))
        else:
            d[f"l{l}e_wr"] = _c(f(moe_router)[l // 2]); d[f"l{l}e_mwg"] = _c(f(moe_w_gate)[l // 2]); d[f"l{l}e_mwu"] = _c(f(moe_w_up)[l // 2])
            d[f"l{l}e_mwd"] = _c(f(moe_w_down)[l // 2]); d[f"l{l}e_lngb"] = _c(np.stack([f(ln2_g)[l], f(ln2_b)[l]]))
        per_layer.append(d)
    maps = []
    for c in range(ncores):
        b, j = c // 2, c % 2
        xTb = _c(x[b].T)
        m = dict(xT=xTb, xTh=_c(xTb[:, j * T:(j + 1) * T]), selh=_c(np.tile(np.array([[1 - j, j]], np.float32), (128, 1))))
        m.update(shared)
        m["tabaug"] = _c(np.concatenate([rel_bias[:, j * 4:(j + 1) * 4], np.full((1, 4), NEG, np.float32)], 0))
        for l in range(DEPTH):
            W = w_in[l]
            m.update(per_layer[l])
            cs = slice(j * 256, (j + 1) * 256)
            m[f"l{l}h_hwq"] = _c(W[:, 0:512][:, cs]); m[f"l{l}h_hwf"] = _c(W[:, 512:1024][:, cs])
            m[f"l{l}h_hwi"] = _c(W[:, 1024:1536][:, cs]); m[f"l{l}h_hwg"] = _c(W[:, 1536:2048][:, cs])
            nw = hg_norm_w[l, cs]
            m[f"l{l}h_hp"] = _c(np.stack([np.ones(128, np.float32), np.ones(128, np.float32), nw[:128], nw[128:]], axis=1))
            m[f"l{l}h_lbl"] = _c(np.stack([hg_lb_logits[0, cs][:128], hg_lb_logits[0, cs][128:], hg_lb_logits[1, cs][:128], hg_lb_logits[1, cs][128:]], axis=1))
            g = j
            kv = lambda base: W[:, base + g * 64: base + (g + 1) * 64]
            m[f"l{l}n_nwq"] = _c(W[:, 2048 + g * 256:2048 + (g + 1) * 256])
            m[f"l{l}n_nwk"] = _c(np.concatenate([kv(2816), kv(3072), kv(2560), kv(2688)], 1))
            m[f"l{l}n_nwt"] = _c(np.concatenate([kv(2944), kv(3200), W[:, 3328 + g * 12:3328 + (g + 1) * 12]], 1))
        maps.append(m)
    return maps


def kernel(**inputs):
    x = np.asarray(inputs["x"])
    B, S, D = x.shape
    ncores = 2 * B
    nc = _prog(("fused", S, ncores), lambda: build_fused(S, ncores))
    maps = fused_in_maps(S, ncores, **inputs)
    res = run_bass_kernel_spmd(nc, maps, core_ids=list(range(ncores))).results
    T = S // 2
    out = np.empty((B, S, D), np.float32)
    for c in range(ncores):
        out[c // 2, (c % 2) * T:(c % 2 + 1) * T] = res[c]["out"]
    return out
```

```python
import numpy as np
import concourse.bass as bass
import concourse.mybir as mybir

F32 = mybir.dt.float32
BF16 = mybir.dt.bfloat16
I32 = mybir.dt.int32
AF = mybir.ActivationFunctionType
ALU = mybir.AluOpType
AX = mybir.AxisListType

ENGS = ("pe", "act", "dve", "pool", "sp")


class Op:
    __slots__ = ("eng", "fn", "deps", "is_dma", "idx", "inc", "semval", "dma_id", "dma_prev", "cc_id")

    def __init__(self, eng, fn, is_dma):
        self.eng = eng
        self.fn = fn
        self.deps = set()
        self.is_dma = is_dma
        self.inc = False
        self.semval = 0
        self.dma_id = -1
        self.dma_prev = None
        self.cc_id = -1


class Prog:
    NDMA_SEM = 24

    def __init__(self, nc, same_engine_sync=True):
        self.nc = nc
        self.ops = []
        self.last_w = {}
        self.readers = {}
        self.same_engine_sync = same_engine_sync
        self.n_dma = 0
        self.dma_ops = []
        self.cc_ops = []
        self._n = 0

    def sb(self, shape, dt, name=None):
        self._n += 1
        return self.nc.alloc_sbuf_tensor(name or f"sb{self._n}", list(shape), dt)

    def ps(self, shape, dt=F32, name=None):
        self._n += 1
        n = 2048 // (4 if dt == F32 else 2)
        assert shape[1] <= n
        t = self.nc.alloc_psum_tensor(f"psb{self._n}", [128, n], dt)
        return t[0:shape[0], 0:shape[1]]

    def op(self, eng, fn, reads=(), writes=(), dma=False):
        o = Op(eng, fn, dma)
        o.idx = len(self.ops)
        bank_r = [r for r in reads if isinstance(r, tuple) and r[0] == "bank"]
        if bank_r:
            reads = [r for r in reads if r not in bank_r]
            writes = list(writes) + bank_r
        for r in reads:
            w = self.last_w.get(r)
            if w is not None:
                o.deps.add(w)
        for w_ in writes:
            w = self.last_w.get(w_)
            if w is not None:
                o.deps.add(w)
            for r in self.readers.get(w_, ()):
                o.deps.add(r)
        o.deps.discard(o.idx)
        for r in reads:
            self.readers.setdefault(r, []).append(o.idx)
        for w_ in writes:
            self.last_w[w_] = o.idx
            self.readers[w_] = []
        if dma:
            o.dma_id = self.n_dma
            self.n_dma += 1
            self.dma_ops.append(o.idx)
            if o.dma_id >= self.NDMA_SEM:
                o.dma_prev = self.dma_ops[o.dma_id - self.NDMA_SEM]
        self.ops.append(o)
        return o

    def dma(self, eng, out, in_, reads=(), writes=(), **kw):
        return self.op(eng, lambda e: e.dma_start(out=out, in_=in_, **kw), reads, writes, dma=True)

    def cc(self, fn, reads=(), writes=()):
        o = self.op("pool", fn, reads, writes)
        o.cc_id = len(self.cc_ops)
        self.cc_ops.append(o.idx)
        return o

    def barrier(self, wait_cc=True):
        last = {}
        for o in self.ops:
            if o.fn is not None and not o.is_dma and o.cc_id < 0 and o.eng != "sp":
                last[o.eng] = o.idx
        deps = set(last.values()) | set(self.dma_ops[-self.NDMA_SEM:])
        if wait_cc:
            deps |= set(self.cc_ops)
        for e in ENGS:
            b = Op(e, None, False)
            b.idx = len(self.ops)
            b.deps = set(deps)
            self.ops.append(b)
        keep = {t: w for t, w in self.last_w.items() if t in getattr(self, "persist", ())}
        self.last_w = keep
        self.readers = {}

    def emit(self):
        nc = self.nc
        ops = self.ops
        fin = Op("sp", None, False)
        fin.idx = len(ops)
        fin.deps = set(self.dma_ops[-self.NDMA_SEM:]) | {i for i in self.dma_ops}
        ops.append(fin)
        for o in ops:
            nd = set()
            for d in o.deps:
                p = ops[d]
                if p.is_dma or p.cc_id >= 0:
                    nd.add(d)
                    continue
                if p.eng == o.eng and not o.is_dma:
                    if p.eng == "pe" or p.eng == "sp" or not self.same_engine_sync:
                        continue
                nd.add(d)
            if o.dma_prev is not None:
                nd.add(o.dma_prev)
            o.deps = nd
            for d in nd:
                ops[d].inc = True
        val = {e: 0 for e in ENGS}
        for o in ops:
            if o.is_dma:
                o.semval = 16 * (o.dma_id // self.NDMA_SEM + 1)
            elif o.cc_id >= 0:
                o.semval = 1
            elif o.inc:
                val[o.eng] += 1
                o.semval = val[o.eng]
        self.maxval = dict(val)
        sem = {e: nc.alloc_semaphore(f"s_{e}") for e in ENGS if e != "sp"}
        dsem = [nc.alloc_semaphore(f"s_dma{i}") for i in range(min(self.NDMA_SEM, max(1, self.n_dma)))]
        csem = [nc.alloc_semaphore(f"s_cc{i}") for i in range(len(self.cc_ops))]
        per = {e: [o for o in ops if o.eng == e] for e in ENGS}

        def run(e, engobj):
            seen = {}
            for o in per[e]:
                need = {}
                for d in o.deps:
                    p = ops[d]
                    key = ("d", p.dma_id % self.NDMA_SEM) if p.is_dma else (("c", p.cc_id) if p.cc_id >= 0 else ("e", p.eng))
                    if p.semval > need.get(key, 0):
                        need[key] = p.semval
                for key, v in need.items():
                    if seen.get(key, 0) >= v:
                        continue
                    seen[key] = v
                    s = dsem[key[1]] if key[0] == "d" else (csem[key[1]] if key[0] == "c" else sem[key[1]])
                    engobj.wait_ge(s, v)
                if o.fn is None:
                    continue
                ins = o.fn(engobj)
                if o.is_dma:
                    ins.then_inc(dsem[o.dma_id % self.NDMA_SEM], 16)
                elif o.cc_id >= 0:
                    ins.then_inc(csem[o.cc_id])
                elif o.inc:
                    ins.then_inc(sem[e], 1)

        with nc.Block() as block:
            if per["pe"]:
                block.tensor(lambda t: run("pe", t))
            if per["act"]:
                block.scalar(lambda t: run("act", t))
            if per["dve"]:
                block.vector(lambda t: run("dve", t))
            if per["pool"]:
                block.gpsimd(lambda t: run("pool", t))
            block.sync(lambda t: run("sp", t))


D_MODEL = 1024
DEPTH = 2
DN_ALPHA = (2 * DEPTH) ** 0.25
LN_EPS = 1e-5
D_FF = 2752


def new_nc():
    return bass.Bass("TRN2", target_bir_lowering=False)


class Ctx:
    def __init__(self, nc=None, P=None, banks=None, prefix="", ov=None):
        self.standalone = nc is None
        self.nc = nc if nc is not None else new_nc()
        self.P = P if P is not None else Prog(self.nc)
        self.banks = banks if banks is not None else [self.P.ps([128, 512]) for _ in range(8)]
        self.prefix = prefix
        self.ov = ov or {}
        if not hasattr(self.P, "_tens"):
            self.P._tens = {}

    def inp(self, name, shape, dt=F32, shared=False):
        if name in self.ov:
            return self.ov[name]
        full = name if shared else self.prefix + name
        if full not in self.P._tens:
            self.P._tens[full] = self.nc.dram_tensor(full, list(shape), dt, kind="ExternalInput").ap()
        return self.P._tens[full]

    def out(self, name, shape, dt=F32):
        if name in self.ov:
            return self.ov[name]
        return self.nc.dram_tensor(name, list(shape), dt, kind="ExternalOutput").ap()

    def done(self):
        if self.standalone:
            self.P.emit()
            return self.nc
        return None


def mm_acc(P, out, pairs, reads, writes):
    n = len(pairs)
    for i, (l, r) in enumerate(pairs):
        P.op("pe", (lambda e, l=l, r=r, i=i: e.matmul(out, l, r, start=(i == 0), stop=(i == n - 1))),
             reads=reads[i] if (len(reads) and isinstance(reads[0], list)) else reads, writes=writes)


def load_w_bf16(P, dst, w, K, N, tok, nsplit=4):
    kc_full = K // 128
    step = max(1, (kc_full + nsplit - 1) // nsplit)
    for k0 in range(0, kc_full, step):
        k1 = min(kc_full, k0 + step)
        P.dma("pool", dst[:, k0:k1, :], w[k0 * 128:k1 * 128, :].rearrange("(c p) n -> p c n", p=128), writes=[tok])
    rem = K - kc_full * 128
    if rem:
        P.dma("pool", dst[0:rem, kc_full, :], w[kc_full * 128:K, :], writes=[tok])


def load_w_cols(P, dst, w, K, c0, c1, tok):
    kc_full = K // 128
    P.dma("pool", dst[:, 0:kc_full, c0:c1], w[0:kc_full * 128, c0:c1].rearrange("(c p) n -> p c n", p=128), writes=[tok])
    rem = K - kc_full * 128
    if rem:
        P.dma("pool", dst[0:rem, kc_full, c0:c1], w[kc_full * 128:K, c0:c1], writes=[tok])


def layer_norm_T(P, xf, b, NT, ones_b, rb, rsq, ps_mean, ps_msq, st, lng, lnb, tag,
                 rbt=lambda mo: ("rb", mo), rsqt=lambda mo: ("rsq", mo)):
    for mo in range(8):
        P.op("act", lambda e, mo=mo: e.activation(out=rb[:, mo, :], in_=xf[:, mo, :], func=AF.Copy),
             reads=[("xf", b, mo)], writes=[rbt(mo)])
        P.op("act", lambda e, mo=mo: e.activation(out=rsq[:, mo, :], in_=xf[:, mo, :], func=AF.Square),
             reads=[("xf", b, mo)], writes=[rsqt(mo)])
    mm_acc(P, ps_mean, [(ones_b[:], rb[:, mo, :]) for mo in range(8)],
           [[rbt(mo), "ones"] for mo in range(8)], [("bank", 6)])
    mm_acc(P, ps_msq, [(ones_b[:], rsq[:, mo, :]) for mo in range(8)],
           [[rsqt(mo), "ones"] for mo in range(8)], [("bank", 7)])
    mean_s, m2, rstd = st[:, 0, :], st[:, 1, :], st[:, 2, :]
    P.op("dve", lambda e: e.tensor_copy(out=mean_s, in_=ps_mean), reads=[("bank", 6)], writes=["mean_s"])
    P.op("dve", lambda e: e.tensor_tensor(out=m2, in0=mean_s, in1=mean_s, op=ALU.mult), reads=["mean_s"], writes=["m2"])
    P.op("dve", lambda e: e.tensor_tensor(out=rstd, in0=ps_msq, in1=m2, op=ALU.subtract), reads=[("bank", 7), "m2"], writes=["rstd"])
    P.op("dve", lambda e: e.tensor_scalar(out=rstd, in0=rstd, scalar1=LN_EPS, scalar2=None, op0=ALU.add),
         reads=["rstd"], writes=["rstd"])
    P.op("act", lambda e: e.activation(out=rstd, in_=rstd, func=AF.Sqrt), reads=["rstd"], writes=["rstd"])
    P.op("dve", lambda e: e.reciprocal(out=rstd, in_=rstd), reads=["rstd"], writes=["rstd"])
    for mo in range(8):
        P.op("dve", lambda e, mo=mo: e.tensor_tensor(out=xf[:, mo, :], in0=xf[:, mo, :], in1=mean_s, op=ALU.subtract),
             reads=[("xf", b, mo), "mean_s"], writes=[("xf", b, mo)])
        P.op("dve", lambda e, mo=mo: e.tensor_tensor(out=xf[:, mo, :], in0=xf[:, mo, :], in1=rstd, op=ALU.mult),
             reads=[("xf", b, mo), "rstd"], writes=[("xf", b, mo)])
        P.op("dve", lambda e, mo=mo: e.tensor_scalar(out=xf[:, mo, :], in0=xf[:, mo, :], scalar1=lng[:, mo:mo + 1],
                                                      scalar2=lnb[:, mo:mo + 1], op0=ALU.mult, op1=ALU.add),
             reads=[("xf", b, mo), "lnp"], writes=[("xf", b, mo)])


def build_ffn(T, NT=512, cx=None):
    cx = cx or Ctx()
    nc, P = cx.nc, cx.P
    xT = cx.inp("xT", [1024, T])
    wg = cx.inp("wg", [1024, D_FF]); wu = cx.inp("wu", [1024, D_FF]); wd = cx.inp("wd", [D_FF, 1024])
    lnp = cx.inp("lnp", [128, 16])
    outT = cx.out("outT", [1024, T])
    outb = cx.ov.get("outb")
    MT = 22
    wgb = P.sb([128, 8, D_FF], BF16)
    wub = P.sb([128, 8, D_FF], BF16)
    wdb = P.sb([128, MT, 1024], BF16)
    lns = P.sb([128, 16], F32)
    ones_b = P.sb([128, 128], BF16)
    xb = [P.sb([128, 8, NT], BF16) for _ in range(2)]
    xf1 = P.sb([128, 8, NT], F32)
    xf = [xf1, xf1]
    h = P.sb([128, MT, NT], BF16)
    sg = [P.sb([128, NT], F32) for _ in range(2)]
    rb = h[:, 0:8, :]
    rsq = h[:, 8:16, :]
    st = P.sb([128, 3, NT], F32)
    bk = [b_[:, 0:NT] for b_ in cx.banks]
    psg, psu, psf, ps_mean, ps_msq = bk[0:2], bk[2:4], bk[4:6], bk[6], bk[7]
    BG, BU, BF_, BMEAN, BMSQ = (0, 1), (2, 3), (4, 5), 6, 7

    P.dma("sp", lns[:], lnp, writes=["lnp"])
    P.op("dve", lambda e: e.memset(ones_b[:], 1.0 / 1024), writes=["ones"])
    xTv = xT.rearrange("(c p) t -> p c t", p=128)
    oTv = outT.rearrange("(c p) t -> p c t", p=128)
    ntiles = T // NT

    def load_x(ti):
        b = ti % 2
        P.dma("pool", xb[b][:], xTv[:, :, ti * NT:(ti + 1) * NT], writes=[("xb", b)])

    def load_xf(ti):
        P.dma("sp", xf1[:], xTv[:, :, ti * NT:(ti + 1) * NT], writes=[("xf", 0, mo) for mo in range(8)])

    load_x(0)
    load_xf(0)
    WB = 6
    for blk in range((MT + WB - 1) // WB):
        c0, c1 = blk * WB * 128, min(D_FF, (blk + 1) * WB * 128)
        load_w_cols(P, wgb, wg, 1024, c0, c1, ("wg", blk))
        load_w_cols(P, wub, wu, 1024, c0, c1, ("wu", blk))
    load_w_bf16(P, wdb, wd, D_FF, 1024, "wd")
    for ti in range(ntiles):
        b = ti % 2
        if ti + 1 < ntiles:
            load_x(ti + 1)
        for m in range(MT):
            msz = min(128, D_FF - m * 128)
            ms = slice(m * 128, m * 128 + msz)
            pb = m % 2
            mm_acc(P, psg[pb][0:msz, :], [(wgb[:, kc, ms], xb[b][:, kc, :]) for kc in range(8)],
                   [("wg", m // WB), ("xb", b)], [("bank", BG[pb])])
            mm_acc(P, psu[pb][0:msz, :], [(wub[:, kc, ms], xb[b][:, kc, :]) for kc in range(8)],
                   [("wu", m // WB), ("xb", b)], [("bank", BU[pb])])
            P.op("act", lambda e, pb=pb, msz=msz: e.activation(out=sg[pb][0:msz, :], in_=psg[pb][0:msz, :], func=AF.Silu),
                 reads=[("bank", BG[pb])], writes=[("sg", pb)])
            P.op("dve", lambda e, pb=pb, msz=msz, m=m: e.tensor_tensor(out=h[0:msz, m, :], in0=sg[pb][0:msz, :],
                                                                       in1=psu[pb][0:msz, :], op=ALU.mult),
                 reads=[("sg", pb), ("bank", BU[pb])], writes=[("h", m)])
        for mo in range(8):
            pb = mo % 2
            pairs, rds = [], []
            for kc in range(MT):
                ksz = min(128, D_FF - kc * 128)
                pairs.append((wdb[0:ksz, kc, mo * 128:(mo + 1) * 128], h[0:ksz, kc, :]))
                rds.append(["wd", ("h", kc)])
            mm_acc(P, psf[pb], pairs, rds, [("bank", BF_[pb])])
            P.op("dve", lambda e, pb=pb, mo=mo: e.scalar_tensor_tensor(
                out=xf1[:, mo, :], in0=xf1[:, mo, :], scalar=float(DN_ALPHA), in1=psf[pb],
                op0=ALU.mult, op1=ALU.add), reads=[("bank", BF_[pb]), ("xf", 0, mo)], writes=[("xf", 0, mo)])
        layer_norm_T(P, xf1, 0, NT, ones_b, rb, rsq, ps_mean, ps_msq, st, lns[:, 0:8], lns[:, 8:16], "ln2",
                     rbt=lambda mo: ("h", mo), rsqt=lambda mo: ("h", 8 + mo))
        P.dma("sp", oTv[:, :, ti * NT:(ti + 1) * NT], xf1[:], reads=[("xf", 0, mo) for mo in range(8)])
        if outb is not None:
            P.dma("pool", outb(ti * NT, NT), xf1[:], reads=[("xf", 0, mo) for mo in range(8)], writes=cx.ov.get("otok", lambda t0: [])(ti * NT))
            if cx.ov.get("after_store") is not None:
                cx.ov["after_store"]((ti + 1) * NT)
        if ti + 1 < ntiles:
            load_xf(ti + 1)
    return cx.done()


def build_merge(T, NT=512, cx=None):
    cx = cx or Ctx()
    nc, P = cx.nc, cx.P
    xT = cx.inp("xT", [1024, T])
    osrc = cx.ov.get("osrc")
    if osrc is None:
        oaT = cx.inp("oaT", [512, T], BF16); obT = cx.inp("obT", [512, T], BF16)
    wgt = cx.inp("wgt", [1024, 2048]); wa = cx.inp("wa", [512, 1024]); wb = cx.inp("wb", [512, 1024]); wo = cx.inp("wo", [1024, 1024])
    lnp = cx.inp("lnp", [128, 16])
    outT = cx.out("outT", [1024, T])
    wgtb = P.sb([128, 8, 2048], BF16)
    wab = P.sb([128, 4, 1024], BF16)
    wbb = P.sb([128, 4, 1024], BF16)
    wob = P.sb([128, 8, 1024], BF16)
    lns = P.sb([128, 16], F32)
    ones_b = P.sb([128, 128], BF16)
    xb = [P.sb([128, 8, NT], BF16) for _ in range(2)]
    xf = [P.sb([128, 8, NT], F32) for _ in range(2)]
    oa = [P.sb([128, 4, NT], BF16) for _ in range(2)]
    ob = [P.sb([128, 4, NT], BF16) for _ in range(2)]
    y = P.sb([128, 8, NT], BF16)
    sga = [P.sb([128, NT], F32) for _ in range(2)]
    sgb = [P.sb([128, NT], F32) for _ in range(2)]
    rb = P.sb([128, 8, NT], BF16)
    rsq = P.sb([128, 8, NT], BF16)
    st = P.sb([128, 3, NT], F32)
    bk = [b_[:, 0:NT] for b_ in cx.banks]
    pq, pmx, ps_mean, ps_msq = bk[0:4], bk[4:6], bk[6], bk[7]
    P.dma("sp", lns[:], lnp, writes=["lnp"])
    P.op("dve", lambda e: e.memset(ones_b[:], 1.0 / 1024), writes=["ones"])
    xTv = xT.rearrange("(c p) t -> p c t", p=128)
    if osrc is None:
        oaTv = oaT.rearrange("(c p) t -> p c t", p=128)
        obTv = obT.rearrange("(c p) t -> p c t", p=128)
        osrc = lambda which, t0, n: [(0, 4, (oaTv if which == "a" else obTv)[:, :, t0:t0 + n])]
    oTv = outT.rearrange("(c p) t -> p c t", p=128)
    ntiles = T // NT

    selh = cx.ov.get("selh")
    if selh is not None:
        sels = P.sb([128, 2], F32)
        P.dma("sp", sels[:], selh, writes=["sels"])
        cand = {(w_, h_): P.sb([128, 4, NT], BF16) for w_ in "ab" for h_ in range(2)}

    def load_x(ti):
        b = ti % 2
        ts = slice(ti * NT, (ti + 1) * NT)
        P.dma("pool", xb[b][:], xTv[:, :, ts], writes=[("xb", b)])
        P.dma("sp", xf[b][:], xTv[:, :, ts], writes=[("xf", b, mo) for mo in range(8)])
        if selh is not None:
            for w_, dst_, tk_ in (("a", oa, "oa"), ("b", ob, "ob")):
                for h_ in range(2):
                    for (c0, c1, ap_) in cx.ov["osrc2"](w_, h_, ti * NT, NT):
                        P.dma("sp", cand[(w_, h_)][:, c0:c1, :], ap_, reads=cx.ov.get("ctok", lambda h, t0: [])(h_, ti * NT),
                              writes=[("cand", w_, h_)])
                P.op("dve", lambda e, w_=w_: e.tensor_scalar(out=cand[(w_, 0)][:], in0=cand[(w_, 0)][:], scalar1=sels[:, 0:1], scalar2=None, op0=ALU.mult),
                     reads=["sels", ("cand", w_, 0)], writes=[("cand", w_, 0)])
                P.op("dve", lambda e, w_=w_, dst_=dst_, b=b: e.scalar_tensor_tensor(out=dst_[b][:], in0=cand[(w_, 1)][:], scalar=sels[:, 1:2], in1=cand[(w_, 0)][:],
                                                                                 op0=ALU.mult, op1=ALU.add),
                     reads=["sels", ("cand", w_, 0), ("cand", w_, 1)], writes=[(tk_, b)])
            return
        for (c0, c1, ap_) in osrc("a", ti * NT, NT):
            P.dma("sp", oa[b][:, c0:c1, :], ap_, writes=[("oa", b)])
        for (c0, c1, ap_) in osrc("b", ti * NT, NT):
            P.dma("sp", ob[b][:, c0:c1, :], ap_, writes=[("ob", b)])

    load_x(0)
    for blk in range(4):
        c0, c1 = blk * 256, (blk + 1) * 256
        load_w_cols(P, wgtb, wgt, 1024, c0, c1, ("wgt", blk))
        load_w_cols(P, wab, wa, 512, c0, c1, ("wa", blk))
        load_w_cols(P, wgtb, wgt, 1024, 1024 + c0, 1024 + c1, ("wgt", blk))
        load_w_cols(P, wbb, wb, 512, c0, c1, ("wb", blk))
    load_w_bf16(P, wob, wo, 1024, 1024, "wo", nsplit=2)
    for ti in range(ntiles):
        b = ti % 2
        if ti + 1 < ntiles:
            load_x(ti + 1)
        for mo in range(8):
            pb = mo % 2
            ms = slice(mo * 128, (mo + 1) * 128)
            ms2 = slice(1024 + mo * 128, 1024 + (mo + 1) * 128)
            mm_acc(P, pq[0], [(wgtb[:, kc, ms], xb[b][:, kc, :]) for kc in range(8)], [("wgt", mo // 2), ("xb", b)], [("bank", 0)])
            mm_acc(P, pq[1], [(wab[:, kc, ms], oa[b][:, kc, :]) for kc in range(4)], [("wa", mo // 2), ("oa", b)], [("bank", 1)])
            mm_acc(P, pq[2], [(wgtb[:, kc, ms2], xb[b][:, kc, :]) for kc in range(8)], [("wgt", mo // 2), ("xb", b)], [("bank", 2)])
            mm_acc(P, pq[3], [(wbb[:, kc, ms], ob[b][:, kc, :]) for kc in range(4)], [("wb", mo // 2), ("ob", b)], [("bank", 3)])
            P.op("act", lambda e, pb=pb: e.activation(out=sga[pb][:], in_=pq[0], func=AF.Sigmoid),
                 reads=[("bank", 0)], writes=[("sga", pb)])
            P.op("act", lambda e, pb=pb: e.activation(out=sgb[pb][:], in_=pq[2], func=AF.Sigmoid),
                 reads=[("bank", 2)], writes=[("sgb", pb)])
            P.op("dve", lambda e, pb=pb: e.tensor_tensor(out=sga[pb][:], in0=sga[pb][:], in1=pq[1], op=ALU.mult),
                 reads=[("sga", pb), ("bank", 1)], writes=[("sga", pb)])
            P.op("dve", lambda e, pb=pb: e.tensor_tensor(out=sgb[pb][:], in0=sgb[pb][:], in1=pq[3], op=ALU.mult),
                 reads=[("sgb", pb), ("bank", 3)], writes=[("sgb", pb)])
            P.op("dve", lambda e, pb=pb, mo=mo: e.tensor_tensor(out=y[:, mo, :], in0=sga[pb][:], in1=sgb[pb][:], op=ALU.add),
                 reads=[("sga", pb), ("sgb", pb)], writes=[("y", mo)])
        for mo in range(8):
            pb = mo % 2
            ms = slice(mo * 128, (mo + 1) * 128)
            mm_acc(P, pmx[pb], [(wob[:, kc, ms], y[:, kc, :]) for kc in range(8)],
                   [["wo", ("y", kc)] for kc in range(8)], [("bank", 4 + pb)])
            P.op("dve", lambda e, pb=pb, mo=mo, b=b: e.scalar_tensor_tensor(
                out=xf[b][:, mo, :], in0=xf[b][:, mo, :], scalar=float(DN_ALPHA), in1=pmx[pb],
                op0=ALU.mult, op1=ALU.add), reads=[("bank", 4 + pb), ("xf", b, mo)], writes=[("xf", b, mo)])
        layer_norm_T(P, xf[b], b, NT, ones_b, rb, rsq, ps_mean, ps_msq, st, lns[:, 0:8], lns[:, 8:16], "ln1")
        P.dma("sp", oTv[:, :, ti * NT:(ti + 1) * NT], xf[b][:], reads=[("xf", b, mo) for mo in range(8)])
    return cx.done()


def build_hgrn(S, NT=512, layer=0, cx=None):
    cx = cx or Ctx()
    nc, P = cx.nc, cx.P
    xsrc = cx.ov.get("xsrc")
    if xsrc is None:
        xT = cx.inp("xT", [1024, S])
        xTv_ = xT.rearrange("(c p) t -> p c t", p=128)
        xsrc = lambda t0, n: (xTv_[:, :, t0:t0 + n], "pool")
    wq = cx.inp("hwq", [1024, 256]); wf = cx.inp("hwf", [1024, 256]); wi = cx.inp("hwi", [1024, 256]); wg = cx.inp("hwg", [1024, 256])
    hp = cx.inp("hp", [128, 4]); lbl = cx.inp("lbl", [128, 4])
    cmask = cx.inp("cmask", [128, NT], shared=True); tri = cx.inp("tri", [64, NT], shared=True)
    ident = cx.inp("ident", [128, 128], shared=True)
    odst = cx.ov.get("odst")
    if odst is None:
        oaT = cx.out("oaT", [256, S], BF16)
        odst = lambda row0, t0, n: oaT[row0:row0 + 128, t0:t0 + n]
    NCH = NT // 64
    wqb = P.sb([128, 8, 256], BF16); wfb = P.sb([128, 8, 256], BF16)
    wib = P.sb([128, 8, 256], BF16); wgb = P.sb([128, 8, 256], BF16)
    hps = P.sb([128, 4], F32)
    cm = P.sb([128, NT], F32)
    tr = P.sb([64, NT], F32)
    idb = P.sb([128, 128], BF16)
    ones_b = P.sb([128, 128], BF16)
    xb = [P.sb([128, 8, NT], BF16) for _ in range(2)]
    banks = cx.banks
    rot = [0]

    def rbank():
        i = rot[0] % 3
        rot[0] += 1
        return i
    B_ATT = 3
    H = []
    for hd in range(2):
        d = dict(
            kk=P.sb([128, NT], F32), lf=P.sb([128, NT], F32), bb=P.sb([128, NT], F32), eb=P.sb([128, NT], F32),
            ex=P.sb([128, NT], F32), qf=P.sb([128, NT], F32), sgl=P.sb([128, NT], F32), rs=P.sb([128, NT], F32),
            qt=P.sb([128, NT], BF16), kt=P.sb([128, NT], BF16), kd=P.sb([128, NT], BF16),
            vtm=P.sb([64, NCH, 128], BF16), kdt=P.sb([64, NCH, 128], BF16), attm=P.sb([64, NT], BF16),
            Sf=P.sb([128, 128], F32), Sb=[P.sb([128, 128], BF16) for _ in range(2)], osq=P.sb([128, NT], BF16),
            ob=P.sb([128, NT], BF16))
        H.append(d)
    P.dma("sp", hps[:], hp, writes=["hp"])
    lbs_ = P.sb([128, 4], F32)
    P.dma("sp", lbs_[:], lbl, writes=["lbl"])
    if layer == 0:
        P.op("dve", lambda e: e.memset(hps[:, 0:2], 1.0), reads=["lbl"], writes=["hp"])
    else:
        P.op("dve", lambda e: e.tensor_tensor(out=hps[:, 0:2], in0=lbs_[:, 0:2], in1=lbs_[:, 2:4], op=ALU.subtract), reads=["lbl"], writes=["hp"])
        P.op("act", lambda e: e.activation(out=hps[:, 0:2], in_=hps[:, 0:2], func=AF.Sigmoid), reads=["hp"], writes=["hp"])
    P.dma("sp", cm[:], cmask, writes=["cm"])
    P.dma("sp", tr[:], tri, writes=["tr"])
    P.dma("pool", idb[:], ident, writes=["idb"])
    P.op("dve", lambda e: e.memset(ones_b[:], 1.0 / 128), writes=["ones"])
    for hd in range(2):
        P.op("dve", lambda e, hd=hd: e.memset(H[hd]["Sf"][:], 0.0), writes=[("Sf", hd)])
    ntiles = S // NT

    def load_x(ti):
        b = ti % 2
        r_ = xsrc(ti * NT, NT)
        P.dma(r_[1], xb[b][:], r_[0], reads=(r_[2] if len(r_) > 2 else []), writes=[("xb", b)])
    load_x(0)
    load_w_bf16(P, wqb, wq, 1024, 256, "wq", nsplit=1)
    load_w_bf16(P, wfb, wf, 1024, 256, "wf", nsplit=1)
    load_w_bf16(P, wib, wi, 1024, 256, "wi", nsplit=1)
    load_w_bf16(P, wgb, wg, 1024, 256, "wg", nsplit=1)
    gch = 0
    for ti in range(ntiles):
        b = ti % 2
        if ti + 1 < ntiles:
            load_x(ti + 1)
        def prep(hd):
            h = H[hd]
            hs = slice(hd * 128, (hd + 1) * 128)
            T = lambda n: (n, hd)
            B_O, B_S = 4 + hd, 6 + hd
            bi = rbank()
            mm_acc(P, banks[bi], [(wfb[:, kc, hs], xb[b][:, kc, :]) for kc in range(8)], ["wf", ("xb", b)], [("bank", bi)])
            P.op("act", lambda e, h=h, bi=bi: e.activation(out=h["kk"][:], in_=banks[bi], func=AF.Sigmoid, scale=-1.0),
                 reads=[("bank", bi)], writes=[T("kk")])
            P.op("dve", lambda e, h=h, hd=hd: e.tensor_scalar(out=h["kk"][:], in0=h["kk"][:], scalar1=hps[:, hd:hd + 1], scalar2=None, op0=ALU.mult),
                 reads=[T("kk"), "hp"], writes=[T("kk")])
            P.op("act", lambda e, h=h: e.activation(out=h["lf"][:], in_=h["kk"][:], func=AF.Ln, scale=-1.0, bias=1.0),
                 reads=[T("kk")], writes=[T("lf")])
            P.op("dve", lambda e, h=h: e.tensor_tensor_scan(out=h["bb"][:], data0=cm[:], data1=h["lf"][:], initial=0.0,
                                                            op0=ALU.mult, op1=ALU.add),
                 reads=[T("lf"), "cm"], writes=[T("bb")])
            P.op("act", lambda e, h=h: e.activation(out=h["eb"][:], in_=h["bb"][:], func=AF.Exp), reads=[T("bb")], writes=[T("eb")])
            bi = rbank()
            mm_acc(P, banks[bi], [(wqb[:, kc, hs], xb[b][:, kc, :]) for kc in range(8)], ["wq", ("xb", b)], [("bank", bi)])
            P.op("dve", lambda e, h=h, bi=bi: e.tensor_tensor(out=h["qt"][:], in0=banks[bi], in1=h["eb"][:], op=ALU.mult),
                 reads=[("bank", bi), T("eb")], writes=[T("qt")])
            P.op("act", lambda e, h=h: e.activation(out=h["ex"][:], in_=h["bb"][:], func=AF.Exp, scale=-1.0), reads=[T("bb")], writes=[T("ex")])
            P.op("dve", lambda e, h=h: e.tensor_tensor(out=h["kt"][:], in0=h["kk"][:], in1=h["ex"][:], op=ALU.mult),
                 reads=[T("kk"), T("ex")], writes=[T("kt")])
            for c in range(NCH):
                cs = slice(c * 64, (c + 1) * 64)
                P.op("act", lambda e, h=h, cs=cs, c=c: e.activation(out=h["ex"][:, cs], in_=h["bb"][:, cs], func=AF.Exp, scale=-1.0,
                                                                    bias=h["bb"][:, c * 64 + 63:c * 64 + 64]),
                     reads=[T("bb"), T("kt")], writes=[T("ex")])
            P.op("dve", lambda e, h=h: e.tensor_tensor(out=h["kd"][:], in0=h["kk"][:], in1=h["ex"][:], op=ALU.mult),
                 reads=[T("kk"), T("ex")], writes=[T("kd")])
            bi = rbank()
            mm_acc(P, banks[bi], [(wgb[:, kc, hs], xb[b][:, kc, :]) for kc in range(8)], ["wg", ("xb", b)], [("bank", bi)])
            P.op("act", lambda e, h=h, bi=bi: e.activation(out=h["sgl"][:], in_=banks[bi], func=AF.Silu), reads=[("bank", bi)], writes=[T("sgl")])
            for half in range(NCH // 4):
                bi = rbank()
                for cc in range(4):
                    c = half * 4 + cc
                    mm_acc(P, banks[bi][0:64, cc * 128:(cc + 1) * 128],
                           [(xb[b][:, kc, c * 64:(c + 1) * 64], wib[:, kc, hs]) for kc in range(8)], ["wi", ("xb", b)], [("bank", bi)])
                P.op("act", lambda e, h=h, bi=bi, half=half: e.activation(
                    out=h["vtm"][:, half * 4:(half + 1) * 4, :], in_=banks[bi][0:64, :].rearrange("p (c v) -> p c v", v=128), func=AF.Copy),
                    reads=[("bank", bi)], writes=[T("vtm")])
            for half in range(NCH // 4):
                bi = rbank()
                for cc in range(4):
                    c = half * 4 + cc
                    P.op("pe", lambda e, h=h, bi=bi, cc=cc, c=c: e.matmul(banks[bi][0:64, cc * 128:(cc + 1) * 128], h["kd"][:, c * 64:(c + 1) * 64],
                                                                          idb[:], start=True, stop=True),
                         reads=[T("kd"), "idb"], writes=[("bank", bi)])
                P.op("dve", lambda e, h=h, bi=bi, half=half: e.tensor_copy(
                    out=h["kdt"][:, half * 4:(half + 1) * 4, :], in_=banks[bi][0:64, :].rearrange("p (c v) -> p c v", v=128)),
                    reads=[("bank", bi)], writes=[T("kdt")])
            for c in range(NCH):
                cs = slice(c * 64, (c + 1) * 64)
                P.op("pe", lambda e, h=h, cs=cs: e.matmul(banks[B_ATT][0:64, cs], h["kt"][:, cs], h["qt"][:, cs], start=True, stop=True),
                     reads=[T("kt"), T("qt")], writes=[("bank", B_ATT)])
            P.op("dve", lambda e, h=h: e.tensor_tensor(out=h["attm"][:], in0=banks[B_ATT][0:64, 0:NT], in1=tr[:], op=ALU.mult),
                 reads=[("bank", B_ATT), "tr"], writes=[T("attm")])
        def chunk(hd, c):
            h = H[hd]
            hs = slice(hd * 128, (hd + 1) * 128)
            T = lambda n: (n, hd)
            B_O, B_S = 4 + hd, 6 + hd
            cs = slice(c * 64, (c + 1) * 64)
            g = ti * NCH + c
            sb_cur = h["Sb"][g % 2]
            first = (g == 0)
            P.op("pe", lambda e, h=h, cs=cs, c=c, first=first: e.matmul(banks[B_O][:, cs], h["vtm"][:, c, :], h["attm"][:, cs],
                                                                        start=True, stop=first),
                 reads=[T("vtm"), T("attm")], writes=[("bank", B_O)])
            if not first:
                P.op("pe", lambda e, h=h, cs=cs, sb_cur=sb_cur: e.matmul(banks[B_O][:, cs], sb_cur[:], h["qt"][:, cs], start=False, stop=True),
                     reads=[("Sb", hd, g % 2), T("qt")], writes=[("bank", B_O)])
            P.op("pe", lambda e, h=h, c=c: e.matmul(banks[B_S][:, 0:128], h["kdt"][:, c, :], h["vtm"][:, c, :], start=True, stop=True),
                 reads=[T("kdt"), T("vtm")], writes=[("bank", B_S)])
            P.op("dve", lambda e, h=h, c=c: e.scalar_tensor_tensor(out=h["Sf"][:], in0=h["Sf"][:], scalar=h["eb"][:, c * 64 + 63:c * 64 + 64],
                                                                     in1=banks[B_S][:, 0:128], op0=ALU.mult, op1=ALU.add),
                 reads=[("Sf", hd), ("bank", B_S), T("eb")], writes=[("Sf", hd)])
            nxt = h["Sb"][(g + 1) % 2]
            P.op("act", lambda e, h=h, nxt=nxt: e.activation(out=nxt[:], in_=h["Sf"][:], func=AF.Copy),
                 reads=[("Sf", hd)], writes=[("Sb", hd, (g + 1) % 2)])
        def post(hd):
            h = H[hd]
            hs = slice(hd * 128, (hd + 1) * 128)
            T = lambda n: (n, hd)
            B_O, B_S = 4 + hd, 6 + hd
            P.op("act", lambda e, h=h: e.activation(out=h["osq"][:], in_=banks[B_O][:, 0:NT], func=AF.Square), reads=[("bank", B_O)], writes=[T("osq")])
            bi = rbank()
            P.op("pe", lambda e, h=h, bi=bi: e.matmul(banks[bi][:, 0:NT], ones_b[:], h["osq"][:], start=True, stop=True),
                 reads=["ones", T("osq")], writes=[("bank", bi)])
            P.op("dve", lambda e, h=h, bi=bi: e.tensor_scalar(out=h["rs"][:], in0=banks[bi][:, 0:NT], scalar1=1e-6, scalar2=None, op0=ALU.add),
                 reads=[("bank", bi)], writes=[T("rs")])
            P.op("act", lambda e, h=h: e.activation(out=h["rs"][:], in_=h["rs"][:], func=AF.Sqrt), reads=[T("rs")], writes=[T("rs")])
            P.op("dve", lambda e, h=h: e.reciprocal(out=h["rs"][:], in_=h["rs"][:]), reads=[T("rs")], writes=[T("rs")])
            P.op("dve", lambda e, h=h: e.tensor_tensor(out=h["rs"][:], in0=h["rs"][:], in1=banks[B_O][:, 0:NT], op=ALU.mult),
                 reads=[T("rs"), ("bank", B_O)], writes=[T("rs")])
            P.op("dve", lambda e, h=h, hd=hd: e.scalar_tensor_tensor(out=h["ob"][:], in0=h["rs"][:], scalar=hps[:, 2 + hd:3 + hd], in1=h["sgl"][:],
                                                                      op0=ALU.mult, op1=ALU.mult),
                 reads=[T("rs"), T("sgl"), "hp"], writes=[T("ob")])
            P.dma("sp", odst(hd * 128, ti * NT, NT), h["ob"][:], reads=[T("ob")])
        for hd in range(2):
            prep(hd)
        for c in range(NCH):
            for hd in range(2):
                chunk(hd, c)
        for hd in range(2):
            post(hd)
    return cx.done()


import math

NEG = -30000.0
GELU_C2 = 2.0 * 0.7978845608028654
W_C, W_S, W_W = 4224, 2176, 640
OFFD, ND = 2064, 6272
OFFW, NDW = 128, 768


def _bucket(d):
    n = np.maximum(d, 0)
    large = 16 + (np.log(np.maximum(n, 16).astype(np.float32) / np.float32(16)) / np.float32(math.log(2048 / 16))
                  * np.float32(16)).astype(np.int32)
    return np.where(n < 16, n, np.minimum(large, 31))


def nsa_static():
    d = np.arange(ND) - OFFD
    ohd = np.zeros((33, ND), np.float32)
    bk = _bucket(d)
    ohd[bk[d >= 0], np.nonzero(d >= 0)[0]] = 1.0
    ohd[32, d < 0] = 1.0
    dw = np.arange(NDW) - OFFW
    ohw = np.zeros((33, NDW), np.float32)
    okw = (dw >= 0) & (dw < 512)
    ohw[_bucket(dw)[okw], np.nonzero(okw)[0]] = 1.0
    ohw[32, ~okw] = 1.0
    n = np.arange(512)[:, None]
    s = np.arange(128)[None, :]
    ov = np.clip(np.minimum(n * 16 + 32, s * 64 + 64) - np.maximum(n * 16, s * 64), 0, None) / 32.0
    ov[511] = 0.0
    c2s = np.ascontiguousarray(ov.reshape(4, 128, 128).transpose(1, 0, 2)).astype(np.float32)
    q = np.arange(128)[:, None]
    c = np.arange(256)[None, :]
    hi = (q >= 64).astype(np.int64)
    vt = ((c - 126) <= hi - 2).astype(np.float32)
    ft = (((c - 126) == hi) | ((c - 126) == hi - 1)).astype(np.float32)
    ident = np.eye(128, dtype=np.float32)
    return dict(ohd=ohd, ohw=ohw, c2s=c2s, vt=vt, vm1=vt - 1.0, ft=ft, ident=ident, ident4=np.tile(ident, (1, 4)), aident=np.ascontiguousarray(ident[::-1]),
                lstrict=np.triu(np.ones((128, 128), np.float32), 1))


def build_nsa(S, upto=99, qbs=None, cx=None):
    cx = cx or Ctx()
    nc, P = cx.nc, cx.P
    NQB = S // 128
    NT = 512
    I = cx.inp
    SI = lambda name, shape, dt=F32: cx.inp(name, shape, dt, shared=True)
    xsrc = cx.ov.get("xsrc")
    if xsrc is None:
        xT = I("xT", [1024, S])
        xTv_ = xT.rearrange("(c p) t -> p c t", p=128)
        xsrc = lambda t0, n: (xTv_[:, :, t0:t0 + n], "pool")
    wq = I("nwq", [1024, 256]); wk = I("nwk", [1024, 256]); wt = I("nwt", [1024, 140])
    w1 = I("w1", [2, 64, 32, 128]); posT = I("posT", [2, 64, 32]); w2 = I("w2", [2, 128, 64])
    tabaug = SI("tabaug", [33, 4])
    ohd = SI("ohd", [33, ND]); ohw = SI("ohw", [33, NDW])
    c2s_d = SI("c2s", [128, 4, 128]); vt_d = SI("vt", [128, 256]); vm1_d = SI("vm1", [128, 256]); ft_d = SI("ft", [128, 256])
    ident_d = SI("ident", [128, 128]); ident4_d = SI("ident4", [128, 512]); aident_d = SI("aident", [128, 128])
    odst = cx.ov.get("odst")
    if odst is None:
        obT = cx.out("obT", [256, S], BF16)
        odst = lambda row0, t0, n: obT[row0:row0 + 128, t0:t0 + n]
    otok = cx.ov.get("otok", lambda t0: [])
    after_store = cx.ov.get("after_store")
    fD = nc.dram_tensor(cx.prefix + "fD", [4, ND], F32, kind="Internal")
    fW = nc.dram_tensor(cx.prefix + "fW", [4, NDW], F32, kind="Internal")
    banks = cx.banks
    B_OC, B_U, B_OS, B_OW, B_M = 3, 4, 5, 6, 7
    rot = [0]

    def rbank():
        i = rot[0] % 3
        rot[0] += 1
        return i
    wqb = P.sb([128, 8, 256], BF16)
    KsT = P.sb([65, S], BF16); KwT = P.sb([64, S], BF16)
    Vs = P.sb([128, NQB, 65], BF16); Vw = P.sb([128, NQB, 65], BF16)
    gsig = P.sb([128, NQB, 12], F32)
    kcT = P.sb([65, 512], BF16); vc = P.sb([128, 4, 65], BF16)
    c2s = P.sb([128, 4, 128], BF16)
    vt = P.sb([128, 256], F32); vm1 = P.sb([128, 256], F32); ft = P.sb([128, 256], F32)
    idb = P.sb([128, 128], BF16); id4 = P.sb([128, 512], BF16); jdb = P.sb([128, 128], BF16)
    tq = P.sb([65, 4], F32)
    AR = max(4 * (W_C + W_S + W_W), 2 * 8 * NT + 8 * 256 + 8 * 140 + 2 * 32 * 128 + 2 * S)
    arena = P.sb([128, AR], BF16)
    o = 0

    def carve(n):
        nonlocal o
        a = arena[:, o:o + n]
        o += n
        return a
    xb = [carve(8 * NT).rearrange("p (c t) -> p c t", t=NT) for _ in range(2)]
    wkb = carve(8 * 256).rearrange("p (c n) -> p c n", n=256)
    wtb = carve(8 * 140).rearrange("p (c n) -> p c n", n=140)
    w1b = carve(2 * 32 * 128).rearrange("p (k l j) -> p k l j", k=2, l=32)
    KcT = carve(S); VcT = carve(S)
    assert o <= AR, (o, AR)
    TcT = arena[:, 0:4 * W_C].rearrange("p (h w) -> p h w", h=4)
    TsT = arena[:, 4 * W_C:4 * (W_C + W_S)].rearrange("p (h w) -> p h w", h=4)
    TwT = arena[:, 4 * (W_C + W_S):4 * (W_C + W_S + W_W)].rearrange("p (h w) -> p h w", h=4)
    ARENA_TOK = [("xb", 0), ("xb", 1), "wk", "wt", "w1", "KcT", "VcT"]
    posb = P.sb([64, 2, 32], BF16); w2b = P.sb([128, 2, 64], BF16)
    cst = P.sb([128, 2], F32)
    g1 = P.sb([128, 512], F32); g2 = P.sb([128, 512], F32); h1g = P.sb([128, 512], BF16)
    tabs = P.sb([33, 4], F32); ohs = [P.sb([33, 512], F32) for _ in range(2)]
    fsb = P.sb([4, ND], F32); fwsb = P.sb([4, NDW], F32)
    xq = [P.sb([128, 8, 128], BF16) for _ in range(2)]
    Qb = [P.sb([65, 4, 128], BF16) for _ in range(2)]
    pT = [P.sb([128, 512], BF16) for _ in range(3)]
    rz = P.sb([128, 12], F32); coef = P.sb([128, 12], F32)
    imp = P.sb([128, 128], F32); imp2 = P.sb([128, 128], F32); m8a = P.sb([128, 8], F32); m8b = P.sb([128, 8], F32)
    selneg = P.sb([128, 128], BF16)
    selx = P.sb([128, S], BF16)
    ofin = P.sb([128, 256], F32); ofb = P.sb([128, 256], BF16); oT = [P.sb([128, 2, 128], BF16) for _ in range(2)]

    P.dma("pool", wqb[:], wq.rearrange("(c p) n -> p c n", p=128), writes=["wq"])
    P.dma("pool", wkb, wk.rearrange("(c p) n -> p c n", p=128), writes=["wk"])
    P.dma("pool", wtb, wt.rearrange("(c p) n -> p c n", p=128), writes=["wt"])
    P.dma("pool", w1b[0:64], w1.rearrange("k e l j -> e k l j"), writes=["w1"])
    P.dma("pool", posb[:], posT.rearrange("k e l -> e k l"), writes=["pos"])
    P.dma("pool", w2b[:], w2.rearrange("k j d -> j k d"), writes=["w2"])
    P.dma("pool", c2s[:], c2s_d, writes=["c2s"])
    P.dma("pool", idb[:], ident_d, writes=["idb"])
    P.dma("pool", id4[:], ident4_d, writes=["id4"])
    P.dma("pool", jdb[:], aident_d, writes=["jdb"])
    P.dma("sp", vt[:], vt_d, writes=["vt"]); P.dma("sp", vm1[:], vm1_d, writes=["vm1"]); P.dma("sp", ft[:], ft_d, writes=["ft"])
    P.dma("sp", tabs[:], tabaug, writes=["tabs"])
    P.dma("sp", tq[64:65, :], tabaug[31:32, :], writes=["tq"])
    P.op("dve", lambda e: e.memset(KsT[64:65, :], 1.0), writes=["KsT_aug"])
    P.op("dve", lambda e: e.memset(kcT[:], 0.0), writes=["kcT"])
    P.op("dve", lambda e: e.memset(kcT[64:65, :], 1.0), writes=["kcT"])
    P.op("dve", lambda e: e.memset(vc[:], 0.0), writes=["vc"])
    P.op("dve", lambda e: e.memset(vc[:, :, 64:65], 1.0), writes=["vc"])
    P.op("pool", lambda e: e.memset(Vs[:, :, 64:65], 1.0), writes=["Vs_aug"])
    P.op("pool", lambda e: e.memset(Vw[:, :, 64:65], 1.0), writes=["Vw_aug"])
    for b in range(2):
        for h in range(4):
            P.op("pool", lambda e, b=b, h=h: e.memset(Qb[b][64:65, h, :], 0.0), writes=[("Qaug", b)])
            P.op("pool", lambda e, b=b, h=h: e.tensor_scalar(out=Qb[b][64:65, h, :], in0=Qb[b][64:65, h, :], scalar1=tq[64:65, h:h + 1],
                                                             scalar2=None, op0=ALU.add), reads=["tq"], writes=[("Qaug", b)])
    for (oh_d, n_d, dst_s, dst_d, tk) in ((ohd, ND, fsb, fD, "fD"), (ohw, NDW, fwsb, fW, "fW")):
        for ci, c0 in enumerate(range(0, n_d, 512)):
            c1 = min(n_d, c0 + 512)
            ob_ = ohs[ci % 2]
            P.dma("sp", ob_[:, 0:c1 - c0], oh_d[:, c0:c1], writes=[("ohs", ci % 2)])
            P.op("pe", lambda e, ob_=ob_, n=c1 - c0: e.matmul(banks[B_M][0:4, 0:n], tabs[:], ob_[:, 0:n], start=True, stop=True),
                 reads=["tabs", ("ohs", ci % 2)], writes=[("bank", B_M)])
            P.op("act", lambda e, dst_s=dst_s, c0=c0, c1=c1: e.activation(out=dst_s[:, c0:c1], in_=banks[B_M][0:4, 0:c1 - c0], func=AF.Copy),
                 reads=[("bank", B_M)], writes=[tk + "s"])
        P.dma("sp", dst_d.ap(), dst_s[:], reads=[tk + "s"], writes=[tk])

    if upto < 1:
        return cx.done()
    def load_x(ti):
        r_ = xsrc(ti * NT, NT)
        P.dma(r_[1], xb[ti % 2], r_[0], reads=(r_[2] if len(r_) > 2 else []), writes=[("xb", ti % 2)])
    load_x(0)
    for ti in range(S // NT):
        b = ti % 2
        ts = slice(ti * NT, (ti + 1) * NT)
        if ti + 1 < S // NT:
            load_x(ti + 1)
        for j, (dst, tk) in enumerate(((KsT, "KsT"), (KwT, "KwT"), (KcT, "KcT"), (VcT, "VcT"))):
            bi = rbank()
            mm_acc(P, banks[bi][0:64, :], [(wkb[:, kc, j * 64:(j + 1) * 64], xb[b][:, kc, :]) for kc in range(8)], ["wk", ("xb", b)], [("bank", bi)])
            eng = "act" if j % 2 == 0 else "dve"
            if eng == "act":
                P.op("act", lambda e, dst=dst, bi=bi, ts=ts: e.activation(out=dst[0:64, ts], in_=banks[bi][0:64, :], func=AF.Copy),
                     reads=[("bank", bi)], writes=[tk])
            else:
                P.op("dve", lambda e, dst=dst, bi=bi, ts=ts: e.tensor_copy(out=dst[0:64, ts], in_=banks[bi][0:64, :]),
                     reads=[("bank", bi)], writes=[tk])
        for st in range(4):
            bi = rbank()
            qi = ti * 4 + st
            mm_acc(P, banks[bi][:, 0:140], [(xb[b][:, kc, st * 128:(st + 1) * 128], wtb[:, kc, :]) for kc in range(8)], ["wt", ("xb", b)], [("bank", bi)])
            P.op("dve", lambda e, bi=bi, qi=qi: e.tensor_copy(out=Vs[:, qi, 0:64], in_=banks[bi][:, 0:64]), reads=[("bank", bi)], writes=["Vs"])
            P.op("dve", lambda e, bi=bi, qi=qi: e.tensor_copy(out=Vw[:, qi, 0:64], in_=banks[bi][:, 64:128]), reads=[("bank", bi)], writes=["Vw"])
            P.op("act", lambda e, bi=bi, qi=qi: e.activation(out=gsig[:, qi, :], in_=banks[bi][:, 128:140], func=AF.Sigmoid),
                 reads=[("bank", bi)], writes=["gsig"])
    if upto < 2:
        return cx.done()
    NCB = (S - 32) // 16 + 1
    for kv, (src, tk) in enumerate(((KcT, "KcT"), (VcT, "VcT"))):
        bi = rbank()
        mm_acc(P, banks[bi][:, 0:1], [(w1b[0:64, kv, l, :], posb[:, kv, l:l + 1]) for l in range(32)], ["w1", "pos"], [("bank", bi)])
        P.op("dve", lambda e, bi=bi, kv=kv: e.tensor_copy(out=cst[:, kv:kv + 1], in_=banks[bi][:, 0:1]), reads=[("bank", bi)], writes=["cst"])
        bi = rbank()
        srcv = src[0:64, :]
        mm_acc(P, banks[bi][:, 0:NCB],
               [(w1b[0:64, kv, l, :], src[0:64, l:l + 16 * (NCB - 1) + 1:16]) for l in range(32)], ["w1", tk], [("bank", bi)])
        P.op("dve", lambda e, bi=bi, kv=kv: e.tensor_scalar(out=g1[:, 0:NCB], in0=banks[bi][:, 0:NCB], scalar1=cst[:, kv:kv + 1], scalar2=None, op0=ALU.add),
             reads=[("bank", bi), "cst"], writes=["g1"])
        P.op("dve", lambda e: e.tensor_tensor(out=g2[:, 0:NCB], in0=g1[:, 0:NCB], in1=g1[:, 0:NCB], op=ALU.mult), reads=["g1"], writes=["g2"])
        P.op("dve", lambda e: e.tensor_scalar(out=g2[:, 0:NCB], in0=g2[:, 0:NCB], scalar1=0.044715, scalar2=1.0, op0=ALU.mult, op1=ALU.add),
             reads=["g2"], writes=["g2"])
        P.op("dve", lambda e: e.tensor_tensor(out=g2[:, 0:NCB], in0=g2[:, 0:NCB], in1=g1[:, 0:NCB], op=ALU.mult), reads=["g1", "g2"], writes=["g2"])
        P.op("act", lambda e: e.activation(out=g2[:, 0:NCB], in_=g2[:, 0:NCB], func=AF.Sigmoid, scale=GELU_C2), reads=["g2"], writes=["g2"])
        P.op("dve", lambda e: e.memset(h1g[:], 0.0), reads=["h1g"], writes=["h1g"])
        P.op("dve", lambda e: e.tensor_tensor(out=h1g[:, 0:NCB], in0=g2[:, 0:NCB], in1=g1[:, 0:NCB], op=ALU.mult), reads=["g1", "g2"], writes=["h1g"])
        if kv == 0:
            bi = rbank()
            P.op("pe", lambda e, bi=bi: e.matmul(banks[bi][0:64, 0:NCB], w2b[:, 0, :], h1g[:, 0:NCB], start=True, stop=True),
                 reads=["w2", "h1g"], writes=[("bank", bi)])
            P.op("act", lambda e, bi=bi: e.activation(out=kcT[0:64, 0:NCB], in_=banks[bi][0:64, 0:NCB], func=AF.Copy), reads=[("bank", bi)], writes=["kcT"])
        else:
            bi = rbank()
            for i in range((NCB + 127) // 128):
                P.op("pe", lambda e, bi=bi, i=i: e.matmul(banks[bi][:, i * 64:(i + 1) * 64], h1g[:, i * 128:(i + 1) * 128], w2b[:, 1, :], start=True, stop=True),
                     reads=["w2", "h1g"], writes=[("bank", bi)])
            ni = (NCB + 127) // 128
            P.op("act", lambda e, bi=bi, ni=ni: e.activation(out=vc[:, 0:ni, 0:64], in_=banks[bi][:, 0:ni * 64].rearrange("p (i d) -> p i d", d=64), func=AF.Copy),
                 reads=[("bank", bi)], writes=["vc"])
    if upto < 3:
        return cx.done()
    for h in range(4):
        P.dma("pool", TcT[:, h, :], bass.AP(fD, h * ND + OFFD - 2063, [[16, 128], [1, W_C]]), reads=["fD"], writes=ARENA_TOK + ["TcT"])
        P.dma("pool", TsT[:, h, :], bass.AP(fD, h * ND + OFFD - 127, [[1, 128], [1, W_S]]), reads=["fD"], writes=ARENA_TOK + ["TsT"])
        P.dma("pool", TwT[:, h, :], bass.AP(fW, h * NDW + OFFW - 127, [[1, 128], [1, W_W]]), reads=["fW"], writes=ARENA_TOK + ["TwT"])

    if upto < 4:
        return cx.done()
    def load_xq(qb):
        r_ = xsrc(qb * 128, 128)
        P.dma(r_[1], xq[qb % 2][:], r_[0], reads=(r_[2] if len(r_) > 2 else []), writes=[("xq", qb % 2)])

    def score_tile(qb, Qa, lhs_full, near, table, wbase, mask_j, acc_bank, acc_tok, v_rhs, first, extra=None):
        bi = rbank()
        kk = 64 if near else 65
        nmm = 1 + (1 if near else 0) + (1 if mask_j is not None else 0)
        k = 0
        P.op("pe", lambda e: e.matmul(banks[bi], lhs_full[0:kk, :], Qa[0:kk].rearrange("p h q -> p (h q)"), start=True, stop=(nmm == 1)),
             reads=[("Q", qb % 2), ("Qaug", qb % 2), "kcT", "KsT", "KwT", "KsT_aug"], writes=[("bank", bi)])
        k += 1
        if near:
            P.op("pe", lambda e, k=k: e.matmul(banks[bi].rearrange("p (h q) -> p h q", h=4), jdb[:], table[:, :, wbase:wbase + 128], start=False, stop=(k == nmm - 1)),
                 reads=["jdb", "TcT", "TsT", "TwT"], writes=[("bank", bi)])
            k += 1
        if mask_j is not None:
            lw = selx[:, mask_j * 128:(mask_j + 1) * 128]
            P.op("pe", lambda e: e.matmul(banks[bi], lw, id4[:], start=False, stop=True),
                 reads=[("selx", mask_j // 8), "id4"], writes=[("bank", bi)])
        pt = pT[bi]
        P.op("act", lambda e: e.activation(out=pt[:], in_=banks[bi], func=AF.Exp), reads=[("bank", bi)], writes=[("pT", bi)])

        def pv():
            for h in range(4):
                P.op("pe", lambda e, h=h: e.matmul(banks[acc_bank][:, h * 65:(h + 1) * 65], pt[:, h * 128:(h + 1) * 128], v_rhs,
                                                   start=(first and h == 0), stop=False, skip_group_check=True),
                     reads=[("pT", bi), "vc", "Vs", "Vw", "Vs_aug", "Vw_aug"], writes=[("bank", acc_bank)])
                if extra is not None:
                    extra(pt, bi, h, first and h == 0)
        pend.append(pv)
        while len(pend) > 2:
            pend.pop(0)()

    pend = []
    deferred = []
    oacc = [P.sb([128, 3, 260], F32) for _ in range(2)]

    def flush():
        while pend:
            pend.pop(0)()

    qdone = {}

    def qproj(qb_):
        b_ = qb_ % 2
        for h in range(4):
            mm_acc(P, banks[B_M][0:64, h * 128:(h + 1) * 128], [(wqb[:, kc, h * 64:(h + 1) * 64], xq[b_][:, kc, :]) for kc in range(8)],
                   ["wq", ("xq", b_)], [("bank", B_M)])
        P.op("act", lambda e: e.activation(out=Qb[b_][0:64], in_=banks[B_M][0:64, :].rearrange("p (h q) -> p h q", h=4), func=AF.Copy, scale=0.125),
             reads=[("bank", B_M)], writes=[("Q", b_)])
        qdone[qb_] = True

    load_xq(0)
    for qb in range(NQB):
        b = qb % 2
        t0 = qb * 128
        if qb + 1 < NQB:
            load_xq(qb + 1)
        if qbs is not None and qb not in qbs:
            continue
        Qa = Qb[b]
        if not qdone.get(qb):
            qproj(qb)
        ni = min(3, (t0 + 96) // 2048)
        for i in range(ni + 1):
            wbase = t0 - 2048 * i
            near = wbase < W_C

            def extra(pt, bi, h, st, i=i):
                P.op("pe", lambda e: e.matmul(banks[B_U][:, h * 128:(h + 1) * 128], pt[:, h * 128:(h + 1) * 128], c2s[:, i, :],
                                              start=st, stop=False, skip_group_check=True),
                     reads=[("pT", bi), "c2s"], writes=[("bank", B_U)])
            score_tile(qb, Qa, kcT[:, i * 128:(i + 1) * 128], near, TcT, wbase if near else 0, None, B_OC, "oc", vc[:, i, :], i == 0,
                       extra if qb >= 8 else None)
        while deferred:
            deferred.pop(0)()
        flush()
        if qb >= 8:
            ocv = banks[B_OC][:, 0:260].rearrange("p (h c) -> p h c", c=65)
            P.op("dve", lambda e: e.tensor_scalar(out=rz[:, 0:4], in0=ocv[:, :, 64], scalar1=1e-30, scalar2=None, op0=ALU.max),
                 reads=[("bank", B_OC)], writes=["rzc"])
            P.op("dve", lambda e: e.reciprocal(out=rz[:, 0:4], in_=rz[:, 0:4]), reads=["rzc"], writes=["rzc"])
            for h in range(4):
                if h == 0:
                    P.op("dve", lambda e: e.tensor_scalar(out=imp[:], in0=banks[B_U][:, 0:128], scalar1=rz[:, 0:1], scalar2=None, op0=ALU.mult),
                         reads=[("bank", B_U), "rzc"], writes=["imp"])
                else:
                    P.op("dve", lambda e, h=h: e.scalar_tensor_tensor(out=imp[:], in0=banks[B_U][:, h * 128:(h + 1) * 128], scalar=rz[:, h:h + 1],
                                                                       in1=imp[:], op0=ALU.mult, op1=ALU.add),
                         reads=[("bank", B_U), "rzc", "imp"], writes=["imp"])
            c0 = 126 - 2 * qb
            P.op("dve", lambda e, c0=c0: e.tensor_tensor(out=imp[:], in0=imp[:], in1=vt[:, c0:c0 + 128], op=ALU.mult), reads=["imp", "vt"], writes=["imp"])
            P.op("dve", lambda e, c0=c0: e.tensor_tensor(out=imp[:], in0=imp[:], in1=vm1[:, c0:c0 + 128], op=ALU.add), reads=["imp", "vm1"], writes=["imp"])
            P.op("dve", lambda e: e.memset(imp[:, 0:1], -1.0), reads=["imp"], writes=["imp"])
            P.op("dve", lambda e: e.max(out=m8a[:], in_=imp[:]), reads=["imp"], writes=["m8a"])
            P.op("dve", lambda e: e.match_replace(out=imp2[:], in_to_replace=m8a[:], in_values=imp[:], imm_value=-2.0), reads=["imp", "m8a"], writes=["imp2"])
            P.op("dve", lambda e: e.max(out=m8b[:], in_=imp2[:]), reads=["imp2"], writes=["m8b"])
            P.op("dve", lambda e: e.tensor_scalar(out=imp2[:], in0=imp[:], scalar1=m8b[:, 4:5], scalar2=None, op0=ALU.is_ge), reads=["imp", "m8b"], writes=["imp2"])
            P.op("dve", lambda e, c0=c0: e.tensor_tensor(out=imp2[:], in0=imp2[:], in1=ft[:, c0:c0 + 128], op=ALU.max), reads=["imp2", "ft"], writes=["imp2"])
            P.op("dve", lambda e: e.memset(imp2[:, 0:1], 1.0), reads=["imp2"], writes=["imp2"])
            P.op("dve", lambda e: e.tensor_scalar(out=selneg[:], in0=imp2[:], scalar1=-NEG, scalar2=NEG, op0=ALU.mult, op1=ALU.add),
                 reads=["imp2"], writes=["selneg"])
            nkb = 2 * (qb + 1)
            for k0 in range(0, nkb, 16):
                k1 = min(nkb, k0 + 16)
                P.op("dve", lambda e, k0=k0, k1=k1: e.tensor_copy(out=selx[:, k0 * 64:k1 * 64].rearrange("p (k j) -> p k j", j=64),
                                                                  in_=selneg[:, k0:k1].unsqueeze(2).to_broadcast([128, k1 - k0, 64])),
                     reads=["selneg"], writes=[("selx", k0 // 16)])
        j0 = max(0, qb - 4)
        for j in range(j0, qb + 1):
            score_tile(qb, Qa, KwT[:, j * 128:(j + 1) * 128], True, TwT, 128 * (qb - j), None, B_OW, "ow", Vw[:, j, :], j == j0)
        if qb + 1 < NQB and (qbs is None or (qb + 1) in qbs):
            qproj(qb + 1)
        for j in range(qb + 1):
            near = (qb - j) <= 16
            score_tile(qb, Qa, KsT[:, j * 128:(j + 1) * 128], near, TsT, 128 * (qb - j) if near else 0, j if qb >= 8 else None,
                       B_OS, "os", Vs[:, j, :], j == 0)
        flush()
        oa_ = oacc[b]
        for x_, bk in enumerate((B_OC, B_OS, B_OW)):
            P.op("dve", lambda e, x_=x_, bk=bk, oa_=oa_: e.tensor_copy(out=oa_[:, x_, :], in_=banks[bk][:, 0:260]),
                 reads=[("bank", bk)], writes=[("oacc", b, x_)])
        oav = oa_[:].rearrange("p x (h c) -> p x h c", c=65)
        P.op("dve", lambda e, oav=oav: e.tensor_scalar(out=rz[:].rearrange("p (x h) -> p x h", x=3), in0=oav[:, :, :, 64], scalar1=1e-30, scalar2=None, op0=ALU.max),
             reads=[("oacc", b, 0), ("oacc", b, 1), ("oacc", b, 2), "rzc"], writes=["rz"])
        P.op("dve", lambda e: e.reciprocal(out=rz[:], in_=rz[:]), reads=["rz"], writes=["rz"])
        P.op("dve", lambda e, qb=qb: e.tensor_tensor(out=coef[:].rearrange("p (x h) -> p x h", x=3), in0=rz[:].rearrange("p (x h) -> p x h", x=3),
                                                      in1=gsig[:, qb, :].rearrange("p (h x) -> p x h", x=3), op=ALU.mult),
             reads=["rz", "gsig"], writes=["coef"])
        for h in range(4):
            for x_ in range(3):
                src = oa_[:, x_, h * 65:h * 65 + 64]
                dst = ofin[:, h * 64:(h + 1) * 64]
                if x_ == 0:
                    P.op("dve", lambda e, src=src, dst=dst, h=h, x_=x_: e.tensor_scalar(out=dst, in0=src, scalar1=coef[:, x_ * 4 + h:x_ * 4 + h + 1], scalar2=None, op0=ALU.mult),
                         reads=[("oacc", b, x_), "coef"], writes=[("ofin", h)])
                else:
                    P.op("dve", lambda e, src=src, dst=dst, h=h, x_=x_: e.scalar_tensor_tensor(out=dst, in0=src, scalar=coef[:, x_ * 4 + h:x_ * 4 + h + 1], in1=dst,
                                                                                             op0=ALU.mult, op1=ALU.add),
                         reads=[("oacc", b, x_), "coef", ("ofin", h)], writes=[("ofin", h)])
        P.op("act", lambda e: e.activation(out=ofb[:], in_=ofin[:], func=AF.Copy), reads=[("ofin", h_) for h_ in range(4)], writes=["ofb"])

        def finish(b=b, t0=t0):
            for f in range(2):
                P.op("pe", lambda e, f=f: e.matmul(banks[B_M][:, f * 128:(f + 1) * 128], ofb[:, f * 128:(f + 1) * 128], idb[:], start=True, stop=True),
                     reads=["ofb", "idb"], writes=[("bank", B_M)])
            P.op("act", lambda e, b=b: e.activation(out=oT[b][:], in_=banks[B_M][:, 0:256].rearrange("p (f q) -> p f q", f=2), func=AF.Copy),
                 reads=[("bank", B_M)], writes=[("oT", b)])
            for f in range(2):
                P.dma("sp", odst(f * 128, t0, 128), oT[b][:, f, :], reads=[("oT", b)], writes=otok(t0))
            if after_store is not None:
                after_store(t0 + 128)
        deferred.append(finish)
    while deferred:
        deferred.pop(0)()
    return cx.done()


D_FFE = 3584
N_EXP = 8


def build_moe(T, GT=2048, cx=None):
    cx = cx or Ctx()
    nc, P = cx.nc, cx.P
    I = cx.inp
    xT = I("xT", [1024, T])
    wr = I("wr", [1024, N_EXP])
    wg = I("mwg", [N_EXP, 1024, D_FFE]); wu = I("mwu", [N_EXP, 1024, D_FFE]); wd = I("mwd", [N_EXP, D_FFE, 1024])
    lngb = I("lngb", [2, 1024])
    ident_d = cx.inp("ident", [128, 128], shared=True)
    out = cx.out("out", [T, 1024])
    GT = min(GT, T)
    NSUB = GT // 128
    NTT = GT // 512
    NSL = D_FFE // 512
    banks = cx.banks
    idf = P.sb([128, 128], F32)
    P.dma("sp", idf[:], ident_d, writes=["idf"])
    acc = P.sb([128, NSUB, 1024], F32)
    xb = P.sb([128, 8, GT], BF16)
    xf = [P.sb([128, 8, 128], F32) for _ in range(2)]
    wrs = P.sb([128, 8, N_EXP], F32)
    wgs = [P.sb([128, 8, 512], BF16) for _ in range(2)]
    wus = [P.sb([128, 8, 512], BF16) for _ in range(2)]
    wds = [P.sb([128, 4, 1024], BF16) for _ in range(2)]
    hs = [P.sb([128, 4, 512], BF16) for _ in range(2)]
    sg = [P.sb([128, 512], F32) for _ in range(2)]
    lg = P.sb([128, N_EXP], F32); m8 = P.sb([128, 8], F32); gt = P.sb([128, 4], F32); eq = P.sb([128, N_EXP], F32)
    wts = P.sb([128, NSUB, N_EXP], F32)
    gbc = P.sb([128, 2, 1024], F32)
    st = P.sb([128, 8], F32)
    junk = P.sb([128, 1024], F32)
    P.dma("sp", wrs[:], wr.rearrange("(c p) e -> p c e", p=128), writes=["wr"])
    P.dma("sp", gbc[:, 0, :], lngb[0:1, :].partition_broadcast(128), writes=["gbc"])
    P.dma("sp", gbc[:, 1, :], lngb[1:2, :].partition_broadcast(128), writes=["gbc"])
    xTv = xT.rearrange("(c p) t -> p c t", p=128)
    pr = [0]

    def rb(lo, n):
        i = lo + pr[0] % n
        pr[0] += 1
        return i
    nload = [0]
    for g0 in range(0, T, GT):
        P.dma("pool", xb[:], xTv[:, :, g0:g0 + GT], writes=["xb"])
        for sub in range(NSUB):
            fb = sub % 2
            P.dma("sp", xf[fb][:], xTv[:, :, g0 + sub * 128:g0 + (sub + 1) * 128], writes=[("xf", fb)])
            mm_acc(P, banks[7][:, 0:N_EXP], [(xf[fb][:, kc, :], wrs[:, kc, :]) for kc in range(8)], [("xf", fb), "wr"], [("bank", 7)])
            P.op("dve", lambda e: e.tensor_copy(out=lg[:], in_=banks[7][:, 0:N_EXP]), reads=[("bank", 7)], writes=["lg"])
            P.op("dve", lambda e: e.max(out=m8[:], in_=lg[:]), reads=["lg"], writes=["m8"])
            P.op("dve", lambda e: e.tensor_tensor(out=gt[:, 0:1], in0=m8[:, 1:2], in1=m8[:, 0:1], op=ALU.subtract), reads=["m8"], writes=["gt"])
            P.op("act", lambda e: e.activation(out=gt[:, 0:1], in_=gt[:, 0:1], func=AF.Exp), reads=["gt"], writes=["gt"])
            P.op("dve", lambda e: e.tensor_scalar(out=gt[:, 0:1], in0=gt[:, 0:1], scalar1=1.0, scalar2=None, op0=ALU.add), reads=["gt"], writes=["gt"])
            P.op("dve", lambda e: e.reciprocal(out=gt[:, 1:2], in_=gt[:, 0:1]), reads=["gt"], writes=["gt"])
            P.op("dve", lambda e: e.tensor_scalar(out=gt[:, 2:3], in0=gt[:, 1:2], scalar1=-1.0, scalar2=1.0, op0=ALU.mult, op1=ALU.add),
                 reads=["gt"], writes=["gt"])
            P.op("dve", lambda e: e.tensor_scalar(out=eq[:], in0=lg[:], scalar1=m8[:, 0:1], scalar2=gt[:, 1:2], op0=ALU.is_equal, op1=ALU.mult),
                 reads=["lg", "m8", "gt"], writes=["eq"])
            P.op("dve", lambda e, sub=sub: e.tensor_scalar(out=wts[:, sub, :], in0=lg[:], scalar1=m8[:, 1:2], scalar2=gt[:, 2:3], op0=ALU.is_equal, op1=ALU.mult),
                 reads=["lg", "m8", "gt"], writes=["wts"])
            P.op("dve", lambda e, sub=sub: e.tensor_tensor(out=wts[:, sub, :], in0=wts[:, sub, :], in1=eq[:], op=ALU.add), reads=["wts", "eq"], writes=["wts"])
            for half in range(2):
                bt = 4 + half
                for c4 in range(4):
                    c = half * 4 + c4
                    P.op("pe", lambda e, fb=fb, c=c, c4=c4, bt=bt: e.matmul(banks[bt][:, c4 * 128:(c4 + 1) * 128], xf[fb][:, c, :], idf[:], start=True, stop=True),
                         reads=[("xf", fb), "idf"], writes=[("bank", bt)])
                P.op("act", lambda e, sub=sub, half=half, bt=bt: e.activation(out=acc[:, sub, half * 512:(half + 1) * 512], in_=banks[bt], func=AF.Copy,
                                                                              scale=float(DN_ALPHA)),
                     reads=[("bank", bt)], writes=[("acc", sub)])
        for ex in range(N_EXP):
            for s in range(NSL):
                wbuf = nload[0] % 2
                nload[0] += 1
                fs = slice(s * 512, (s + 1) * 512)
                P.dma("pool", wgs[wbuf][:], wg[ex, :, fs].rearrange("(c p) n -> p c n", p=128), writes=[("wgs", wbuf)])
                P.dma("pool", wus[wbuf][:], wu[ex, :, fs].rearrange("(c p) n -> p c n", p=128), writes=[("wus", wbuf)])
                P.dma("pool", wds[wbuf][:], wd[ex, fs, :].rearrange("(c p) n -> p c n", p=128), writes=[("wds", wbuf)])
                for tt in range(NTT):
                    hb = tt % 2
                    tsl = slice(tt * 512, (tt + 1) * 512)
                    for m in range(4):
                        ms = slice(m * 128, (m + 1) * 128)
                        bg = rb(0, 2); bu = 2 + (bg % 2)
                        mm_acc(P, banks[bg], [(wgs[wbuf][:, kc, ms], xb[:, kc, tsl]) for kc in range(8)], [("wgs", wbuf), "xb"], [("bank", bg)])
                        mm_acc(P, banks[bu], [(wus[wbuf][:, kc, ms], xb[:, kc, tsl]) for kc in range(8)], [("wus", wbuf), "xb"], [("bank", bu)])
                        P.op("act", lambda e, bg=bg: e.activation(out=sg[bg][:], in_=banks[bg], func=AF.Silu), reads=[("bank", bg)], writes=[("sg", bg)])
                        P.op("dve", lambda e, bg=bg, bu=bu, hb=hb, m=m: e.tensor_tensor(out=hs[hb][:, m, :], in0=sg[bg][:], in1=banks[bu], op=ALU.mult),
                             reads=[("sg", bg), ("bank", bu)], writes=[("hs", hb)])
                    for sub4 in range(4):
                        sub = tt * 4 + sub4
                        for half in range(2):
                            bd = 4 + (sub4 * 2 + half) % 3
                            mm_acc(P, banks[bd], [(hs[hb][:, m, sub4 * 128:(sub4 + 1) * 128], wds[wbuf][:, m, half * 512:(half + 1) * 512]) for m in range(4)],
                                   [("hs", hb), ("wds", wbuf)], [("bank", bd)])
                            P.op("dve", lambda e, bd=bd, sub=sub, half=half, ex=ex: e.scalar_tensor_tensor(
                                out=acc[:, sub, half * 512:(half + 1) * 512], in0=banks[bd], scalar=wts[:, sub, ex:ex + 1],
                                in1=acc[:, sub, half * 512:(half + 1) * 512], op0=ALU.mult, op1=ALU.add),
                                reads=[("bank", bd), "wts", ("acc", sub)], writes=[("acc", sub)])
        for sub in range(NSUB):
            a = acc[:, sub, :]
            P.op("act", lambda e, a=a: e.activation(out=junk[:], in_=a, func=AF.Copy, accum_out=st[:, 0:1]), reads=[("acc", sub)], writes=["junk", "st"])
            P.op("act", lambda e, a=a: e.activation(out=junk[:], in_=a, func=AF.Square, accum_out=st[:, 1:2]), reads=[("acc", sub)], writes=["junk", "st"])
            P.op("dve", lambda e: e.tensor_scalar(out=st[:, 0:2], in0=st[:, 0:2], scalar1=1.0 / 1024, scalar2=None, op0=ALU.mult), reads=["st"], writes=["st"])
            P.op("dve", lambda e: e.tensor_tensor(out=st[:, 2:3], in0=st[:, 0:1], in1=st[:, 0:1], op=ALU.mult), reads=["st"], writes=["st"])
            P.op("dve", lambda e: e.tensor_tensor(out=st[:, 2:3], in0=st[:, 1:2], in1=st[:, 2:3], op=ALU.subtract), reads=["st"], writes=["st"])
            P.op("dve", lambda e: e.tensor_scalar(out=st[:, 2:3], in0=st[:, 2:3], scalar1=LN_EPS, scalar2=None, op0=ALU.add), reads=["st"], writes=["st"])
            P.op("act", lambda e: e.activation(out=st[:, 2:3], in_=st[:, 2:3], func=AF.Sqrt), reads=["st"], writes=["st"])
            P.op("dve", lambda e: e.reciprocal(out=st[:, 2:3], in_=st[:, 2:3]), reads=["st"], writes=["st"])
            P.op("dve", lambda e, a=a: e.tensor_scalar(out=a, in0=a, scalar1=st[:, 0:1], scalar2=st[:, 2:3], op0=ALU.subtract, op1=ALU.mult),
                 reads=["st", ("acc", sub)], writes=[("acc", sub)])
            P.op("dve", lambda e, a=a: e.tensor_tensor(out=a, in0=a, in1=gbc[:, 0, :], op=ALU.mult), reads=["gbc", ("acc", sub)], writes=[("acc", sub)])
            P.op("dve", lambda e, a=a: e.tensor_tensor(out=a, in0=a, in1=gbc[:, 1, :], op=ALU.add), reads=["gbc", ("acc", sub)], writes=[("acc", sub)])
        P.dma("sp", out[g0:g0 + GT, :].rearrange("(s p) d -> p s d", p=128), acc[:], reads=[("acc", s) for s in range(NSUB)])
    return cx.done()


MOE_CAP = 1408
MOE_SL = 256
MOE_BIG = 1.0e6
MOE_DEBUG = False


def build_moe2(T, cx=None, CAP=None):
    cx = cx or Ctx()
    nc, P = cx.nc, cx.P
    I = cx.inp
    CAP = CAP or min(MOE_CAP, ((T * 2) // N_EXP * 5 // 4 + 127) // 128 * 128 + 128)
    xT = I("xT", [1024, T])
    wr = I("wr", [1024, N_EXP])
    wg = I("mwg", [N_EXP, 1024, D_FFE]); wu = I("mwu", [N_EXP, 1024, D_FFE]); wd = I("mwd", [N_EXP, D_FFE, 1024])
    lngb = I("lngb", [2, 1024])
    ident_d = cx.inp("ident", [128, 128], shared=True)
    lstr_d = cx.inp("lstrict", [128, 128], shared=True)
    out = cx.out("out", [T, 1024])
    NSA_ = T // 128
    SL = MOE_SL
    NSL = D_FFE // SL
    MT = D_FFE // 128
    NROW = N_EXP * CAP
    xe = nc.dram_tensor(cx.prefix + "xe", [NROW, 1024], BF16)
    ye = nc.dram_tensor(cx.prefix + "ye", [NROW, 1024], F32)
    banks = cx.banks
    xTv = xT.rearrange("(c p) t -> p c t", p=128)
    base = (nc.sbuf_base, nc.sbuf_top)
    idf = P.sb([128, 128], F32); idb = P.sb([128, 128], BF16); lstr = P.sb([128, 128], BF16); onesb = P.sb([128, 128], BF16)
    wrs = P.sb([128, 8, N_EXP], F32)
    E1 = P.sb([128, NSA_, N_EXP], F32); E2 = P.sb([128, NSA_, N_EXP], F32); G = P.sb([128, NSA_, 2], F32)
    S1i = P.sb([128, NSA_], I32); S2i = P.sb([128, NSA_], I32)
    P.dma("sp", idf[:], ident_d, writes=["idf"])
    P.dma("pool", idb[:], ident_d, writes=["idb"])
    P.dma("pool", lstr[:], lstr_d, writes=["lstr"])
    P.op("dve", lambda e: e.memset(onesb[:], 1.0), writes=["onesb"])
    P.dma("sp", wrs[:], wr.rearrange("(c p) e -> p c e", p=128), writes=["wr"])
    base2 = (nc.sbuf_base, nc.sbuf_top)
    breg = {}

    def mk_breg(e):
        breg["r"] = e.alloc_register("moe_bound")
        return e.reg_mov(breg["r"], NROW - 1)
    P.op("pool", mk_breg)
    xf = [P.sb([128, 8, 128], F32) for _ in range(2)]
    xtmb = P.sb([128, NSA_, 1024], BF16)
    lg = P.sb([128, N_EXP], F32); m8 = P.sb([128, 8], F32); gt = P.sb([128, 4], F32)
    zt = P.sb([128, CAP // 128, 1024], BF16)
    P.op("pool", lambda e: e.memset(zt[:], 0.0), writes=["zt"])
    for ex in range(N_EXP):
        P.dma("sp", xe.ap()[ex * CAP:(ex + 1) * CAP, :].rearrange("(n p) d -> p n d", p=128), zt[:], reads=["zt"], writes=["xe_zero"])
    for sub in range(NSA_):
        fb = sub % 2
        P.dma("sp", xf[fb][:], xTv[:, :, sub * 128:(sub + 1) * 128], writes=[("xf", fb)])
        mm_acc(P, banks[7][:, 0:N_EXP], [(xf[fb][:, kc, :], wrs[:, kc, :]) for kc in range(8)], [("xf", fb), "wr"], [("bank", 7)])
        P.op("dve", lambda e: e.tensor_copy(out=lg[:], in_=banks[7][:, 0:N_EXP]), reads=[("bank", 7)], writes=["lg"])
        P.op("dve", lambda e: e.max(out=m8[:], in_=lg[:]), reads=["lg"], writes=["m8"])
        P.op("dve", lambda e: e.tensor_tensor(out=gt[:, 0:1], in0=m8[:, 1:2], in1=m8[:, 0:1], op=ALU.subtract), reads=["m8"], writes=["gt"])
        P.op("act", lambda e: e.activation(out=gt[:, 0:1], in_=gt[:, 0:1], func=AF.Exp), reads=["gt"], writes=["gt"])
        P.op("dve", lambda e: e.tensor_scalar(out=gt[:, 0:1], in0=gt[:, 0:1], scalar1=1.0, scalar2=None, op0=ALU.add), reads=["gt"], writes=["gt"])
        P.op("dve", lambda e, sub=sub: e.reciprocal(out=G[:, sub, 0:1], in_=gt[:, 0:1]), reads=["gt"], writes=["G"])
        P.op("dve", lambda e, sub=sub: e.tensor_scalar(out=G[:, sub, 1:2], in0=G[:, sub, 0:1], scalar1=-1.0, scalar2=1.0, op0=ALU.mult, op1=ALU.add),
             reads=["G"], writes=["G"])
        P.op("dve", lambda e, sub=sub: e.tensor_scalar(out=E1[:, sub, :], in0=lg[:], scalar1=m8[:, 0:1], scalar2=None, op0=ALU.is_equal),
             reads=["lg", "m8"], writes=["E1"])
        P.op("dve", lambda e, sub=sub: e.tensor_scalar(out=E2[:, sub, :], in0=lg[:], scalar1=m8[:, 1:2], scalar2=None, op0=ALU.is_equal),
             reads=["lg", "m8"], writes=["E2"])
        for half in range(2):
            bt = 4 + half
            for c4 in range(4):
                c = half * 4 + c4
                P.op("pe", lambda e, fb=fb, c=c, c4=c4, bt=bt: e.matmul(banks[bt][:, c4 * 128:(c4 + 1) * 128], xf[fb][:, c, :], idf[:], start=True, stop=True),
                     reads=[("xf", fb), "idf"], writes=[("bank", bt)])
            P.op("act", lambda e, sub=sub, half=half, bt=bt: e.activation(out=xtmb[:, sub, half * 512:(half + 1) * 512], in_=banks[bt], func=AF.Copy),
                 reads=[("bank", bt)], writes=[("xtmb", sub)])
    Mb = P.sb([128, NSA_ * N_EXP], BF16)
    Mf = P.sb([128, NSA_, N_EXP], F32); tots = P.sb([128, NSA_, N_EXP], F32); incl = P.sb([128, NSA_, N_EXP], F32)
    pos = P.sb([128, NSA_, N_EXP], F32); tmp = P.sb([128, NSA_, N_EXP], F32); onesf = P.sb([128, NSA_], F32)
    s12 = P.sb([128, 2, NSA_], F32)
    NE = NSA_ * N_EXP
    P.op("dve", lambda e: e.tensor_tensor(out=Mf[:], in0=E1[:], in1=E2[:], op=ALU.add), reads=["E1", "E2"], writes=["Mf"])
    P.op("dve", lambda e: e.tensor_copy(out=Mb[:], in_=Mf[:].rearrange("p s e -> p (s e)")), reads=["Mf"], writes=["Mb"])
    P.op("dve", lambda e: e.memset(onesf[:], 1.0), writes=["onesf"])
    P.op("pe", lambda e: e.matmul(banks[0][:, 0:NE], lstr[:], Mb[:], start=True, stop=True), reads=["lstr", "Mb"], writes=[("bank", 0)])
    P.op("pe", lambda e: e.matmul(banks[1][:, 0:NE], onesb[:], Mb[:], start=True, stop=True), reads=["onesb", "Mb"], writes=[("bank", 1)])
    P.op("dve", lambda e: e.tensor_copy(out=tots[:].rearrange("p s e -> p (s e)"), in_=banks[1][:, 0:NE]), reads=[("bank", 1)], writes=["tots"])
    if MOE_DEBUG:
        dbg = nc.dram_tensor("dbg_tots", [128, NSA_ * N_EXP], F32, kind="ExternalOutput").ap()
        P.dma("sp", dbg, tots[:].rearrange("p s e -> p (s e)"), reads=["tots"])
    for ex in range(N_EXP):
        P.op("dve", lambda e, ex=ex: e.tensor_tensor_scan(out=incl[:, :, ex], data0=onesf[:], data1=tots[:, :, ex], initial=0.0, op0=ALU.mult, op1=ALU.add),
             reads=["tots", "onesf"], writes=["incl"])
    P.op("dve", lambda e: e.tensor_tensor(out=pos[:].rearrange("p s e -> p (s e)"), in0=banks[0][:, 0:NE], in1=incl[:].rearrange("p s e -> p (s e)"), op=ALU.add),
         reads=[("bank", 0), "incl"], writes=["pos"])
    P.op("dve", lambda e: e.tensor_tensor(out=pos[:], in0=pos[:], in1=tots[:], op=ALU.subtract), reads=["pos", "tots"], writes=["pos"])
    P.op("dve", lambda e: e.tensor_scalar(out=tmp[:], in0=pos[:], scalar1=float(CAP), scalar2=MOE_BIG, op0=ALU.is_ge, op1=ALU.mult), reads=["pos"], writes=["tmp"])
    P.op("dve", lambda e: e.tensor_tensor(out=pos[:], in0=pos[:], in1=tmp[:], op=ALU.add), reads=["pos", "tmp"], writes=["pos"])
    for ex in range(1, N_EXP):
        P.op("dve", lambda e, ex=ex: e.tensor_scalar(out=pos[:, :, ex], in0=pos[:, :, ex], scalar1=float(ex * CAP), scalar2=None, op0=ALU.add),
             reads=["pos"], writes=["pos"])
    for k_, EE in enumerate((E1, E2)):
        P.op("dve", lambda e, EE=EE: e.tensor_tensor(out=tmp[:], in0=pos[:], in1=EE[:], op=ALU.mult), reads=["pos", "E1", "E2", "s12"], writes=["tmp"])
        P.op("dve", lambda e, k_=k_: e.reduce_sum(out=s12[:, k_, :], in_=tmp[:], axis=AX.X), reads=["tmp"], writes=["s12"])
    P.op("dve", lambda e: e.tensor_copy(out=S1i[:], in_=s12[:, 0, :]), reads=["s12"], writes=["S1i"])
    P.op("dve", lambda e: e.tensor_copy(out=S2i[:], in_=s12[:, 1, :]), reads=["s12"], writes=["S2i"])
    for sub in range(NSA_):
        for Si, tk in ((S1i, "S1i"), (S2i, "S2i")):
            P.op("pool", lambda e, sub=sub, Si=Si: e.indirect_dma_start(
                out=xe.ap(), out_offset=bass.IndirectOffsetOnAxis(ap=Si[:, sub:sub + 1], axis=0), in_=xtmb[:, sub, :], in_offset=None,
                bounds_check=breg["r"], oob_is_err=False), reads=[tk, ("xtmb", sub), "xe_zero"], dma=True)
    P.barrier()
    nc.sbuf_base, nc.sbuf_top = base2
    NST = CAP // 128
    tiles = []
    t_ = 0
    while t_ < CAP:
        n_ = min(512, CAP - t_)
        tiles.append((t_, n_))
        t_ += n_
    xrow = [P.sb([128, 1024], BF16) for _ in range(2)]
    xeT = P.sb([128, 8, CAP], BF16)
    hT = P.sb([128, MT, CAP], BF16)
    wgs = [P.sb([128, 8, SL], BF16) for _ in range(2)]
    wus = [P.sb([128, 8, SL], BF16) for _ in range(2)]
    wds = P.sb([128, MT, 1024], BF16)
    sg = [P.sb([128, 512], F32) for _ in range(2)]
    yb = [P.sb([128, 1024], F32) for _ in range(2)]
    nl = 0
    nrow = 0
    for ex in range(N_EXP):
        P.dma("pool", wds[:], wd[ex].rearrange("(c p) n -> p c n", p=128), writes=["wds"])
        for stl in range(NST):
            rb_ = nrow % 2
            nrow += 1
            r0 = ex * CAP + stl * 128
            P.dma("sp", xrow[rb_][:], xe.ap()[r0:r0 + 128, :], writes=[("xrow", rb_)])
            for half in range(2):
                bt = 4 + half
                for c4 in range(4):
                    c = half * 4 + c4
                    P.op("pe", lambda e, rb_=rb_, c=c, c4=c4, bt=bt: e.matmul(banks[bt][:, c4 * 128:(c4 + 1) * 128], xrow[rb_][:, c * 128:(c + 1) * 128], idb[:],
                                                                             start=True, stop=True),
                         reads=[("xrow", rb_), "idb"], writes=[("bank", bt)])
                P.op("act" if half == 0 else "dve",
                     (lambda e, half=half, stl=stl, bt=bt: e.activation(out=xeT[:, half * 4:(half + 1) * 4, stl * 128:(stl + 1) * 128],
                                                                         in_=banks[bt].rearrange("p (c t) -> p c t", c=4), func=AF.Copy)) if half == 0 else
                     (lambda e, half=half, stl=stl, bt=bt: e.tensor_copy(out=xeT[:, half * 4:(half + 1) * 4, stl * 128:(stl + 1) * 128],
                                                                          in_=banks[bt].rearrange("p (c t) -> p c t", c=4))),
                     reads=[("bank", bt)], writes=["xeT"])
        for s_ in range(NSL):
            wb_ = nl % 2
            nl += 1
            fs = slice(s_ * SL, (s_ + 1) * SL)
            P.dma("pool", wgs[wb_][:], wg[ex, :, fs].rearrange("(c p) n -> p c n", p=128), writes=[("wgs", wb_)])
            P.dma("pool", wus[wb_][:], wu[ex, :, fs].rearrange("(c p) n -> p c n", p=128), writes=[("wus", wb_)])
            for (t0_, n_) in tiles:
                for m in range(SL // 128):
                    ms = slice(m * 128, (m + 1) * 128)
                    pb = m % 2
                    bg, bu = pb, 2 + pb
                    mm_acc(P, banks[bg][:, 0:n_], [(wgs[wb_][:, kc, ms], xeT[:, kc, t0_:t0_ + n_]) for kc in range(8)], [("wgs", wb_), "xeT"], [("bank", bg)])
                    mm_acc(P, banks[bu][:, 0:n_], [(wus[wb_][:, kc, ms], xeT[:, kc, t0_:t0_ + n_]) for kc in range(8)], [("wus", wb_), "xeT"], [("bank", bu)])
                    P.op("act", lambda e, bg=bg, n_=n_: e.activation(out=sg[bg][:, 0:n_], in_=banks[bg][:, 0:n_], func=AF.Silu), reads=[("bank", bg)], writes=[("sg", bg)])
                    P.op("dve", lambda e, bg=bg, bu=bu, n_=n_, t0_=t0_, mm_=s_ * (SL // 128) + m: e.tensor_tensor(out=hT[:, mm_, t0_:t0_ + n_], in0=sg[bg][:, 0:n_], in1=banks[bu][:, 0:n_], op=ALU.mult),
                         reads=[("sg", bg), ("bank", bu)], writes=[("hT", s_ * (SL // 128) + m)])
        for stl in range(NST):
            yb_ = yb[stl % 2]
            for half in range(2):
                bd = 4 + (stl * 2 + half) % 3
                mm_acc(P, banks[bd], [(hT[:, m, stl * 128:(stl + 1) * 128], wds[:, m, half * 512:(half + 1) * 512]) for m in range(MT)],
                       [[("hT", m), "wds"] for m in range(MT)], [("bank", bd)])
                if half == 0:
                    P.op("act", lambda e, bd=bd, yb_=yb_: e.activation(out=yb_[:, 0:512], in_=banks[bd], func=AF.Copy), reads=[("bank", bd)], writes=[("yb", stl % 2)])
                else:
                    P.op("dve", lambda e, bd=bd, yb_=yb_: e.tensor_copy(out=yb_[:, 512:1024], in_=banks[bd]), reads=[("bank", bd)], writes=[("yb", stl % 2)])
            r0 = ex * CAP + stl * 128
            P.dma("sp", ye.ap()[r0:r0 + 128, :], yb_[:], reads=[("yb", stl % 2)])
    P.barrier()
    nc.sbuf_base, nc.sbuf_top = base2
    xf = [P.sb([128, 8, 128], F32) for _ in range(2)]
    y1 = [P.sb([128, 1024], F32) for _ in range(2)]
    y2 = [P.sb([128, 1024], F32) for _ in range(2)]
    accs = [P.sb([128, 1024], F32) for _ in range(2)]
    gbc = P.sb([128, 2, 1024], F32)
    P.dma("sp", gbc[:, 0, :], lngb[0:1, :].partition_broadcast(128), writes=["gbc"])
    P.dma("sp", gbc[:, 1, :], lngb[1:2, :].partition_broadcast(128), writes=["gbc"])
    sts = [P.sb([128, 8], F32) for _ in range(2)]
    junks = [P.sb([128, 1024], F32) for _ in range(2)]
    for sub in range(NSA_):
        fb = sub % 2
        a = accs[fb]
        st = sts[fb]
        junk = junks[fb]
        P.dma("sp", xf[fb][:], xTv[:, :, sub * 128:(sub + 1) * 128], writes=[("xf", fb)])
        for yy, Si, tk in ((y1, S1i, "y1"), (y2, S2i, "y2")):
            P.op("pool", lambda e, yy=yy, fb=fb: e.memset(yy[fb][:], 0.0), writes=[(tk, fb)])
            P.op("pool", lambda e, yy=yy, fb=fb, Si=Si, sub=sub: e.indirect_dma_start(
                out=yy[fb][:], out_offset=None, in_=ye.ap(), in_offset=bass.IndirectOffsetOnAxis(ap=Si[:, sub:sub + 1], axis=0),
                bounds_check=breg["r"], oob_is_err=False), reads=["S1i", "S2i"], writes=[(tk, fb)], dma=True)
        for half in range(2):
            bt = 4 + half
            for c4 in range(4):
                c = half * 4 + c4
                P.op("pe", lambda e, fb=fb, c=c, c4=c4, bt=bt: e.matmul(banks[bt][:, c4 * 128:(c4 + 1) * 128], xf[fb][:, c, :], idf[:], start=True, stop=True),
                     reads=[("xf", fb), "idf"], writes=[("bank", bt)])
            P.op("act", lambda e, a=a, half=half, bt=bt: e.activation(out=a[:, half * 512:(half + 1) * 512], in_=banks[bt], func=AF.Copy, scale=float(DN_ALPHA)),
                 reads=[("bank", bt)], writes=[("acc", fb)])
        P.op("dve", lambda e, a=a, fb=fb, sub=sub: e.scalar_tensor_tensor(out=a[:], in0=y1[fb][:], scalar=G[:, sub, 0:1], in1=a[:], op0=ALU.mult, op1=ALU.add),
             reads=[("y1", fb), "G", ("acc", fb)], writes=[("acc", fb)])
        P.op("dve", lambda e, a=a, fb=fb, sub=sub: e.scalar_tensor_tensor(out=a[:], in0=y2[fb][:], scalar=G[:, sub, 1:2], in1=a[:], op0=ALU.mult, op1=ALU.add),
             reads=[("y2", fb), "G", ("acc", fb)], writes=[("acc", fb)])
        T_ = ("acc", fb)
        P.op("act", lambda e, a=a, st=st, junk=junk: e.activation(out=junk[:], in_=a[:], func=AF.Copy, accum_out=st[:, 0:1]), reads=[T_], writes=[("junk", fb), ("st", fb)])
        P.op("act", lambda e, a=a, st=st, junk=junk: e.activation(out=junk[:], in_=a[:], func=AF.Square, accum_out=st[:, 1:2]), reads=[T_], writes=[("junk", fb), ("st", fb)])
        P.op("dve", lambda e, st=st: e.tensor_scalar(out=st[:, 0:2], in0=st[:, 0:2], scalar1=1.0 / 1024, scalar2=None, op0=ALU.mult), reads=[("st", fb)], writes=[("st", fb)])
        P.op("dve", lambda e, st=st: e.tensor_tensor(out=st[:, 2:3], in0=st[:, 0:1], in1=st[:, 0:1], op=ALU.mult), reads=[("st", fb)], writes=[("st", fb)])
        P.op("dve", lambda e, st=st: e.tensor_tensor(out=st[:, 2:3], in0=st[:, 1:2], in1=st[:, 2:3], op=ALU.subtract), reads=[("st", fb)], writes=[("st", fb)])
        P.op("dve", lambda e, st=st: e.tensor_scalar(out=st[:, 2:3], in0=st[:, 2:3], scalar1=LN_EPS, scalar2=None, op0=ALU.add), reads=[("st", fb)], writes=[("st", fb)])
        P.op("act", lambda e, st=st: e.activation(out=st[:, 2:3], in_=st[:, 2:3], func=AF.Sqrt), reads=[("st", fb)], writes=[("st", fb)])
        P.op("dve", lambda e, st=st: e.reciprocal(out=st[:, 2:3], in_=st[:, 2:3]), reads=[("st", fb)], writes=[("st", fb)])
        P.op("dve", lambda e, a=a, st=st, junk=junk: e.tensor_scalar(out=a[:], in0=a[:], scalar1=st[:, 0:1], scalar2=st[:, 2:3], op0=ALU.subtract, op1=ALU.mult), reads=[("st", fb), T_], writes=[T_])
        P.op("dve", lambda e, a=a, st=st, junk=junk: e.tensor_tensor(out=a[:], in0=a[:], in1=gbc[:, 0, :], op=ALU.mult), reads=["gbc", T_], writes=[T_])
        P.op("dve", lambda e, a=a, st=st, junk=junk: e.tensor_tensor(out=a[:], in0=a[:], in1=gbc[:, 1, :], op=ALU.add), reads=["gbc", T_], writes=[T_])
        P.dma("sp", out[sub * 128:(sub + 1) * 128, :], a[:], reads=[T_])
    if not cx.standalone:
        nc.sbuf_base, nc.sbuf_top = base
    return cx.done()


from concourse.bass_utils import run_bass_kernel_spmd

S_FULL = 8192
_PROGS = {}


def _prog(key, fn):
    if key not in _PROGS:
        _PROGS[key] = fn()
    return _PROGS[key]


def _c(a):
    return np.ascontiguousarray(a, dtype=np.float32)


def _pc(v):
    return _c(np.asarray(v).reshape(8, 128).T)


def _run(nc, in_maps):
    res = run_bass_kernel_spmd(nc, in_maps, core_ids=list(range(8)))
    return res.results


def kernel_unfused(x, w_in, hg_lb_logits, hg_norm_w, cmp_pos, cmp_w1, cmp_w2, rel_bias, w_branch_a, w_branch_b,
           w_out, ln1_g, ln1_b, ln2_g, ln2_b, ffn_w_gate, ffn_w_up, ffn_w_down, moe_router, moe_w_gate,
           moe_w_up, moe_w_down):
    f = lambda a: np.asarray(a, dtype=np.float32)
    x = f(x); w_in = f(w_in); hg_lb_logits = f(hg_lb_logits); hg_norm_w = f(hg_norm_w)
    cmp_pos = f(cmp_pos); cmp_w1 = f(cmp_w1); cmp_w2 = f(cmp_w2); rel_bias = f(rel_bias)
    B, S, D = x.shape
    NT = 512
    cmask = np.ones((128, NT), np.float32); cmask[:, ::64] = 0
    tri = np.zeros((64, NT), np.float32)
    for c in range(NT // 64):
        tri[:, c * 64:(c + 1) * 64] = np.triu(np.ones((64, 64), np.float32))
    ident = np.eye(128, dtype=np.float32)
    st = nsa_static()
    xT_b = [_c(x[b].T) for b in range(B)]
    out = None
    for l in range(DEPTH):
        W = w_in[l]
        nc_h = _prog(("hgrn", l), lambda: build_hgrn(S, NT, layer=l))
        maps = []
        for c in range(8):
            b, j = c // 2, c % 2
            cs = slice(j * 256, (j + 1) * 256)
            lbl = np.stack([hg_lb_logits[0, cs][:128], hg_lb_logits[0, cs][128:], hg_lb_logits[1, cs][:128], hg_lb_logits[1, cs][128:]], axis=1)
            nw = hg_norm_w[l, cs]
            hp = np.stack([np.ones(128, np.float32), np.ones(128, np.float32), nw[:128], nw[128:]], axis=1)
            maps.append(dict(xT=xT_b[b], hwq=_c(W[:, 0:512][:, cs]), hwf=_c(W[:, 512:1024][:, cs]), hwi=_c(W[:, 1024:1536][:, cs]),
                             hwg=_c(W[:, 1536:2048][:, cs]), hp=_c(hp), lbl=_c(lbl), cmask=cmask, tri=tri, ident=ident))
        r_h = _run(nc_h, maps)
        nc_n = _prog(("nsa",), lambda: build_nsa(S))
        maps = []
        for c in range(8):
            b, g = c // 2, c % 2
            kv = lambda base: W[:, base + g * 64: base + (g + 1) * 64]
            d = dict(xT=xT_b[b], nwq=_c(W[:, 2048 + g * 256:2048 + (g + 1) * 256]),
                     nwk=_c(np.concatenate([kv(2816), kv(3072), kv(2560), kv(2688)], 1)),
                     nwt=_c(np.concatenate([kv(2944), kv(3200), W[:, 3328 + g * 12:3328 + (g + 1) * 12]], 1)),
                     w1=_c(cmp_w1[l].reshape(2, 32, 64, 128).transpose(0, 2, 1, 3)), posT=_c(cmp_pos[l].transpose(0, 2, 1)), w2=_c(cmp_w2[l]),
                     tabaug=_c(np.concatenate([rel_bias[:, g * 4:(g + 1) * 4], np.full((1, 4), NEG, np.float32)], 0)))
            d.update(st)
            maps.append(d)
        r_n = _run(nc_n, maps)
        T = S // 2
        nc_m = _prog(("merge",), lambda: build_merge(T))
        lnp1 = _c(np.concatenate([_pc(ln1_g[l]), _pc(ln1_b[l])], axis=1))
        maps = []
        for c in range(8):
            b, hf = c // 2, c % 2
            ts = slice(hf * T, (hf + 1) * T)
            oaT = np.ascontiguousarray(np.concatenate([r_h[2 * b]["oaT"][:, ts], r_h[2 * b + 1]["oaT"][:, ts]], axis=0))
            obT = np.ascontiguousarray(np.concatenate([r_n[2 * b]["obT"][:, ts], r_n[2 * b + 1]["obT"][:, ts]], axis=0))
            maps.append(dict(xT=_c(xT_b[b][:, ts]), oaT=oaT, obT=obT, wgt=_c(W[:, 3352:5400]), wa=_c(f(w_branch_a)[l]), wb=_c(f(w_branch_b)[l]),
                             wo=_c(f(w_out)[l]), lnp=lnp1))
        r_m = _run(nc_m, maps)
        lnp2 = _c(np.concatenate([_pc(ln2_g[l]), _pc(ln2_b[l])], axis=1))
        if l % 2 == 0:
            nc_f = _prog(("ffn",), lambda: build_ffn(T))
            maps = [dict(xT=r_m[c]["outT"], wg=_c(f(ffn_w_gate)[l // 2]), wu=_c(f(ffn_w_up)[l // 2]), wd=_c(f(ffn_w_down)[l // 2]), lnp=lnp2)
                    for c in range(8)]
            r_f = _run(nc_f, maps)
            xT_b = [np.ascontiguousarray(np.concatenate([r_f[2 * b]["outT"], r_f[2 * b + 1]["outT"]], axis=1)) for b in range(B)]
            if l == DEPTH - 1:
                out = np.stack([xT_b[b].T for b in range(B)])
        else:
            nc_f = _prog(("moe",), lambda: build_moe(T))
            wgm, wum, wdm = _c(f(moe_w_gate)[l // 2]), _c(f(moe_w_up)[l // 2]), _c(f(moe_w_down)[l // 2])
            maps = [dict(xT=r_m[c]["outT"], wr=_c(f(moe_router)[l // 2]), mwg=wgm, mwu=wum, mwd=wdm, ident=ident,
                         lngb=_c(np.stack([f(ln2_g)[l], f(ln2_b)[l]]))) for c in range(8)]
            r_f = _run(nc_f, maps)
            xtm = [np.concatenate([r_f[2 * b]["out"], r_f[2 * b + 1]["out"]], axis=0) for b in range(B)]
            xT_b = [_c(a.T) for a in xtm]
            if l == DEPTH - 1:
                out = np.stack(xtm)
    return np.ascontiguousarray(out, dtype=np.float32)


def build_fused(S=S_FULL if False else 8192, ncores=8):
    T = S // 2
    nc = new_nc()
    P = Prog(nc)
    banks = [P.ps([128, 512]) for _ in range(8)]
    groups = [[2 * i, 2 * i + 1] for i in range(ncores // 2)]
    EI = lambda name, shape, dt=F32: nc.dram_tensor(name, list(shape), dt, kind="ExternalInput").ap()
    xT = EI("xT", [1024, S]); xTh = EI("xTh", [1024, T]); selh = EI("selh", [128, 2])
    out = nc.dram_tensor("out", [T, 1024], F32, kind="ExternalOutput").ap()
    OC = min(2048, S)
    XC = min(1024, T)
    NOC, NXC = S // OC, T // XC
    obuf = [[nc.dram_tensor(f"obuf{l}_{k}", [512, OC], BF16) for k in range(NOC)] for l in range(DEPTH)]
    og = [[nc.dram_tensor(f"og{l}_{k}", [1024, OC], BF16) for k in range(NOC)] for l in range(DEPTH)]
    x2b = [nc.dram_tensor(f"x2b_{k}", [1024, XC], BF16) for k in range(NXC)]
    xg = [nc.dram_tensor(f"xg_{k}", [2048, XC], BF16) for k in range(NXC)]
    x1f = [nc.dram_tensor(f"x1f_{l}", [1024, T], F32) for l in range(DEPTH)]
    x2f = nc.dram_tensor("x2f", [1024, T], F32)
    base = (nc.sbuf_base, nc.sbuf_top)

    P.persist = set()

    def phase_end(wait_cc=True):
        P.barrier(wait_cc=wait_cc)
        nc.sbuf_base, nc.sbuf_top = base

    xTv = xT.rearrange("(c p) t -> p c t", p=128)

    def xsrc0(t0, n):
        return xTv[:, :, t0:t0 + n], "pool"

    def xsrc1(t0, n):
        r, k, tt = t0 // T, (t0 % T) // XC, t0 % XC
        return xg[k].ap()[r * 1024:(r + 1) * 1024, tt:tt + n].rearrange("(c p) t -> p c t", p=128), "sp", [("xg", k)]

    for l in range(DEPTH):
        xsrc = xsrc0 if l == 0 else xsrc1

        def odst_h(row0, t0, n, l=l):
            return obuf[l][t0 // OC].ap()[row0:row0 + 128, t0 % OC:t0 % OC + n]

        def odst_n(row0, t0, n, l=l):
            return obuf[l][t0 // OC].ap()[256 + row0:256 + row0 + 128, t0 % OC:t0 % OC + n]
        build_hgrn(S, 512, layer=l, cx=Ctx(nc, P, banks, f"l{l}h_", dict(xsrc=xsrc, odst=odst_h)))
        phase_end()
        def o_after(tend, l=l):
            if tend % OC == 0:
                k = tend // OC - 1
                P.cc(lambda e, l=l, k=k: e.collective_compute("AllGather", ALU.bypass, replica_groups=groups,
                                                              ins=[obuf[l][k].ap().opt()], outs=[og[l][k].ap().opt()]),
                     reads=[("obuf", k)], writes=[("og", l, k)])
                P.persist.add(("og", l, k))
        build_nsa(S, cx=Ctx(nc, P, banks, f"l{l}n_", dict(xsrc=xsrc, odst=odst_n, otok=lambda t0: [("obuf", t0 // OC)], after_store=o_after)))
        phase_end(wait_cc=False)

        def osrc2(which, h, t0, n, l=l):
            g0 = h * T + t0
            k, tt = g0 // OC, g0 % OC
            off = 0 if which == "a" else 256
            return [(2 * r, 2 * r + 2, og[l][k].ap()[r * 512 + off:r * 512 + off + 256, tt:tt + n].rearrange("(c p) t -> p c t", p=128))
                    for r in range(2)]
        xres = xTh if l == 0 else x2f.ap()
        ctok = lambda h, t0, l=l: [("og", l, (h * T + t0) // OC)]
        build_merge(T, cx=Ctx(nc, P, banks, f"l{l}m_", dict(xT=xres, outT=x1f[l].ap(), osrc=True, osrc2=osrc2, selh=selh, ctok=ctok)))
        phase_end()
        if l % 2 == 0:
            def outb(t0, n):
                return x2b[t0 // XC].ap()[:, t0 % XC:t0 % XC + n].rearrange("(c p) t -> p c t", p=128)
            def x_after(tend):
                if tend % XC == 0:
                    k = tend // XC - 1
                    P.cc(lambda e, k=k: e.collective_compute("AllGather", ALU.bypass, replica_groups=groups,
                                                             ins=[x2b[k].ap().opt()], outs=[xg[k].ap().opt()]),
                         reads=[("x2b", k)], writes=[("xg", k)])
                    P.persist.add(("xg", k))
            build_ffn(T, cx=Ctx(nc, P, banks, f"l{l}f_", dict(xT=x1f[l].ap(), outT=x2f.ap(), outb=outb,
                                                              otok=lambda t0: [("x2b", t0 // XC)], after_store=x_after)))
            phase_end(wait_cc=False)
        else:
            build_moe2(T, cx=Ctx(nc, P, banks, f"l{l}e_", dict(xT=x1f[l].ap(), out=out)))
            phase_end()
    P.emit()
    return nc


def fused_in_maps(S, ncores, x, w_in, hg_lb_logits, hg_norm_w, cmp_pos, cmp_w1, cmp_w2, rel_bias, w_branch_a, w_branch_b,
                  w_out, ln1_g, ln1_b, ln2_g, ln2_b, ffn_w_gate, ffn_w_up, ffn_w_down, moe_router, moe_w_gate,
                  moe_w_up, moe_w_down):
    f = lambda a: np.asarray(a, dtype=np.float32)
    x = f(x); w_in = f(w_in); hg_lb_logits = f(hg_lb_logits); hg_norm_w = f(hg_norm_w)
    cmp_pos = f(cmp_pos); cmp_w1 = f(cmp_w1); cmp_w2 = f(cmp_w2); rel_bias = f(rel_bias)
    T = S // 2
    NT = 512
    cmask = np.ones((128, NT), np.float32); cmask[:, ::64] = 0
    tri = np.zeros((64, NT), np.float32)
    for c in range(NT // 64):
        tri[:, c * 64:(c + 1) * 64] = np.triu(np.ones((64, 64), np.float32))
    st = nsa_static()
    shared = dict(cmask=cmask, tri=tri, **st)
    per_layer = []
    for l in range(DEPTH):
        W = w_in[l]
        d = {}
        d[f"l{l}m_wgt"] = _c(W[:, 3352:5400]); d[f"l{l}m_wa"] = _c(f(w_branch_a)[l]); d[f"l{l}m_wb"] = _c(f(w_branch_b)[l])
        d[f"l{l}m_wo"] = _c(f(w_out)[l]); d[f"l{l}m_lnp"] = _c(np.concatenate([_pc(f(ln1_g)[l]), _pc(f(ln1_b)[l])], axis=1))
        d[f"l{l}n_w1"] = _c(cmp_w1[l].reshape(2, 32, 64, 128).transpose(0, 2, 1, 3)); d[f"l{l}n_posT"] = _c(cmp_pos[l].transpose(0, 2, 1))
        d[f"l{l}n_w2"] = _c(cmp_w2[l])
        if l % 2 == 0:
            d[f"l{l}f_wg"] = _c(f(ffn_w_gate)[l // 2]); d[f"l{l}f_wu"] = _c(f(ffn_w_up)[l // 2]); d[f"l{l}f_wd"] = _c(f(ffn_w_down)[l // 2])
            d[f"l{l}f_lnp"] = _c(np.concatenate([_pc(f(ln2_g)[l]), _pc(f(ln2_b)[l])], axis=1))
        else:
            d[f"l{l}e_wr"] = _c(f(moe_router)[l // 2]); d[f"l{l}e_mwg"] = _c(f(moe_w_gate)[l // 2]); d[f"l{l}e_mwu"] = _c(f(moe_w_up)[l // 2])
            d[f"l{l}e_mwd"] = _c(f(moe_w_down)[l // 2]); d[f"l{l}e_lngb"] = _c(np.stack([f(ln2_g)[l], f(ln2_b)[l]]))
        per_layer.append(d)
    maps = []
    for c in range(ncores):
        b, j = c // 2, c % 2
        xTb = _c(x[b].T)
        m = dict(xT=xTb, xTh=_c(xTb[:, j * T:(j + 1) * T]), selh=_c(np.tile(np.array([[1 - j, j]], np.float32), (128, 1))))
        m.update(shared)
        m["tabaug"] = _c(np.concatenate([rel_bias[:, j * 4:(j + 1) * 4], np.full((1, 4), NEG, np.float32)], 0))
        for l in range(DEPTH):
            W = w_in[l]
            m.update(per_layer[l])
            cs = slice(j * 256, (j + 1) * 256)
            m[f"l{l}h_hwq"] = _c(W[:, 0:512][:, cs]); m[f"l{l}h_hwf"] = _c(W[:, 512:1024][:, cs])
            m[f"l{l}h_hwi"] = _c(W[:, 1024:1536][:, cs]); m[f"l{l}h_hwg"] = _c(W[:, 1536:2048][:, cs])
            nw = hg_norm_w[l, cs]
            m[f"l{l}h_hp"] = _c(np.stack([np.ones(128, np.float32), np.ones(128, np.float32), nw[:128], nw[128:]], axis=1))
            m[f"l{l}h_lbl"] = _c(np.stack([hg_lb_logits[0, cs][:128], hg_lb_logits[0, cs][128:], hg_lb_logits[1, cs][:128], hg_lb_logits[1, cs][128:]], axis=1))
            g = j
            kv = lambda base: W[:, base + g * 64: base + (g + 1) * 64]
            m[f"l{l}n_nwq"] = _c(W[:, 2048 + g * 256:2048 + (g + 1) * 256])
            m[f"l{l}n_nwk"] = _c(np.concatenate([kv(2816), kv(3072), kv(2560), kv(2688)], 1))
            m[f"l{l}n_nwt"] = _c(np.concatenate([kv(2944), kv(3200), W[:, 3328 + g * 12:3328 + (g + 1) * 12]], 1))
        maps.append(m)
    return maps


def kernel(**inputs):
    x = np.asarray(inputs["x"])
    B, S, D = x.shape
    ncores = 2 * B
    nc = _prog(("fused", S, ncores), lambda: build_fused(S, ncores))
    maps = fused_in_maps(S, ncores, **inputs)
    res = run_bass_kernel_spmd(nc, maps, core_ids=list(range(ncores))).results
    T = S // 2
    out = np.empty((B, S, D), np.float32)
    for c in range(ncores):
        out[c // 2, (c % 2) * T:(c % 2 + 1) * T] = res[c]["out"]
    return out
```
